# Optimizing a Trainium2 kernel written in Bass

```python
import math
import jax, jax.numpy as jnp
from jax import lax
import numpy as np

D_MODEL = 1024
BATCH = 8
SEQ = 8192
DEPTH = 4

CTX_LEN = 256
GRID_W = 64
N_MIXERS = 3
EPS = 1e-6
ROPE_BASE = 10000.0
BLOCK = 128

A_HEADS = 8
A_DK = 128
A_DV = 128
A_CONV = 5
A_CHUNK = 64
A_QKV = 2 * A_HEADS * A_DK + A_HEADS * A_DV
A_IN = A_QKV + A_HEADS * A_DV + 4 * A_HEADS

B_QHEADS = 16
B_KVHEADS = 4
B_GROUP = B_QHEADS // B_KVHEADS
B_HD = 64
B_WINDOW = 128
B_IN = (B_QHEADS + 2 * B_KVHEADS) * B_HD

C_HEADS = 8
C_HD = 64
C_QK = C_HEADS * 2 * C_HD
C_IN = 3 * C_QK

N_EXPERTS = 16
EC_CAPACITY = 2
D_EXPERT = 1024

N_A = (DEPTH + N_MIXERS - 1) // N_MIXERS
N_B = (DEPTH + N_MIXERS - 2) // N_MIXERS
N_C = DEPTH // N_MIXERS

kernel_name = "hybrid_diffusion_delta_swa_diff_ec"

F32 = jnp.float32


def rmsnorm(x, g):
    xf = x.astype(F32)
    y = xf * lax.rsqrt(jnp.mean(xf * xf, -1, keepdims=True) + EPS)
    return (y * g.astype(F32)).astype(x.dtype)


def l2norm(x):
    return x * lax.rsqrt(jnp.sum(x * x, -1, keepdims=True) + EPS)


def modulate(h, shift, scale):
    return h * (1 + scale) + shift


def rope_tables(n_tok, head_dim):
    rows = n_tok // GRID_W
    t_row = jnp.broadcast_to(jnp.arange(rows)[:, None], (rows, GRID_W)).reshape(-1).astype(F32)
    t_col = jnp.broadcast_to(jnp.arange(GRID_W)[None, :], (rows, GRID_W)).reshape(-1).astype(F32)
    n_freq = head_dim // 4
    inv = ROPE_BASE ** (-jnp.arange(n_freq, dtype=F32) / n_freq)
    ang = jnp.concatenate([t_row[:, None] * inv, t_col[:, None] * inv], -1)
    return jnp.cos(ang), jnp.sin(ang)


def apply_rope(x, cos, sin):
    half = x.shape[-1] // 2
    shape = (cos.shape[0],) + (1,) * (x.ndim - 3) + (half,)
    cs, sn = cos.reshape(shape), sin.reshape(shape)
    x1, x2 = x[..., :half].astype(F32), x[..., half:].astype(F32)
    return jnp.concatenate([x1 * cs - x2 * sn, x2 * cs + x1 * sn], -1).astype(x.dtype)


def short_conv(x, w):
    pad = w.shape[0] // 2
    return lax.conv_general_dilated(x, w[:, None, :], window_strides=(1,), padding=[(pad, pad)],
                                    dimension_numbers=('NWC', 'WIO', 'NWC'), feature_group_count=x.shape[-1])


def gated_delta(q, k, v, g, beta, s0):
    bn, h, n, _ = q.shape
    dv = v.shape[-1]
    nc = n // A_CHUNK
    ch = lambda t: t.reshape(bn, h, nc, A_CHUNK, *t.shape[3:])
    q, k, v, g, beta = ch(q), ch(k), ch(v), ch(g), ch(beta)
    gcum = jnp.cumsum(g, -1)
    ar = jnp.arange(A_CHUNK)
    incl = ar[:, None] >= ar[None, :]
    strict = ar[:, None] > ar[None, :]
    decay = jnp.exp(jnp.where(incl, gcum[..., :, None] - gcum[..., None, :], -jnp.inf))
    a_mat = jnp.where(strict, beta[..., :, None] * jnp.einsum('bhcid,bhcjd->bhcij', k, k) * decay, 0.0)
    rhs = jnp.concatenate([v * beta[..., None], k * (beta * jnp.exp(gcum))[..., None]], -1)
    sol = lax.linalg.triangular_solve(a_mat, rhs, left_side=True, lower=True, unit_diagonal=True)
    u, w = sol[..., :dv], sol[..., dv:]
    qk = jnp.einsum('bhcid,bhcjd->bhcij', q, k) * decay

    def step(s, inp):
        q_c, k_c, u_c, w_c, g_c, qk_c = inp
        v_new = u_c - jnp.einsum('bhld,bhde->bhle', w_c, s)
        o_c = (jnp.einsum('bhld,bhde->bhle', q_c * jnp.exp(g_c)[..., None], s)
               + jnp.einsum('bhij,bhje->bhie', qk_c, v_new))
        g_last = g_c[..., -1:]
        s = s * jnp.exp(g_last)[..., None] + jnp.einsum('bhld,bhle->bhde', k_c * jnp.exp(g_last - g_c)[..., None], v_new)
        return s, o_c

    xs = tuple(jnp.moveaxis(t, 2, 0) for t in (q, k, u, w, gcum, qk))
    s_fin, o = lax.scan(step, s0, xs)
    return jnp.moveaxis(o, 0, 2).reshape(bn, h, n, dv), s_fin


def delta_inputs(h, w_in, conv_w, a_log, dt_bias):
    bn, n, _ = h.shape
    p = h @ w_in
    qkv = jax.nn.silu(short_conv(p[..., :A_QKV], conv_w)).astype(F32)
    z = p[..., A_QKV:A_QKV + A_HEADS * A_DV]
    ab = p[..., A_QKV + A_HEADS * A_DV:].astype(F32).reshape(bn, n, 2, 2, A_HEADS)
    q = l2norm(qkv[..., :A_HEADS * A_DK].reshape(bn, n, A_HEADS, A_DK)) * (A_DK ** -0.5)
    k = l2norm(qkv[..., A_HEADS * A_DK:2 * A_HEADS * A_DK].reshape(bn, n, A_HEADS, A_DK))
    v = qkv[..., 2 * A_HEADS * A_DK:].reshape(bn, n, A_HEADS, A_DV)
    g = -jnp.exp(a_log.astype(F32)) * jax.nn.softplus(ab[:, :, 0] + dt_bias.astype(F32))
    beta = jax.nn.sigmoid(ab[:, :, 1])
    bh = lambda t: jnp.moveaxis(t, 1, 2)
    return bh(q), bh(k), bh(v), z, jnp.moveaxis(g, 1, 3), jnp.moveaxis(beta, 1, 3)


def delta_out(o, z, g_out, w_out, dtype):
    o = jnp.moveaxis(o, 1, 2)
    bn, n = o.shape[:2]
    o = rmsnorm(o, g_out) * jax.nn.silu(z.reshape(bn, n, A_HEADS, A_DV).astype(F32))
    return o.reshape(bn, n, A_HEADS * A_DV).astype(dtype) @ w_out


def mixer_delta(hx, hc, w_in, conv_w, a_log, dt_bias, g_out, w_out, need_ctx):
    qx, kx, vx, zx, gx, bx = delta_inputs(hx, w_in, conv_w, a_log, dt_bias)
    qc, kc, vc, zc, gc, bc = delta_inputs(hc, w_in, conv_w, a_log, dt_bias)
    s0 = jnp.zeros((hx.shape[0], A_HEADS, A_DK, A_DV), F32)
    ident = lambda t: t
    rev = lambda t: jnp.flip(t, 2)
    oc_f, sc_f = gated_delta(qc, kc, vc, gc[:, 0], bc[:, 0], s0)
    ox_f, _ = gated_delta(qx, kx, vx, gx[:, 0], bx[:, 0], sc_f)
    oc_b, sc_b = gated_delta(rev(qc), rev(kc), rev(vc), rev(gc[:, 1]), rev(bc[:, 1]), s0)
    ox_b, _ = gated_delta(rev(qx), rev(kx), rev(vx), rev(gx[:, 1]), rev(bx[:, 1]), sc_b)
    yx = delta_out(ident(ox_f) + rev(ox_b), zx, g_out, w_out, hx.dtype)
    yc = delta_out(oc_f + rev(oc_b), zc, g_out, w_out, hc.dtype) if need_ctx else None
    return yc, yx


def swa_project(h, w_in, qn, kn):
    bn, n, _ = h.shape
    p = h @ w_in
    q = rmsnorm(p[..., :B_QHEADS * B_HD].reshape(bn, n, B_KVHEADS, B_GROUP, B_HD), qn)
    k = rmsnorm(p[..., B_QHEADS * B_HD:(B_QHEADS + B_KVHEADS) * B_HD].reshape(bn, n, B_KVHEADS, B_HD), kn)
    v = p[..., (B_QHEADS + B_KVHEADS) * B_HD:].reshape(bn, n, B_KVHEADS, B_HD)
    return q, k, v


def sink_attend(q, k, v, sink, mask):
    s = jnp.einsum('bqkgd,bskd->bkgqs', q, k).astype(F32) * (B_HD ** -0.5)
    if mask is not None:
        s = jnp.where(mask, s, -jnp.inf)
    sk = jnp.broadcast_to(sink.astype(F32)[None, :, :, None, None], s.shape[:-1] + (1,))
    p = jax.nn.softmax(jnp.concatenate([s, sk], -1), -1)[..., :-1]
    return jnp.einsum('bkgqs,bskd->bqkgd', p.astype(v.dtype), v)


def mixer_swa(hx, hc, w_in, qn, kn, sink, w_out, cos, sin, need_ctx):
    bn, n, _ = hx.shape
    ctx_len = hc.shape[1]
    qx, kx, vx = swa_project(hx, w_in, qn, kn)
    qx, kx = apply_rope(qx, cos, sin), apply_rope(kx, cos, sin)
    qc, kc, vc = swa_project(hc, w_in, qn, kn)
    sink = sink.reshape(B_KVHEADS, B_GROUP)
    pad = ((0, 0), (BLOCK, BLOCK), (0, 0), (0, 0))
    kpad, vpad = jnp.pad(kx, pad), jnp.pad(vx, pad)
    ar_q = jnp.arange(BLOCK)
    ar_k = jnp.arange(3 * BLOCK) - BLOCK

    def block(i):
        start = i * BLOCK
        qb = lax.dynamic_slice_in_dim(qx, start, BLOCK, axis=1)
        kb = jnp.concatenate([lax.dynamic_slice_in_dim(kpad, start, 3 * BLOCK, axis=1), kc], 1)
        vb = jnp.concatenate([lax.dynamic_slice_in_dim(vpad, start, 3 * BLOCK, axis=1), vc], 1)
        t = start + ar_q
        sp = start + ar_k
        win = (jnp.abs(t[:, None] - sp[None, :]) <= B_WINDOW) & (sp >= 0)[None, :] & (sp < n)[None, :]
        mask = jnp.concatenate([win, jnp.ones((BLOCK, ctx_len), bool)], 1)
        return sink_attend(qb, kb, vb, sink, mask)

    ox = lax.map(block, jnp.arange(n // BLOCK))
    yx = jnp.moveaxis(ox, 0, 1).reshape(bn, n, B_QHEADS * B_HD) @ w_out
    yc = sink_attend(qc, kc, vc, sink, None).reshape(bn, ctx_len, B_QHEADS * B_HD) @ w_out if need_ctx else None
    return yc, yx


def diff_project(h, w_in, qn, kn):
    bn, n, _ = h.shape
    p = h @ w_in
    q = rmsnorm(p[..., :C_QK].reshape(bn, n, C_HEADS, 2, C_HD), qn)
    k = rmsnorm(p[..., C_QK:2 * C_QK].reshape(bn, n, C_HEADS, 2, C_HD), kn)
    v = p[..., 2 * C_QK:].reshape(bn, n, C_HEADS, 2 * C_HD)
    return q, k, v


def diff_attend(q, k, v, lam):
    s = jnp.einsum('bqhmd,bshmd->bhmqs', q, k).astype(F32) * (C_HD ** -0.5)
    p = jax.nn.softmax(s, -1)
    w = p[:, :, 0] - lam * p[:, :, 1]
    return jnp.einsum('bhqs,bshe->bqhe', w.astype(v.dtype), v)


def mixer_diff(hx, hc, w_in, qn, kn, lam_vecs, g_sub, w_out, cos, sin, lam_init, need_ctx):
    bn, n, _ = hx.shape
    lv = lam_vecs.astype(F32)
    lam = jnp.exp(jnp.sum(lv[0] * lv[1])) - jnp.exp(jnp.sum(lv[2] * lv[3])) + lam_init
    qx, kx, vx = diff_project(hx, w_in, qn, kn)
    qx, kx = apply_rope(qx, cos, sin), apply_rope(kx, cos, sin)
    qc, kc, vc = diff_project(hc, w_in, qn, kn)
    kcat = jnp.concatenate([kx, kc], 1)
    vcat = jnp.concatenate([vx, vc], 1)

    def block(i):
        qb = lax.dynamic_slice_in_dim(qx, i * BLOCK, BLOCK, axis=1)
        return diff_attend(qb, kcat, vcat, lam)

    def finish(o):
        return (rmsnorm(o, g_sub) * (1.0 - lam_init)).reshape(bn, o.shape[1], C_HEADS * 2 * C_HD) @ w_out

    ox = lax.map(block, jnp.arange(n // BLOCK))
    yx = finish(jnp.moveaxis(ox, 0, 1).reshape(bn, n, C_HEADS, 2 * C_HD))
    yc = finish(diff_attend(qc, kc, vc, lam)) if need_ctx else None
    return yc, yx


def ec_moe(h, w_router, w_gate_up, w_down):
    bn, n, d = h.shape
    cap = EC_CAPACITY * n // N_EXPERTS
    aff = jax.nn.softmax((h @ w_router).astype(F32), -1)
    gate, idx = lax.top_k(jnp.swapaxes(aff, 1, 2), cap)
    xg = jax.vmap(lambda hb, ib: hb[ib])(h, idx)
    gu = jnp.einsum('becd,edf->becf', xg, w_gate_up)
    act = jax.nn.silu(gu[..., :D_EXPERT]) * gu[..., D_EXPERT:]
    y = jnp.einsum('becf,efd->becd', act, w_down) * gate[..., None].astype(h.dtype)
    return jax.vmap(lambda ib, yb: jnp.zeros((n, d), yb.dtype).at[ib.reshape(-1)].add(yb.reshape(-1, d)))(idx, y)


def setup_inputs(seed: int = 0) -> dict:
    key = jax.random.key(seed)
    ks = jax.random.split(key, 32)
    D = D_MODEL

    def nrm(i, shape, scale):
        return jax.random.normal(ks[i], shape, F32) * scale

    dt = jnp.exp(jax.random.uniform(ks[10], (N_A, 2, A_HEADS), F32, minval=math.log(1e-3), maxval=math.log(1e-1)))
    return {
        "x": nrm(0, (BATCH, SEQ, D), 1.0),
        "c": nrm(1, (BATCH, D), 1.0),
        "ctx": nrm(2, (BATCH, CTX_LEN, D), 1.0),
        "c_ctx": nrm(3, (D,), 1.0),
        "w_ada": nrm(4, (DEPTH, D, 6 * D), 0.5 * D ** -0.5),
        "b_ada": nrm(5, (DEPTH, 6 * D), 0.02),
        "g_mix": 1.0 + nrm(6, (DEPTH, D), 0.02),
        "g_ffn": 1.0 + nrm(7, (DEPTH, D), 0.02),
        "a_w_in": nrm(8, (N_A, D, A_IN), D ** -0.5),
        "a_conv": nrm(9, (N_A, A_CONV, A_QKV), A_CONV ** -0.5),
        "a_log": jnp.log(jax.random.uniform(ks[11], (N_A, 2, A_HEADS), F32, minval=1.0, maxval=16.0)),
        "a_dt_bias": dt + jnp.log(-jnp.expm1(-dt)),
        "a_g_out": 1.0 + nrm(12, (N_A, A_DV), 0.02),
        "a_w_out": nrm(13, (N_A, A_HEADS * A_DV, D), (A_HEADS * A_DV) ** -0.5),
        "b_w_in": nrm(14, (N_B, D, B_IN), D ** -0.5),
        "b_q_norm": 1.0 + nrm(15, (N_B, B_HD), 0.02),
        "b_k_norm": 1.0 + nrm(16, (N_B, B_HD), 0.02),
        "b_sink": nrm(17, (N_B, B_QHEADS), 1.0),
        "b_w_out": nrm(18, (N_B, B_QHEADS * B_HD, D), (B_QHEADS * B_HD) ** -0.5),
        "c_w_in": nrm(19, (N_C, D, C_IN), D ** -0.5),
        "c_q_norm": 1.0 + nrm(20, (N_C, C_HD), 0.02),
        "c_k_norm": 1.0 + nrm(21, (N_C, C_HD), 0.02),
        "c_lambda": nrm(22, (N_C, 4, C_HD), 0.1),
        "c_g_sub": 1.0 + nrm(23, (N_C, 2 * C_HD), 0.02),
        "c_w_out": nrm(24, (N_C, C_HEADS * 2 * C_HD, D), (C_HEADS * 2 * C_HD) ** -0.5),
        "w_router": nrm(25, (DEPTH, D, N_EXPERTS), D ** -0.5),
        "w_gate_up": nrm(26, (DEPTH, N_EXPERTS, D, 2 * D_EXPERT), D ** -0.5),
        "w_down": nrm(27, (DEPTH, N_EXPERTS, D_EXPERT, D), D_EXPERT ** -0.5),
    }


def reference(x, c, ctx, c_ctx, w_ada, b_ada, g_mix, g_ffn, a_w_in, a_conv, a_log, a_dt_bias, a_g_out, a_w_out,
              b_w_in, b_q_norm, b_k_norm, b_sink, b_w_out, c_w_in, c_q_norm, c_k_norm, c_lambda, c_g_sub, c_w_out,
              w_router, w_gate_up, w_down):
    n = x.shape[1]
    cos_b, sin_b = rope_tables(n, B_HD)
    cos_c, sin_c = rope_tables(n, C_HD)
    silu_c = jax.nn.silu(c)
    silu_cc = jax.nn.silu(c_ctx)
    for l in range(DEPTH):
        last = l == DEPTH - 1
        mx = jnp.split((silu_c @ w_ada[l] + b_ada[l])[:, None, :], 6, axis=-1)
        mc = jnp.split(silu_cc @ w_ada[l] + b_ada[l], 6, axis=-1)
        hx = modulate(rmsnorm(x, g_mix[l]), mx[0], mx[1])
        hc = modulate(rmsnorm(ctx, g_mix[l]), mc[0], mc[1])
        kind, j = l % N_MIXERS, l // N_MIXERS
        if kind == 0:
            yc, yx = mixer_delta(hx, hc, a_w_in[j], a_conv[j], a_log[j], a_dt_bias[j], a_g_out[j], a_w_out[j], not last)
        elif kind == 1:
            yc, yx = mixer_swa(hx, hc, b_w_in[j], b_q_norm[j], b_k_norm[j], b_sink[j], b_w_out[j], cos_b, sin_b, not last)
        else:
            lam_init = 0.8 - 0.6 * math.exp(-0.3 * l)
            yc, yx = mixer_diff(hx, hc, c_w_in[j], c_q_norm[j], c_k_norm[j], c_lambda[j], c_g_sub[j], c_w_out[j],
                                cos_c, sin_c, lam_init, not last)
        x = x + mx[2] * yx
        x = x + mx[5] * ec_moe(modulate(rmsnorm(x, g_ffn[l]), mx[3], mx[4]), w_router[l], w_gate_up[l], w_down[l])
        if not last:
            ctx = ctx + mc[2] * yc
            ctx = ctx + mc[5] * ec_moe(modulate(rmsnorm(ctx, g_ffn[l]), mc[3], mc[4]), w_router[l], w_gate_up[l], w_down[l])
    return x
```

```python
import numpy as np
from contextlib import ExitStack
import concourse.bass as bass
import concourse.mybir as mybir

F32 = mybir.dt.float32
BF16 = mybir.dt.bfloat16
I32 = mybir.dt.int32
AF = mybir.ActivationFunctionType
ALU = mybir.AluOpType
AX = mybir.AxisListType

SELF_SYNC = True


class V:
    __slots__ = ("buf", "ap")

    def __init__(s, buf, ap):
        s.buf = buf
        s.ap = ap

    def __getitem__(s, k):
        return V(s.buf, s.ap[k])

    def re(s, pat, **kw):
        return V(s.buf, s.ap.rearrange(pat, **kw))

    def bc(s, shape):
        return V(s.buf, s.ap.to_broadcast(shape))

    def bitcast(s, dt):
        return V(s.buf, s.ap.bitcast(dt))


class Buf:
    def __init__(s, kb, handle, name):
        s.kb = kb
        s.h = handle
        s.name = name
        s.w = None
        s.r = {}
        s.sem = None
        s.semcnt = 0

    def __getitem__(s, k):
        return V(s, s.h[k])

    def v(s):
        return V(s, s.h[:])


class Pool:
    def __init__(s, bufs):
        s.bufs = bufs
        s.i = 0

    def next(s):
        b = s.bufs[s.i % len(s.bufs)]
        s.i += 1
        return b


def _ap(x):
    return x.ap if isinstance(x, V) else x


class KB:
    def __init__(s, nc):
        s.nc = nc
        s.E = {"pe": nc.tensor, "act": nc.scalar, "dve": nc.vector, "pool": nc.gpsimd, "sp": nc.sync}
        s.sem = {}
        s.cnt = {}
        s.waited = {e: {} for e in s.E}
        s.semh = {}
        for e in s.E:
            h = nc.alloc_semaphore("sem_" + e)
            s.sem[e] = h
            s.semh[e] = h
            s.cnt[e] = 0
        s.bar = nc.alloc_semaphore("sem_bar")
        s.barcnt = 0
        s.nbuf = 0
        s.dmabufs = []
        s.stack = [ExitStack()]
        s.phase_bufs = [[]]
        s.free_sems = []
        s.bar_t = s.tile([128, 8], F32, name="bar_t")
        s.n_ins = 0

    def tile(s, shape, dtype, name=None, space="sbuf"):
        s.nbuf += 1
        name = (name or "t") + "_%d" % s.nbuf
        if space == "sbuf":
            h = s.stack[-1].enter_context(s.nc.sbuf_tensor(name, list(shape), dtype))
        else:
            h = s.stack[-1].enter_context(s.nc.psum_tensor(name, list(shape), dtype))
        b = Buf(s, h, name)
        s.phase_bufs[-1].append(b)
        return b

    def pool(s, n, shape, dtype, name=None, space="sbuf"):
        return Pool([s.tile(shape, dtype, name=name, space=space) for _ in range(n)])

    def push(s):
        s.stack.append(ExitStack())
        s.phase_bufs.append([])

    def pop(s):
        s.barrier()
        for b in s.phase_bufs.pop():
            if b.sem is not None:
                s.free_sems.append((b.sem, b.semcnt, ("d", b.name)))
                b.sem = None
        s.stack.pop().close()

    def _getsem(s, b):
        if b.sem is None:
            if s.free_sems:
                h, c, oldkey = s.free_sems.pop()
                b.sem = h
                b.semcnt = c
                for e in s.E:
                    s.waited[e][("d", b.name)] = c
            else:
                b.sem = s.nc.alloc_semaphore("sd_" + b.name)
                b.semcnt = 0
            s.semh[("d", b.name)] = b.sem
            s.dmabufs.append(b)
        return b.sem

    def _wait(s, eng, ev):
        key, val = ev
        if s.waited[eng].get(key, 0) >= val:
            return
        if key == eng and (eng == "pe" or eng == "sp" or not SELF_SYNC):
            return
        s.E[eng].wait_ge(s.semh[key], val)
        s.waited[eng][key] = val

    def _deps(s, eng, reads, writes, acc=False):
        for b in reads:
            if b.w is not None:
                s._wait(eng, b.w)
        for b in writes:
            if b.w is not None:
                if not (acc and b.w[0] == eng):
                    s._wait(eng, b.w)
            for k, v in b.r.items():
                s._wait(eng, (k, v))

    def emit(s, eng, fn, reads, writes, acc=False):
        reads = [x.buf if isinstance(x, V) else x for x in reads if isinstance(x, (V, Buf))]
        writes = [x.buf if isinstance(x, V) else x for x in writes if isinstance(x, (V, Buf))]
        s._deps(eng, reads, writes, acc)
        ins = fn(s.E[eng])
        s.cnt[eng] += 1
        ins.then_inc(s.sem[eng], 1)
        ev = (eng, s.cnt[eng])
        for b in reads:
            b.r[eng] = s.cnt[eng]
        for b in writes:
            b.w = ev
            b.r = {}
        s.n_ins += 1
        return ins

    def dma(s, q, out, in_, indirect=None, **kw):
        obuf = out.buf if isinstance(out, V) else None
        ibuf = in_.buf if isinstance(in_, V) else None
        extra = []
        if indirect is not None:
            extra = [indirect["idx"].buf]
        carrier = obuf or ibuf or s.bar_t
        sem = s._getsem(carrier)
        reads = [b for b in [ibuf] + extra if b is not None]
        writes = [b for b in [obuf] if b is not None]
        s._deps(q, reads, writes)
        E = s.E[q]
        if indirect is None:
            ins = E.dma_start(out=_ap(out), in_=_ap(in_), **kw)
        else:
            off = bass.IndirectOffsetOnAxis(ap=indirect["idx"].ap, axis=0)
            if not hasattr(s, "_bregs"):
                s._bregs = {}
            if indirect["bound"] not in s._bregs:
                s._bregs[indirect["bound"]] = E.to_reg(indirect["bound"])
            breg = s._bregs[indirect["bound"]]
            if indirect["side"] == "out":
                ins = E.indirect_dma_start(out=_ap(out), out_offset=off, in_=_ap(in_), in_offset=None,
                                           bounds_check=breg, oob_is_err=False)
            else:
                ins = E.indirect_dma_start(out=_ap(out), out_offset=None, in_=_ap(in_), in_offset=off,
                                           bounds_check=breg, oob_is_err=False)
        carrier.semcnt += 16
        ins.then_inc(sem, 16)
        key = ("d", carrier.name)
        ev = (key, carrier.semcnt)
        for b in reads:
            b.r[key] = carrier.semcnt
        for b in writes:
            b.w = ev
            b.r = {}
        s.n_ins += 1
        return ins

    def barrier(s):
        for e in s.E:
            if e != "pool" and s.cnt[e] > 0:
                s._wait("pool", (e, s.cnt[e]))
        if s.cnt["pool"] > 0:
            key, val = "pool", s.cnt["pool"]
            if s.waited["pool"].get(key, 0) < val:
                s.E["pool"].wait_ge(s.sem["pool"], val)
                s.waited["pool"][key] = val
        for b in s.dmabufs:
            if b.sem is not None and b.semcnt > 0:
                s._wait("pool", (("d", b.name), b.semcnt))
        s.barcnt += 1
        s.E["pool"].memset(s.bar_t.h[:, 0:1], 0.0).then_inc(s.bar, 1)
        for e in s.E:
            if e != "pool":
                s.E[e].wait_ge(s.bar, s.barcnt)
        s.E["pool"].wait_ge(s.bar, s.barcnt)
        for e in s.E:
            for e2 in s.E:
                s.waited[e][e2] = s.cnt[e2]
            for b in s.dmabufs:
                if b.sem is not None:
                    s.waited[e][("d", b.name)] = b.semcnt
        for lst in s.phase_bufs:
            for b in lst:
                b.w = None
                b.r = {}
        s.dmabufs = [b for b in s.dmabufs if b.sem is not None]

    def mm(s, out, lhsT, rhs, start=True, stop=True):
        return s.emit("pe", lambda E: E.matmul(_ap(out), _ap(lhsT), _ap(rhs), start=start, stop=stop),
                      [lhsT, rhs], [out], acc=not start)

    def tr(s, out, in_, ident):
        return s.emit("pe", lambda E: E.transpose(_ap(out), _ap(in_), _ap(ident)), [in_, ident], [out])

    def act(s, out, in_, func, bias=None, scale=None, accum_out=None, eng="act"):
        kw = {}
        rd = [in_]
        wr = [out]
        if bias is not None:
            kw["bias"] = _ap(bias)
            rd.append(bias)
        if scale is not None:
            kw["scale"] = _ap(scale)
            rd.append(scale)
        if accum_out is not None:
            kw["accum_out"] = _ap(accum_out)
            wr.append(accum_out)
        return s.emit(eng, lambda E: E.activation(out=_ap(out), in_=_ap(in_), func=func, **kw), rd, wr)

    def tt(s, out, a, b, op, eng="dve"):
        return s.emit(eng, lambda E: E.tensor_tensor(out=_ap(out), in0=_ap(a), in1=_ap(b), op=op), [a, b], [out])

    def ts(s, out, a, s1, s2, op0, op1=None, eng="dve"):
        kw = {}
        if op1 is not None:
            kw["op1"] = op1
        return s.emit(eng, lambda E: E.tensor_scalar(out=_ap(out), in0=_ap(a), scalar1=_ap(s1), scalar2=_ap(s2),
                                                     op0=op0, **kw), [a, s1, s2], [out])

    def stt(s, out, in0, scalar, in1, op0, op1, eng="dve"):
        return s.emit(eng, lambda E: E.scalar_tensor_tensor(out=_ap(out), in0=_ap(in0), scalar=_ap(scalar),
                                                            in1=_ap(in1), op0=op0, op1=op1),
                      [in0, scalar, in1], [out])

    def copy(s, out, in_, eng="dve"):
        if eng == "act":
            return s.emit(eng, lambda E: E.copy(out=_ap(out), in_=_ap(in_)), [in_], [out])
        return s.emit(eng, lambda E: E.tensor_copy(out=_ap(out), in_=_ap(in_)), [in_], [out])

    def red(s, out, in_, op, axis=AX.X, eng="dve"):
        return s.emit(eng, lambda E: E.tensor_reduce(out=_ap(out), in_=_ap(in_), axis=axis, op=op), [in_], [out])

    def memset(s, out, val, eng="dve"):
        return s.emit(eng, lambda E: E.memset(_ap(out), val), [], [out])

    def recip(s, out, in_):
        return s.emit("dve", lambda E: E.reciprocal(out=_ap(out), in_=_ap(in_)), [in_], [out])


import os as _os

D = 1024
NCTX = 256
EPS = 1e-6
BIG = 32768.0


class MK:
    def __init__(s, N):
        s.N = N
        s.T = N + NCTX
        s.NT = s.T // 128
        s.NLT = N // 128
        s.cap_lat = 2 * N // 16
        s.cap_ctx = 2 * NCTX // 16
        s.S = s.cap_lat + s.cap_ctx
        nc = bass.Bass("TRN2", target_bir_lowering=False)
        s.nc = nc
        s.inp = {}
        s.kb = KB(nc)

    def din(s, name, shape, dt=F32):
        t = s.nc.dram_tensor(name, list(shape), dt, kind="ExternalInput").ap()
        s.inp[name] = t
        return t

    def dscr(s, name, shape, dt):
        return s.nc.dram_tensor(name, list(shape), dt, kind="Internal").ap()

    def setup(s):
        kb = s.kb
        N, T = s.N, s.T
        s.x_in = s.din("x", [N, D])
        s.ctx_in = s.din("ctx", [NCTX, D])
        s.c_in = s.din("c", [D])
        s.cc_in = s.din("c_ctx", [D])
        s.w_ada = s.din("w_ada", [4, D, 6 * D])
        s.b_ada = s.din("b_ada", [4, 6 * D])
        s.g_mix = s.din("g_mix", [4, D])
        s.g_ffn = s.din("g_ffn", [4, D])
        s.a_w_in = s.din("a_w_in", [2, D, 4128])
        s.a_conv = s.din("a_conv", [2, 5, 3072])
        s.a_log = s.din("a_log", [2, 2, 8])
        s.a_dt_bias = s.din("a_dt_bias", [2, 2, 8])
        s.a_g_out = s.din("a_g_out", [2, 128])
        s.a_w_out = s.din("a_w_out", [2, D, D])
        s.b_w_in = s.din("b_w_in", [1, D, 1536])
        s.b_q_norm = s.din("b_q_norm", [1, 64])
        s.b_k_norm = s.din("b_k_norm", [1, 64])
        s.b_sink = s.din("b_sink", [1, 16])
        s.b_w_out = s.din("b_w_out", [1, D, D])
        s.c_w_in = s.din("c_w_in", [1, D, 3072])
        s.c_q_norm = s.din("c_q_norm", [1, 64])
        s.c_k_norm = s.din("c_k_norm", [1, 64])
        s.c_lambda = s.din("c_lambda", [1, 4, 64])
        s.c_g_sub = s.din("c_g_sub", [1, 128])
        s.c_w_out = s.din("c_w_out", [1, D, D])
        s.w_router = s.din("w_router", [4, D, 16])
        s.w_gate_up = s.din("w_gate_up", [4, 16, D, 2 * D])
        s.w_down = s.din("w_down", [4, 16, D, D])
        s.k_ident = s.din("k_ident", [128, 128])
        s.k_cos = s.din("k_cos", [T, 32])
        s.k_sin = s.din("k_sin", [T, 32])
        s.k_tril = s.din("k_tril", [128, 128])
        s.k_triu = s.din("k_triu", [128, 128])
        s.k_bd16 = s.din("k_bd16", [128, 128])
        s.k_off = s.din("k_off", [3, 128, 128])
        s.k_escal = s.din("k_escal", [16, 2])
        s.out = s.nc.dram_tensor("out", [T, D], F32, kind="ExternalOutput").ap()
        s.X = s.dscr("X", [T, D], F32)
        s.H = s.dscr("H", [T, D], BF16)
        s.XG = s.dscr("XG", [16 * s.S, D], BF16)
        s.Y = s.dscr("Y", [16 * s.S, D], BF16)
        s.QKT = s.dscr("QKT", [24, 128, T], BF16)
        s.VA = s.dscr("VA", [T, 4 * 65], BF16)
        s.PA = s.dscr("PA", [T, 3072], BF16)
        s.KVS = s.dscr("KVS", [T, 2048], F32)
        s.OF = s.dscr("OF", [T, D], F32)
        s.ident_f = kb.tile([128, 128], F32, "ident_f")
        s.ident_b = kb.tile([128, 128], BF16, "ident_b")
        kb.dma("sp", s.ident_f.v(), s.k_ident)
        kb.copy(s.ident_b.v(), s.ident_f.v())
        s.epst = kb.tile([128, 1], F32, "eps")
        kb.memset(s.epst.v(), EPS)
        s.scb = []
        for i, cin in enumerate((s.c_in, s.cc_in)):
            ct = kb.tile([128, 8], F32, "c%d" % i)
            kb.dma("sp", ct.v(), cin.rearrange("(k p) -> p k", p=128), allow_slow_non_contiguous=True)
            kb.act(ct.v(), ct.v(), AF.Silu)
            sb = kb.tile([128, 8, 128], F32, "scb%d" % i)
            kb.copy(sb.v(), ct.v().re("p (k o) -> p k o", o=1).bc([128, 8, 128]))
            s.scb.append(sb)
        for r0 in range(0, N, 1024):
            kb.dma("sp", s.X[r0:r0 + 1024, :], s.x_in[r0:r0 + 1024, :])
        kb.dma("sp", s.X[N:T, :], s.ctx_in)
        kb.barrier()

    def adaln(s, l, js, g_ap):
        kb = s.kb
        res = {}
        for w in (0, 1):
            for j in js:
                res[(w, j)] = kb.tile([128, D], F32, "mod%d_%d" % (w, j))
        kb.push()
        wpool = kb.pool(2, [128, 8, 512], F32, "wada")
        bpool = kb.pool(2, [128, 512], F32, "bada")
        pspool = kb.pool(2, [128, 512], F32, "ps_ada", space="psum")
        for j in js:
            for half in (0, 1):
                n0 = j * D + half * 512
                wt = wpool.next()
                kb.dma("sp", wt.v(), s.w_ada[l, :, n0:n0 + 512].rearrange("(k p) n -> p k n", p=128))
                bt = bpool.next()
                kb.dma("sp", bt.v(), s.b_ada[l:l + 1, n0:n0 + 512].to_broadcast([128, 512]))
                for w in (0, 1):
                    ps = pspool.next()
                    for k in range(8):
                        kb.mm(ps.v(), s.scb[w][:, k, :], wt[:, k, :], start=(k == 0), stop=(k == 7))
                    kb.tt(res[(w, j)][:, half * 512:(half + 1) * 512], ps.v(), bt.v(), ALU.add)
        gt = kb.tile([128, D], F32, "gt")
        kb.dma("sp", gt.v(), g_ap.to_broadcast([128, D]))
        for w in (0, 1):
            t = res[(w, js[1])]
            kb.stt(t.v(), t.v(), 1.0, gt.v(), ALU.add, ALU.mult)
        kb.pop()
        return res

    def norm_mod(s, xt, gs, sh, out, tmp_f, junk, ss):
        kb = s.kb
        kb.memset(ss.v(), 0.0)
        kb.act(junk.v(), xt.v(), AF.Square, accum_out=ss[:, 0:1])
        kb.act(ss[:, 1:2], ss[:, 0:1], AF.Sqrt, bias=s.epst[:, 0:1], scale=1.0 / D)
        kb.recip(ss[:, 1:2], ss[:, 1:2])
        kb.stt(tmp_f.v(), xt.v(), ss[:, 1:2], gs.v(), ALU.mult, ALU.mult)
        kb.tt(out.v(), tmp_f.v(), sh.v(), ALU.add)

    def transpose_chunks(s, dst, src, n, pspool, ident, evac="act"):
        kb = s.kb
        c = 0
        while c < n:
            m = min(8, n - c)
            ps = pspool.next()
            psb = ps.v().bitcast(BF16)
            for i in range(m):
                kb.tr(psb[:, i * 128:(i + 1) * 128], src[:, (c + i) * 128:(c + i + 1) * 128], ident.v())
            kb.copy(dst[:, c:c + m, :], psb[:, 0:m * 128].re("p (k t) -> p k t", t=128), eng=evac)
            c += m

    def load_w_bf16(s, dst, src_ap, ncols, stage_pool, chunk=512, engs=("dve", "act")):
        kb = s.kb
        i = 0
        for n0 in range(0, ncols, chunk):
            nsz = min(chunk, ncols - n0)
            st = stage_pool.next()
            kb.dma("sp", st[:, :, 0:nsz], src_ap[:, n0:n0 + nsz].rearrange("(k p) n -> p k n", p=128))
            kb.copy(dst[:, :, n0:n0 + nsz], st[:, :, 0:nsz], eng=engs[i % len(engs)])
            i += 1

    def qk_post(s, pqs, nh, G, cs, sn, qn, sq, ssh, t1, t2):
        kb = s.kb
        x3 = pqs.re("p (h d) -> p h d", d=64)
        kb.tt(sq.v().re("p (h d) -> p h d", d=64)[:, 0:nh, :], x3, x3, ALU.mult)
        kb.red(ssh[:, 0:nh], sq.v().re("p (h d) -> p h d", d=64)[:, 0:nh, :], ALU.add)
        kb.act(ssh[:, 0:nh], ssh[:, 0:nh], AF.Sqrt, bias=s.epst[:, 0:1], scale=1.0 / 64)
        kb.recip(ssh[:, 0:nh], ssh[:, 0:nh])
        q3 = qn.v().re("p (h d) -> p h d", d=64)[:, 0:nh, :]
        kb.tt(q3, x3, ssh[:, 0:nh].re("p (h o) -> p h o", o=1).bc([128, nh, 64]), ALU.mult)
        kb.tt(q3, q3, G.v().re("p (h d) -> p h d", d=64)[:, 0:nh, :], ALU.mult)
        x1 = q3[:, :, 0:32]
        x2 = q3[:, :, 32:64]
        cb = cs.v().re("p (o d) -> p o d", o=1).bc([128, nh, 32])
        sb = sn.v().re("p (o d) -> p o d", o=1).bc([128, nh, 32])
        t13 = t1.v().re("p (h d) -> p h d", d=64)[:, 0:nh, :]
        t23 = t2.v().re("p (h d) -> p h d", d=64)[:, 0:nh, :]
        kb.tt(t13[:, :, 0:32], x1, cb, ALU.mult)
        kb.tt(t13[:, :, 32:64], x2, cb, ALU.mult)
        kb.tt(t23[:, :, 0:32], x2, sb, ALU.mult)
        kb.tt(t23[:, :, 32:64], x1, sb, ALU.mult)
        return t13, t23

    def layer_swa(s, l, j):
        kb = s.kb
        N, T, NT, NLT = s.N, s.T, s.NT, s.NLT
        kb.push()
        mod = s.adaln(l, [0, 1, 2], s.g_mix[l:l + 1, :])
        kb.push()
        stage = kb.pool(2, [128, 8, 512], F32, "stage")
        w_in = kb.tile([128, 8, 1536], BF16, "w_in")
        s.load_w_bf16(w_in, s.b_w_in[j], 1536, stage)
        G = kb.tile([128, 20 * 64], F32, "G")
        kb.dma("sp", G[:, 0:64], s.b_q_norm[j:j + 1, :].to_broadcast([128, 64]))
        kb.dma("sp", G[:, 1024:1088], s.b_k_norm[j:j + 1, :].to_broadcast([128, 64]))
        kb.ts(G[:, 0:64], G[:, 0:64], 0.125, None, ALU.mult)
        for h in range(1, 16):
            kb.copy(G[:, h * 64:(h + 1) * 64], G[:, 0:64])
        for h in range(1, 4):
            kb.copy(G[:, 1024 + h * 64:1024 + (h + 1) * 64], G[:, 1024:1088])
        xp = kb.pool(2, [128, D], F32, "x")
        junk = kb.tile([128, D], F32, "junk")
        tmpf = kb.tile([128, D], F32, "tmpf")
        ssp = kb.pool(2, [128, 2], F32, "ss")
        hbp = kb.pool(2, [128, D], BF16, "hb")
        hTp = kb.pool(2, [128, 8, 128], BF16, "hT")
        psT = kb.pool(2, [128, 512], F32, "psT", space="psum")
        psP = kb.pool(3, [128, 512], F32, "psP", space="psum")
        pqs = kb.tile([128, 1536], F32, "pqs")
        sq = kb.tile([128, 1280], F32, "sq")
        ssh = kb.tile([128, 20], F32, "ssh")
        qn = kb.tile([128, 1280], F32, "qn")
        t1 = kb.tile([128, 1280], F32, "t1")
        t2 = kb.tile([128, 1280], F32, "t2")
        csp = kb.pool(2, [128, 32], F32, "cs")
        snp = kb.pool(2, [128, 32], F32, "sn")
        qkbp = kb.pool(2, [128, 1536], BF16, "qkb")
        qkTp = kb.pool(2, [128, 12, 128], BF16, "qkT")
        vap = kb.pool(2, [128, 4, 65], BF16, "va")
        for b in vap.bufs:
            kb.memset(b.v(), 1.0)
        for ti in range(NT):
            w = 0 if ti < NLT else 1
            t0 = ti * 128
            xt = xp.next()
            kb.dma("sp", xt.v(), s.X[t0:t0 + 128, :])
            hb = hbp.next()
            s.norm_mod(xt, mod[(w, 1)], mod[(w, 0)], hb, tmpf, junk, ssp.next())
            hT = hTp.next()
            s.transpose_chunks(hT.v(), hb.v(), 8, psT, s.ident_b)
            for nb in range(3):
                ps = psP.next()
                for k in range(8):
                    kb.mm(ps.v(), hT[:, k, :], w_in[:, k, nb * 512:(nb + 1) * 512], start=(k == 0), stop=(k == 7))
                kb.copy(pqs[:, nb * 512:(nb + 1) * 512], ps.v(), eng="act")
            cs = csp.next()
            sn = snp.next()
            kb.dma("sp", cs.v(), s.k_cos[t0:t0 + 128, :])
            kb.dma("sp", sn.v(), s.k_sin[t0:t0 + 128, :])
            t13, t23 = s.qk_post(pqs[:, 0:1280], 20, G, cs, sn, qn, sq, ssh, t1, t2)
            qkb = qkbp.next()
            q3 = qkb[:, 0:1024].re("p (h d) -> p h d", d=64)
            k3 = qkb[:, 1024:1536].re("p (h d) -> p h d", d=128)
            kb.tt(q3[:, :, 0:32], t13[:, 0:16, 0:32], t23[:, 0:16, 0:32], ALU.subtract)
            kb.tt(q3[:, :, 32:64], t13[:, 0:16, 32:64], t23[:, 0:16, 32:64], ALU.add)
            kb.tt(k3[:, :, 0:32], t13[:, 16:20, 0:32], t23[:, 16:20, 0:32], ALU.subtract)
            kb.tt(k3[:, :, 32:64], t13[:, 16:20, 32:64], t23[:, 16:20, 32:64], ALU.add)
            kb.copy(k3[:, :, 64:128], k3[:, :, 0:64])
            qkT = qkTp.next()
            s.transpose_chunks(qkT.v(), qkb.v(), 12, psT, s.ident_b)
            kb.dma("pool", s.QKT[0:12, :, t0:t0 + 128].rearrange("c p t -> p c t"), qkT.v())
            va = vap.next()
            kb.copy(va[:, :, 0:64], pqs[:, 1280:1536].re("p (h d) -> p h d", d=64), eng="act")
            kb.dma("pool", s.VA[t0:t0 + 128, :], va.v().re("p h d -> p (h d)"))
        kb.pop()
        kb.push()
        stage = kb.pool(2, [128, 8, 512], F32, "stage")
        w_out = kb.tile([128, 8, D], BF16, "w_out")
        s.load_w_bf16(w_out, s.b_w_out[j], D, stage)
        esink = kb.tile([128, 16], F32, "esink")
        kb.dma("sp", esink.v(), s.b_sink[j:j + 1, :].to_broadcast([128, 16]))
        kb.act(esink.v(), esink.v(), AF.Exp)
        mf = kb.tile([128, 128], F32, "mf")
        m_prev = kb.tile([128, 128], BF16, "m_prev")
        m_next = kb.tile([128, 128], BF16, "m_next")
        kb.dma("sp", mf.v(), s.k_tril)
        kb.copy(m_prev.v(), mf.v())
        kb.dma("sp", mf.v(), s.k_triu)
        kb.copy(m_next.v(), mf.v())
        kTc = kb.tile([128, 4, 256], BF16, "kTc")
        kb.dma("sp", kTc.v(), s.QKT[8:12, :, N:T].rearrange("c p t -> p c t"))
        Vc = kb.tile([128, 2, 260], BF16, "Vc")
        kb.dma("sp", Vc.v(), s.VA[N:T, :].rearrange("(j p) e -> p j e", p=128))
        qTp = kb.pool(2, [128, 8, 128], BF16, "qT")
        kTwp = kb.pool(2, [128, 4, 384], BF16, "kTw")
        Vwp = kb.pool(2, [128, 3, 260], BF16, "Vw")
        psA = kb.pool(2, [128, 512], F32, "psA", space="psum")
        psB = kb.pool(2, [128, 512], F32, "psB", space="psum")
        psO = kb.pool(2, [128, 512], F32, "psO", space="psum")
        psY = kb.pool(2, [128, 512], F32, "psY", space="psum")
        PTp = kb.pool(3, [128, 5, 128], BF16, "PT")
        osbp = kb.pool(2, [128, D], BF16, "osb")
        oTp = kb.pool(2, [128, 8, 128], BF16, "oT")
        denp = kb.pool(4, [128, 2], F32, "den")
        xp = kb.pool(2, [128, D], F32, "x")
        yt = kb.tile([128, D], F32, "yt")
        for ti in range(NT):
            lat = ti < NLT
            t0 = ti * 128
            qT = qTp.next()
            kb.dma("sp", qT.v(), s.QKT[0:8, :, t0:t0 + 128].rearrange("c p t -> p c t"))
            if lat:
                j0 = max(0, ti - 1)
                j1 = min(NLT, ti + 2)
                nw = j1 - j0
                kTw = kTwp.next()
                kb.dma("sp", kTw[:, :, 0:nw * 128], s.QKT[8:12, :, j0 * 128:j1 * 128].rearrange("c p t -> p c t"))
                Vw = Vwp.next()
                kb.dma("sp", Vw[:, 0:nw, :], s.VA[j0 * 128:j1 * 128, :].rearrange("(j p) e -> p j e", p=128))
            else:
                nw = 0
                j0 = 0
            osb = osbp.next()
            for hq in range(16):
                kv = hq // 4
                po = (hq % 2) * 64
                qh = qT[po:po + 64, hq // 2, :]
                pa = psA.next()
                pb = psB.next()
                PT = PTp.next()
                for jj in range(nw):
                    kb.mm(pa[:, jj * 128:(jj + 1) * 128], kTw[po:po + 64, kv, jj * 128:(jj + 1) * 128], qh)
                for jj in range(2):
                    kb.mm(pb[:, jj * 128:(jj + 1) * 128], kTc[po:po + 64, kv, jj * 128:(jj + 1) * 128], qh)
                if nw:
                    kb.act(PT[:, 0:nw, :], pa[:, 0:nw * 128].re("p (j t) -> p j t", t=128), AF.Exp)
                    if ti - 1 >= 0:
                        kb.tt(PT[:, 0, :], PT[:, 0, :], m_prev.v(), ALU.mult)
                    if ti + 1 < NLT:
                        kb.tt(PT[:, nw - 1, :], PT[:, nw - 1, :], m_next.v(), ALU.mult)
                kb.act(PT[:, 3:5, :], pb[:, 0:256].re("p (j t) -> p j t", t=128), AF.Exp)
                po_ = psO.next()
                nmm = nw + 2
                i = 0
                for jj in range(nw):
                    kb.mm(po_[:, 0:65], PT[:, jj, :], Vw[:, jj, kv * 65:(kv + 1) * 65], start=(i == 0), stop=(i == nmm - 1))
                    i += 1
                for jj in range(2):
                    kb.mm(po_[:, 0:65], PT[:, 3 + jj, :], Vc[:, jj, kv * 65:(kv + 1) * 65], start=(i == 0), stop=(i == nmm - 1))
                    i += 1
                den = denp.next()
                kb.tt(den[:, 0:1], po_[:, 64:65], esink[:, hq:hq + 1], ALU.add)
                kb.recip(den[:, 1:2], den[:, 0:1])
                kb.ts(osb[:, hq * 64:(hq + 1) * 64], po_[:, 0:64], den[:, 1:2], None, ALU.mult)
            oT = oTp.next()
            s.transpose_chunks(oT.v(), osb.v(), 8, psY, s.ident_b)
            xt = xp.next()
            kb.dma("sp", xt.v(), s.X[t0:t0 + 128, :])
            w = 0 if lat else 1
            for nb in range(2):
                ps = psY.next()
                for k in range(8):
                    kb.mm(ps.v(), oT[:, k, :], w_out[:, k, nb * 512:(nb + 1) * 512], start=(k == 0), stop=(k == 7))
                kb.tt(yt[:, nb * 512:(nb + 1) * 512], ps.v(), mod[(w, 2)][:, nb * 512:(nb + 1) * 512], ALU.mult)
            kb.tt(xt.v(), xt.v(), yt.v(), ALU.add)
            kb.dma("pool", s.X[t0:t0 + 128, :], xt.v())
        kb.pop()
        kb.pop()

    def out_proj_res(s, ti, oT, w_out, gate, psY, xp, yt):
        kb = s.kb
        t0 = ti * 128
        xt = xp.next()
        kb.dma("sp", xt.v(), s.X[t0:t0 + 128, :])
        for nb in range(2):
            ps = psY.next()
            for k in range(8):
                kb.mm(ps.v(), oT[:, k, :], w_out[:, k, nb * 512:(nb + 1) * 512], start=(k == 0), stop=(k == 7))
            kb.tt(yt[:, nb * 512:(nb + 1) * 512], ps.v(), gate[:, nb * 512:(nb + 1) * 512], ALU.mult)
        kb.tt(xt.v(), xt.v(), yt.v(), ALU.add)
        kb.dma("pool", s.X[t0:t0 + 128, :], xt.v())

    def layer_diff(s, l, j):
        import math
        kb = s.kb
        N, T, NT, NLT = s.N, s.T, s.NT, s.NLT
        lam_init = 0.8 - 0.6 * math.exp(-0.3 * l)
        kb.push()
        mod = s.adaln(l, [0, 1, 2], s.g_mix[l:l + 1, :])
        kb.push()
        stage = kb.pool(2, [128, 8, 512], F32, "stage")
        w_in = kb.tile([128, 8, 3072], BF16, "w_in")
        s.load_w_bf16(w_in, s.c_w_in[j], 3072, stage)
        G = kb.tile([128, 2048], F32, "G")
        kb.dma("sp", G[:, 0:64], s.c_q_norm[j:j + 1, :].to_broadcast([128, 64]))
        kb.dma("sp", G[:, 1024:1088], s.c_k_norm[j:j + 1, :].to_broadcast([128, 64]))
        kb.ts(G[:, 0:64], G[:, 0:64], 0.125, None, ALU.mult)
        for h in range(1, 16):
            kb.copy(G[:, h * 64:(h + 1) * 64], G[:, 0:64])
            kb.copy(G[:, 1024 + h * 64:1024 + (h + 1) * 64], G[:, 1024:1088])
        xp = kb.pool(2, [128, D], F32, "x")
        junk = kb.tile([128, D], F32, "junk")
        tmpf = kb.tile([128, D], F32, "tmpf")
        ssp = kb.pool(2, [128, 2], F32, "ss")
        hbp = kb.pool(2, [128, D], BF16, "hb")
        hTp = kb.pool(2, [128, 8, 128], BF16, "hT")
        psT = kb.pool(2, [128, 512], F32, "psT", space="psum")
        psP = kb.pool(4, [128, 512], F32, "psP", space="psum")
        pqs = kb.tile([128, 2048], F32, "pqs")
        sq = kb.tile([128, 2048], F32, "sq")
        ssh = kb.tile([128, 32], F32, "ssh")
        qn = kb.tile([128, 2048], F32, "qn")
        t1 = kb.tile([128, 2048], F32, "t1")
        t2 = kb.tile([128, 2048], F32, "t2")
        csp = kb.pool(2, [128, 32], F32, "cs")
        snp = kb.pool(2, [128, 32], F32, "sn")
        qkbp = kb.pool(2, [128, 2048], BF16, "qkb")
        qkTp = kb.pool(2, [128, 16, 128], BF16, "qkT")
        vbp = kb.pool(2, [128, D], BF16, "vb")
        for ti in range(NT):
            w = 0 if ti < NLT else 1
            t0 = ti * 128
            xt = xp.next()
            kb.dma("sp", xt.v(), s.X[t0:t0 + 128, :])
            hb = hbp.next()
            s.norm_mod(xt, mod[(w, 1)], mod[(w, 0)], hb, tmpf, junk, ssp.next())
            hT = hTp.next()
            s.transpose_chunks(hT.v(), hb.v(), 8, psT, s.ident_b)
            vb = vbp.next()
            for nb in range(6):
                ps = psP.next()
                for k in range(8):
                    kb.mm(ps.v(), hT[:, k, :], w_in[:, k, nb * 512:(nb + 1) * 512], start=(k == 0), stop=(k == 7))
                if nb < 4:
                    kb.copy(pqs[:, nb * 512:(nb + 1) * 512], ps.v(), eng="act")
                else:
                    kb.copy(vb[:, (nb - 4) * 512:(nb - 3) * 512], ps.v(), eng="act")
            kb.dma("pool", s.H[t0:t0 + 128, :], vb.v())
            cs = csp.next()
            sn = snp.next()
            kb.dma("sp", cs.v(), s.k_cos[t0:t0 + 128, :])
            kb.dma("sp", sn.v(), s.k_sin[t0:t0 + 128, :])
            t13, t23 = s.qk_post(pqs[:, 0:2048], 32, G, cs, sn, qn, sq, ssh, t1, t2)
            qkb = qkbp.next()
            q3 = qkb.v().re("p (h d) -> p h d", d=64)
            kb.tt(q3[:, :, 0:32], t13[:, :, 0:32], t23[:, :, 0:32], ALU.subtract)
            kb.tt(q3[:, :, 32:64], t13[:, :, 32:64], t23[:, :, 32:64], ALU.add)
            qkT = qkTp.next()
            s.transpose_chunks(qkT.v(), qkb.v(), 16, psT, s.ident_b)
            kb.dma("pool", s.QKT[0:16, :, t0:t0 + 128].rearrange("c p t -> p c t"), qkT.v())
        kb.pop()
        kb.push()
        lv = kb.tile([128, 256], F32, "lv")
        kb.dma("sp", lv.v(), s.c_lambda[j:j + 1].rearrange("o a d -> o (a d)").to_broadcast([128, 256]))
        lt = kb.tile([128, 128], F32, "lt")
        lam = kb.tile([128, 4], F32, "lam")
        kb.tt(lt[:, 0:64], lv[:, 0:64], lv[:, 64:128], ALU.mult)
        kb.tt(lt[:, 64:128], lv[:, 128:192], lv[:, 192:256], ALU.mult)
        kb.red(lam[:, 0:2], lt.v().re("p (a d) -> p a d", d=64), ALU.add)
        kb.act(lam[:, 0:2], lam[:, 0:2], AF.Exp)
        kb.tt(lam[:, 2:3], lam[:, 1:2], lam[:, 0:1], ALU.subtract)
        kb.ts(lam[:, 3:4], lam[:, 2:3], -lam_init, None, ALU.add)
        gsub = kb.tile([128, 1], F32, "gsub")
        kb.dma("sp", gsub.v(), s.c_g_sub[j].rearrange("(p o) -> p o", o=1))
        kb.ts(gsub.v(), gsub.v(), 1.0 - lam_init, None, ALU.mult)
        ones_b = kb.tile([128, 128], BF16, "ones_b")
        kb.memset(ones_b.v(), 1.0)
        ones_f = kb.tile([128, 128], F32, "ones_f")
        kb.memset(ones_f.v(), 1.0)
        kTp = kb.pool(2, [128, T], BF16, "kT")
        Vp = kb.pool(2, [128, NT, 128], BF16, "V")
        qTp = kb.pool(2, [128, 512], BF16, "qT")
        psS = kb.pool(2, [128, 512], F32, "psS", space="psum")
        psO = [kb.tile([128, 512], F32, "psO%d" % m, space="psum") for m in range(2)]
        psD = [kb.tile([128, 512], F32, "psD%d" % m, space="psum") for m in range(2)]
        psN = kb.pool(2, [128, 512], F32, "psN", space="psum")
        PTp = kb.pool(4, [128, 512], BF16, "PT")
        rp = kb.pool(2, [128, 512], F32, "r")
        o1p = kb.pool(2, [128, 512], F32, "o1")
        o2p = kb.pool(2, [128, 512], F32, "o2")
        sqp = kb.pool(2, [128, 512], F32, "sqo")
        oTp = kb.pool(2, [128, 512], BF16, "oT")
        groups = [(g0, 512, list(range(NT))) for g0 in range(0, N, 512)] + [(N, NCTX, [NLT, NLT + 1])]
        for h in range(8):
            kT = kTp.next()
            kb.dma("sp", kT.v(), s.QKT[8 + h, :, :])
            Vh = Vp.next()
            kb.dma("sp", Vh.v(), s.H[:, h * 128:(h + 1) * 128].rearrange("(j p) e -> p j e", p=128))
            for (g0, nq, kts) in groups:
                qT = qTp.next()
                kb.dma("sp", qT[:, 0:nq], s.QKT[h, :, g0:g0 + nq])
                for ki, kt in enumerate(kts):
                    for m in range(2):
                        ps = psS.next()
                        kb.mm(ps[:, 0:nq], kT[64 * m:64 * m + 64, kt * 128:(kt + 1) * 128], qT[64 * m:64 * m + 64, 0:nq])
                        PT = PTp.next()
                        kb.act(PT[:, 0:nq], ps[:, 0:nq], AF.Exp)
                        kb.mm(psO[m][:, 0:nq], Vh[:, kt, :], PT[:, 0:nq], start=(ki == 0), stop=(ki == len(kts) - 1))
                        kb.mm(psD[m][:, 0:nq], ones_b.v(), PT[:, 0:nq], start=(ki == 0), stop=(ki == len(kts) - 1))
                o1 = o1p.next()
                o2 = o2p.next()
                for m, o in ((0, o1), (1, o2)):
                    r = rp.next()
                    kb.recip(r[:, 0:nq], psD[m][:, 0:nq])
                    kb.tt(o[:, 0:nq], psO[m][:, 0:nq], r[:, 0:nq], ALU.mult)
                kb.stt(o1[:, 0:nq], o2[:, 0:nq], lam[:, 3:4], o1[:, 0:nq], ALU.mult, ALU.add)
                sqo = sqp.next()
                kb.tt(sqo[:, 0:nq], o1[:, 0:nq], o1[:, 0:nq], ALU.mult)
                pn = psN.next()
                kb.mm(pn[:, 0:nq], ones_f.v(), sqo[:, 0:nq])
                r = rp.next()
                kb.act(r[:, 0:nq], pn[:, 0:nq], AF.Sqrt, bias=s.epst[:, 0:1], scale=1.0 / 128)
                kb.recip(r[:, 0:nq], r[:, 0:nq])
                oT = oTp.next()
                kb.stt(oT[:, 0:nq], o1[:, 0:nq], gsub[:, 0:1], r[:, 0:nq], ALU.mult, ALU.mult)
                kb.dma("pool", s.QKT[16 + h, :, g0:g0 + nq], oT[:, 0:nq])
        kb.pop()
        kb.push()
        stage = kb.pool(2, [128, 8, 512], F32, "stage")
        w_out = kb.tile([128, 8, D], BF16, "w_out")
        s.load_w_bf16(w_out, s.c_w_out[j], D, stage)
        oTp = kb.pool(2, [128, 8, 128], BF16, "oT")
        psY = kb.pool(2, [128, 512], F32, "psY", space="psum")
        xp = kb.pool(2, [128, D], F32, "x")
        yt = kb.tile([128, D], F32, "yt")
        for ti in range(NT):
            t0 = ti * 128
            w = 0 if ti < NLT else 1
            oT = oTp.next()
            kb.dma("sp", oT.v(), s.QKT[16:24, :, t0:t0 + 128].rearrange("c p t -> p c t"))
            s.out_proj_res(ti, oT, w_out, mod[(w, 2)], psY, xp, yt)
        kb.pop()
        kb.pop()

    def layer_delta(s, l, j):
        kb = s.kb
        N, T, NT, NLT = s.N, s.T, s.NT, s.NLT
        kb.push()
        mod = s.adaln(l, [0, 1, 2], s.g_mix[l:l + 1, :])
        ab_all = kb.tile([128, NT, 32], F32, "ab_all")
        gb_all = kb.tile([128, NT, 32], F32, "gb_all")
        kb.push()
        stage = kb.pool(2, [128, 8, 512], F32, "stage")
        w_in = kb.tile([128, 8, 4128], BF16, "w_in")
        s.load_w_bf16(w_in, s.a_w_in[j], 4128, stage)
        xp = kb.pool(2, [128, D], F32, "x")
        junk = kb.tile([128, D], F32, "junk")
        tmpf = kb.tile([128, D], F32, "tmpf")
        ssp = kb.pool(2, [128, 2], F32, "ss")
        hbp = kb.pool(2, [128, D], BF16, "hb")
        hTp = kb.pool(2, [128, 8, 128], BF16, "hT")
        psT = kb.pool(2, [128, 512], F32, "psT", space="psum")
        psP = kb.pool(4, [128, 512], F32, "psP", space="psum")
        pap = kb.pool(2, [128, 3072], BF16, "pa")
        zbp = kb.pool(2, [128, D], BF16, "zb")
        chunks = [(n0, min(512, 4128 - n0)) for n0 in range(0, 4128, 512)]
        for ti in range(NT):
            w = 0 if ti < NLT else 1
            t0 = ti * 128
            xt = xp.next()
            kb.dma("sp", xt.v(), s.X[t0:t0 + 128, :])
            hb = hbp.next()
            s.norm_mod(xt, mod[(w, 1)], mod[(w, 0)], hb, tmpf, junk, ssp.next())
            hT = hTp.next()
            s.transpose_chunks(hT.v(), hb.v(), 8, psT, s.ident_b)
            pa = pap.next()
            zb = zbp.next()
            for ci, (n0, nsz) in enumerate(chunks):
                ps = psP.next()
                for k in range(8):
                    kb.mm(ps[:, 0:nsz], hT[:, k, :], w_in[:, k, n0:n0 + nsz], start=(k == 0), stop=(k == 7))
                eng = "act" if ci % 2 == 0 else "dve"
                if n0 < 3072:
                    kb.copy(pa[:, n0:n0 + nsz], ps[:, 0:nsz], eng=eng)
                elif n0 < 4096:
                    kb.copy(zb[:, n0 - 3072:n0 - 3072 + nsz], ps[:, 0:nsz], eng=eng)
                else:
                    kb.copy(ab_all[:, ti, :], ps[:, 0:32], eng=eng)
            kb.dma("pool", s.PA[t0:t0 + 128, :], pa.v())
            kb.dma("pool", s.H[t0:t0 + 128, :], zb.v())
        kb.pop()
        if getattr(s, "dbg_stop", 9) <= 1:
            kb.pop(); return
        kb.push()
        wc = []
        for k in range(5):
            t = kb.tile([128, 3072], F32, "wc%d" % k)
            kb.dma("sp", t.v(), s.a_conv[j, k:k + 1, :].to_broadcast([128, 3072]))
            wc.append(t)
        nA = kb.tile([128, 16], F32, "nA")
        kb.dma("sp", nA.v(), s.a_log[j:j + 1].rearrange("o d h -> o (d h)").to_broadcast([128, 16]))
        kb.act(nA.v(), nA.v(), AF.Exp)
        kb.ts(nA.v(), nA.v(), -1.0, None, ALU.mult)
        dtb = kb.tile([128, 16], F32, "dtb")
        kb.dma("sp", dtb.v(), s.a_dt_bias[j:j + 1].rearrange("o d h -> o (d h)").to_broadcast([128, 16]))
        onec = kb.tile([128, 1], F32, "onec")
        kb.memset(onec.v(), 1.0)
        shp = kb.pool(5, [128, 3072], BF16, "sh")
        acc = kb.tile([128, 3072], F32, "acc")
        tmp = kb.tile([128, 3072], F32, "tmp")
        qkv = kb.tile([128, 3072], F32, "qkv")
        sq = kb.tile([128, 2048], F32, "sq")
        ssh = kb.tile([128, 16], F32, "ssh")
        gt = kb.tile([128, 16], F32, "gt")
        qknp = kb.pool(1, [128, 2048], BF16, "qkn")
        kvsp = kb.pool(1, [128, 2048], F32, "kvs")
        qkTp = kb.pool(1, [128, 16, 128], BF16, "qkT")
        psT = kb.pool(2, [128, 512], F32, "psT", space="psum")
        for ti in range(NT):
            seg0, seg1 = (0, N) if ti < NLT else (N, T)
            t0 = ti * 128
            shs = []
            for k in range(5):
                r0 = t0 - 2 + k
                r1 = r0 + 128
                lo = max(r0, seg0)
                hi = min(r1, seg1)
                sh = shp.next()
                if lo > r0 or hi < r1:
                    kb.memset(sh.v(), 0.0)
                kb.dma("sp", sh[lo - r0:hi - r0, :], s.PA[lo:hi, :])
                shs.append(sh)
            kb.tt(acc.v(), shs[0].v(), wc[0].v(), ALU.mult)
            for k in range(1, 5):
                kb.tt(tmp.v(), shs[k].v(), wc[k].v(), ALU.mult, eng="pool")
                kb.tt(acc.v(), acc.v(), tmp.v(), ALU.add)
            kb.act(qkv.v(), acc.v(), AF.Silu)
            x3 = qkv[:, 0:2048].re("p (h d) -> p h d", d=128)
            kb.tt(sq.v().re("p (h d) -> p h d", d=128), x3, x3, ALU.mult)
            kb.red(ssh.v(), sq.v().re("p (h d) -> p h d", d=128), ALU.add)
            kb.act(ssh.v(), ssh.v(), AF.Sqrt, bias=s.epst[:, 0:1], scale=1.0)
            kb.recip(ssh.v(), ssh.v())
            kb.ts(ssh[:, 0:8], ssh[:, 0:8], 128.0 ** -0.5, None, ALU.mult)
            qkn = qknp.next()
            rb = ssh.v().re("p (h o) -> p h o", o=1).bc([128, 16, 128])
            kb.tt(qkn.v().re("p (h d) -> p h d", d=128), x3, rb, ALU.mult)
            kvs = kvsp.next()
            kb.tt(kvs[:, 0:1024].re("p (h d) -> p h d", d=128), qkv[:, 1024:2048].re("p (h d) -> p h d", d=128),
                  ssh[:, 8:16].re("p (h o) -> p h o", o=1).bc([128, 8, 128]), ALU.mult)
            kb.copy(kvs[:, 1024:2048], qkv[:, 2048:3072], eng="act")
            qkT = qkTp.next()
            s.transpose_chunks(qkT.v(), qkn.v(), 16, psT, s.ident_b)
            kb.dma("pool", s.QKT[0:16, :, t0:t0 + 128].rearrange("c p t -> p c t"), qkT.v())
            kb.dma("pool", s.KVS[t0:t0 + 128, :], kvs.v())
            kb.tt(gt.v(), ab_all[:, ti, 0:16], dtb.v(), ALU.add)
            kb.act(gt.v(), gt.v(), AF.Exp)
            kb.act(gt.v(), gt.v(), AF.Ln, bias=onec[:, 0:1])
            kb.tt(gb_all[:, ti, 0:16], gt.v(), nA.v(), ALU.mult)
            kb.act(gb_all[:, ti, 16:32], ab_all[:, ti, 16:32], AF.Sigmoid)
        kb.pop()
        if getattr(s, "dbg_stop", 9) <= 2:
            kb.pop(); return
        for d in (0, 1):
            if getattr(s, "dbg_stop", 9) <= 3 + d - 1 + 0 and d == 1:
                break
            kb.push()
            ones_f = kb.tile([128, 128], F32, "ones_f")
            kb.memset(ones_f.v(), 1.0)
            tril_i = kb.tile([128, 128], F32, "tril_i")
            triu_i = kb.tile([128, 128], F32, "triu_i")
            tril_s = kb.tile([128, 128], F32, "tril_s")
            triu_s = kb.tile([128, 128], F32, "triu_s")
            kb.dma("sp", tril_i.v(), s.k_tril)
            kb.dma("sp", triu_i.v(), s.k_triu)
            kb.tt(tril_s.v(), tril_i.v(), s.ident_f.v(), ALU.subtract)
            kb.tt(triu_s.v(), triu_i.v(), s.ident_f.v(), ALU.subtract)
            bd16 = kb.tile([128, 128], F32, "bd16")
            kb.dma("sp", bd16.v(), s.k_bd16)
            offm = []
            for li in range(3):
                t_ = kb.tile([128, 128], F32, "off%d" % li)
                kb.dma("sp", t_.v(), s.k_off[li])
                offm.append(t_)
            if d == 0:
                mA, mAT, mQK, cumM = tril_s, triu_s, triu_i, triu_i
                order = [NLT, NLT + 1] + list(range(NLT))
            else:
                mA, mAT, mQK, cumM = triu_s, tril_s, tril_i, tril_i
                order = [NLT + 1, NLT] + list(range(NLT - 1, -1, -1))
            S = kb.tile([128, 8, 128], F32, "S")
            kb.memset(S.v(), 0.0)
            psR = kb.pool(6, [128, 512], F32, "psR", space="psum")
            kvsp = kb.pool(1, [128, 2048], F32, "kvs")
            qkbp = kb.pool(2, [128, 16, 128], BF16, "qkb")
            qkfp = kb.pool(1, [128, 16, 128], F32, "qkf")
            sc = {nm: kb.pool(2, [128, 8], F32, nm) for nm in ("gcl2", "gam", "e_", "gamL", "bg", "lnb", "gcb", "nb", "tmp8")}
            gclp = kb.pool(2, [128, 16], F32, "gcl")
            m = {nm: kb.pool(6 if nm in ("P", "Q") else 3, [128, 128], F32, nm) for nm in
                 ("dg1", "dg2", "E1", "E2", "E3", "P", "Q", "X", "Xn", "PL", "QL", "I1", "I2", "Mqk", "bV", "bgK", "Kt", "nWT", "Vn", "o1s")}
            otp = kb.pool(2, [128, D], F32, "ot")
            if d == 1:
                stage = kb.pool(1, [128, 8, 512], F32, "stage")
                w_out = kb.tile([128, 8, D], BF16, "w_out")
                s.load_w_bf16(w_out, s.a_w_out[j], D, stage)
                gout = kb.tile([128, 128], F32, "gout")
                kb.dma("sp", gout.v(), s.a_g_out[j:j + 1, :].to_broadcast([128, 128]))
                ofp = kb.pool(1, [128, D], F32, "of")
                zp = kb.pool(2, [128, D], BF16, "z")
                zf = kb.tile([128, D], F32, "zf")
                sqo = kb.tile([128, D], F32, "sqo")
                rs8 = kb.pool(2, [128, 8], F32, "rs8")
                obp = kb.pool(2, [128, D], BF16, "ob")
                oTp = kb.pool(2, [128, 8, 128], BF16, "oT")
                psY = kb.pool(2, [128, 512], F32, "psY", space="psum")
                xp = kb.pool(2, [128, D], F32, "x")
                yt = kb.tile([128, D], F32, "yt")
            for c in order[:int(_os.environ.get('DBG_C', '999'))]:
                t0 = c * 128
                kvs = kvsp.next()
                kb.dma("sp", kvs.v(), s.KVS[t0:t0 + 128, :])
                qkb = qkbp.next()
                kb.dma("sp", qkb.v(), s.QKT[0:16, :, t0:t0 + 128].rearrange("c p t -> p c t"))
                qkf = qkfp.next()
                kb.copy(qkf.v(), qkb.v(), eng="act")
                g8 = gb_all[:, c, d * 8:(d + 1) * 8]
                b8 = gb_all[:, c, 16 + d * 8:16 + (d + 1) * 8]
                ps = psR.next()
                kb.mm(ps[:, 0:8], cumM.v(), g8)
                kb.mm(ps[:, 8:16], ones_f.v(), g8)
                gcl = gclp.next()
                kb.copy(gcl.v(), ps[:, 0:16])
                gc = gcl[:, 0:8]
                gl = gcl[:, 8:16]
                gam = sc["gam"].next(); e_ = sc["e_"].next(); gamL = sc["gamL"].next(); bg = sc["bg"].next()
                lnb = sc["lnb"].next(); gcb = sc["gcb"].next(); nb = sc["nb"].next(); tmp8 = sc["tmp8"].next()
                kb.act(gam.v(), gc, AF.Exp)
                kb.tt(tmp8.v(), gl, gc, ALU.subtract)
                kb.act(e_.v(), tmp8.v(), AF.Exp)
                kb.act(gamL.v(), gl, AF.Exp)
                kb.tt(bg.v(), b8, gam.v(), ALU.mult)
                kb.act(lnb.v(), b8, AF.Ln)
                kb.tt(gcb.v(), gc, lnb.v(), ALU.add)
                kb.ts(nb.v(), b8, -1.0, None, ALU.mult)
                ot = otp.next()
                if int(_os.environ.get("DBG_STEP", "9")) <= 0:
                    continue
                for h in range(int(_os.environ.get("DBG_H", "8"))):
                    Kh = kvs[:, h * 128:(h + 1) * 128]
                    Vh = kvs[:, 1024 + h * 128:1024 + (h + 1) * 128]
                    QT = qkf[:, h, :]
                    KT = qkf[:, 8 + h, :]
                    gch = gcl[:, h:h + 1]
                    pKK = psR.next()
                    kb.mm(pKK[:, 0:128], KT, KT)
                    kb.mm(pKK[:, 128:256], KT, QT)
                    dg1 = m["dg1"].next(); dg2 = m["dg2"].next()
                    kb.ts(dg1.v(), s.ident_f.v(), gch, None, ALU.mult)
                    kb.ts(dg2.v(), s.ident_f.v(), gcb[:, h:h + 1], None, ALU.mult)
                    pBC = psR.next()
                    kb.mm(pBC[:, 0:128], ones_f.v(), dg1.v())
                    kb.mm(pBC[:, 128:256], ones_f.v(), dg2.v())
                    E1 = m["E1"].next(); E2 = m["E2"].next(); E3 = m["E3"].next()
                    P = m["P"].next(); Q = m["Q"].next(); X = m["X"].next(); Mqk = m["Mqk"].next()
                    kb.ts(E1.v(), pBC[:, 0:128], gch, 0.0, ALU.subtract, ALU.max)
                    kb.act(E1.v(), E1.v(), AF.Exp, scale=-1.0)
                    kb.tt(E1.v(), E1.v(), pKK[:, 0:128], ALU.mult)
                    kb.stt(P.v(), E1.v(), nb[:, h:h + 1], mA.v(), ALU.mult, ALU.mult)
                    kb.ts(E2.v(), pBC[:, 128:256], gch, 0.0, ALU.subtract, ALU.min)
                    kb.act(E2.v(), E2.v(), AF.Exp)
                    kb.tt(E2.v(), E2.v(), pKK[:, 0:128], ALU.mult)
                    kb.stt(Q.v(), E2.v(), -1.0, mAT.v(), ALU.mult, ALU.mult)
                    kb.ts(E3.v(), pBC[:, 0:128], gch, 0.0, ALU.subtract, ALU.min)
                    kb.act(E3.v(), E3.v(), AF.Exp)
                    kb.tt(E3.v(), E3.v(), pKK[:, 128:256], ALU.mult)
                    kb.tt(Mqk.v(), E3.v(), mQK.v(), ALU.mult)
                    P0, Q0 = P, Q
                    Pb = m["P"].next(); Qb = m["Q"].next()
                    kb.tt(Pb.v(), P0.v(), bd16.v(), ALU.mult)
                    kb.tt(Qb.v(), Q0.v(), bd16.v(), ALU.mult)
                    Xt = m["X"].next(); Xn = m["Xn"].next()
                    kb.tt(Xt.v(), Qb.v(), s.ident_f.v(), ALU.add)
                    kb.tt(Xn.v(), Pb.v(), s.ident_f.v(), ALU.add)
                    Pk, Qk = Pb, Qb
                    for k in range(1, 4):
                        pp = psR.next()
                        kb.mm(pp[:, 0:128], Qk.v(), Pk.v())
                        kb.mm(pp[:, 128:256], Pk.v(), Qk.v())
                        Pn = m["P"].next(); Qn = m["Q"].next()
                        kb.copy(Pn.v(), pp[:, 0:128])
                        kb.copy(Qn.v(), pp[:, 128:256])
                        pa = psR.next()
                        kb.mm(pa[:, 0:128], Pn.v(), Xt.v())
                        kb.mm(pa[:, 128:256], Qn.v(), Xn.v())
                        Xt2 = m["X"].next(); Xn2 = m["Xn"].next()
                        kb.tt(Xt2.v(), Xt.v(), pa[:, 0:128], ALU.add)
                        kb.tt(Xn2.v(), Xn.v(), pa[:, 128:256], ALU.add)
                        Pk, Qk, Xt, Xn = Pn, Qn, Xt2, Xn2
                    for li in range(3):
                        last = li == 2
                        PL = m["PL"].next()
                        kb.tt(PL.v(), P0.v(), offm[li].v(), ALU.mult)
                        if not last:
                            QL = m["QL"].next()
                            kb.tt(QL.v(), Q0.v(), offm[li].v(), ALU.mult)
                        pi = psR.next()
                        kb.mm(pi[:, 0:128], PL.v(), Xt.v())
                        if not last:
                            kb.mm(pi[:, 128:256], QL.v(), Xn.v())
                        I1 = m["I1"].next()
                        kb.copy(I1.v(), pi[:, 0:128])
                        if not last:
                            I2 = m["I2"].next()
                            kb.copy(I2.v(), pi[:, 128:256])
                        po2 = psR.next()
                        kb.mm(po2[:, 0:128], Xn.v(), I1.v())
                        if not last:
                            kb.mm(po2[:, 128:256], Xt.v(), I2.v())
                        Xt2 = m["X"].next()
                        kb.tt(Xt2.v(), Xt.v(), po2[:, 0:128], ALU.add)
                        if not last:
                            Xn2 = m["Xn"].next()
                            kb.tt(Xn2.v(), Xn.v(), po2[:, 128:256], ALU.add)
                            Xn = Xn2
                        Xt = Xt2
                    X = Xt
                    bV = m["bV"].next(); bgK = m["bgK"].next(); Kt = m["Kt"].next()
                    kb.ts(bV.v(), Vh, b8[:, h:h + 1], None, ALU.mult)
                    kb.ts(bgK.v(), Kh, bg[:, h:h + 1], None, ALU.mult)
                    kb.ts(Kt.v(), Kh, e_[:, h:h + 1], None, ALU.mult)
                    pW = psR.next()
                    kb.mm(pW[:, 0:128], bgK.v(), X.v())
                    nWT = m["nWT"].next()
                    kb.ts(nWT.v(), pW[:, 0:128], -1.0, None, ALU.mult)
                    pV = psR.next()
                    kb.mm(pV[:, 0:128], X.v(), bV.v(), start=True, stop=False)
                    kb.mm(pV[:, 0:128], nWT.v(), S[:, h, :], start=False, stop=True)
                    Vn = m["Vn"].next()
                    kb.copy(Vn.v(), pV[:, 0:128], eng="act")
                    pO = psR.next()
                    kb.mm(pO[:, 0:128], QT, S[:, h, :])
                    kb.mm(pO[:, 128:256], Mqk.v(), Vn.v())
                    o1s = m["o1s"].next()
                    kb.ts(o1s.v(), pO[:, 0:128], gam[:, h:h + 1], None, ALU.mult)
                    kb.tt(ot[:, h * 128:(h + 1) * 128], o1s.v(), pO[:, 128:256], ALU.add)
                    pS = psR.next()
                    kb.mm(pS[:, 0:128], Kt.v(), Vn.v())
                    kb.stt(S[:, h, :], S[:, h, :], gamL[:, h:h + 1], pS[:, 0:128], ALU.mult, ALU.add)
                if d == 0:
                    kb.dma("pool", s.OF[t0:t0 + 128, :], ot.v())
                else:
                    w = 0 if c < NLT else 1
                    of = ofp.next()
                    kb.dma("sp", of.v(), s.OF[t0:t0 + 128, :])
                    kb.tt(ot.v(), ot.v(), of.v(), ALU.add)
                    o3 = ot.v().re("p (h d) -> p h d", d=128)
                    kb.tt(sqo.v(), ot.v(), ot.v(), ALU.mult)
                    r8 = rs8.next()
                    kb.red(r8.v(), sqo.v().re("p (h d) -> p h d", d=128), ALU.add)
                    kb.act(r8.v(), r8.v(), AF.Sqrt, bias=s.epst[:, 0:1], scale=1.0 / 128)
                    kb.recip(r8.v(), r8.v())
                    kb.tt(o3, o3, r8.v().re("p (h o) -> p h o", o=1).bc([128, 8, 128]), ALU.mult)
                    kb.tt(o3, o3, gout.v().re("p (o d) -> p o d", o=1).bc([128, 8, 128]), ALU.mult)
                    z = zp.next()
                    kb.dma("sp", z.v(), s.H[t0:t0 + 128, :])
                    kb.act(zf.v(), z.v(), AF.Silu)
                    ob = obp.next()
                    kb.tt(ob.v(), ot.v(), zf.v(), ALU.mult)
                    oT = oTp.next()
                    s.transpose_chunks(oT.v(), ob.v(), 8, psY, s.ident_b)
                    s.out_proj_res(c, oT, w_out, mod[(w, 2)], psY, xp, yt)
            kb.pop()
        kb.pop()

    def moe(s, l):
        kb = s.kb
        N, T, NT, NLT, S = s.N, s.T, s.NT, s.NLT, s.S
        kb.push()
        mod = s.adaln(l, [3, 4, 5], s.g_ffn[l:l + 1, :])
        affT = kb.tile([16, T], F32, "affT")
        slot_i = kb.tile([128, NT, 16], I32, "slot_i")
        gateT = kb.tile([128, NT, 16], F32, "gateT")
        kb.push()
        wr = kb.tile([128, 8, 16], F32, "wr")
        kb.dma("sp", wr.v(), s.w_router[l].rearrange("(k p) e -> p k e", p=128))
        xp = kb.pool(2, [128, D], F32, "x")
        junk = kb.tile([128, D], F32, "junk")
        tmpf = kb.tile([128, D], F32, "tmpf")
        ssp = kb.pool(2, [128, 2], F32, "ss")
        hfp = kb.pool(2, [128, D], F32, "hf")
        hbp = kb.pool(2, [128, D], BF16, "hb")
        hTf = kb.pool(2, [128, 8, 128], F32, "hTf")
        psT = kb.pool(4, [128, 512], F32, "psT", space="psum")
        psL = kb.pool(2, [128, 512], F32, "psL", space="psum")
        smp = kb.pool(2, [128, 4], F32, "sm")
        ep = kb.pool(2, [128, 16], F32, "e")
        for ti in range(NT):
            w = 0 if ti < NLT else 1
            t0 = ti * 128
            xt = xp.next()
            kb.dma("sp", xt.v(), s.X[t0:t0 + 128, :])
            hf = hfp.next()
            s.norm_mod(xt, mod[(w, 4)], mod[(w, 3)], hf, tmpf, junk, ssp.next())
            hb = hbp.next()
            kb.copy(hb.v(), hf.v(), eng="act")
            kb.dma("pool", s.H[t0:t0 + 128, :], hb.v())
            hT = hTf.next()
            for half in range(2):
                ps = psT.next()
                for i in range(4):
                    k = half * 4 + i
                    kb.tr(ps[:, i * 128:(i + 1) * 128], hf[:, k * 128:(k + 1) * 128], s.ident_f.v())
                kb.copy(hT[:, half * 4:half * 4 + 4, :], ps.v().re("p (k t) -> p k t", t=128), eng="act")
            pl = psL.next()
            for k in range(8):
                kb.mm(pl[:, 0:16], hT[:, k, :], wr[:, k, :], start=(k == 0), stop=(k == 7))
            sm = smp.next()
            e = ep.next()
            kb.red(sm[:, 0:1], pl[:, 0:16], ALU.max)
            kb.ts(sm[:, 1:2], sm[:, 0:1], -1.0, None, ALU.mult)
            kb.memset(sm[:, 2:3], 0.0)
            kb.act(e.v(), pl[:, 0:16], AF.Exp, bias=sm[:, 1:2], accum_out=sm[:, 2:3])
            kb.recip(sm[:, 3:4], sm[:, 2:3])
            kb.ts(e.v(), e.v(), sm[:, 3:4], None, ALU.mult)
            pt = psL.next()
            kb.tr(pt[0:16, 0:128], e.v(), s.ident_f.v())
            kb.copy(affT[:, t0:t0 + 128], pt[0:16, 0:128])
        kb.pop()
        kb.push()
        escal = kb.tile([16, 2], F32, "escal")
        kb.dma("sp", escal.v(), s.k_escal)
        Wmax = max(N, NCTX)
        work = kb.tile([16, Wmax], F32, "work")
        ones = kb.tile([16, 1], F32, "ones")
        kb.memset(ones.v(), 1.0)
        t8 = kb.tile([16, 8], F32, "t8")
        mask = kb.tile([16, T], F32, "mask")
        slotf = kb.tile([16, T], F32, "slotf")
        gsel = affT
        for (c0, c1, cap, ecol) in ((0, N, s.cap_lat, 0), (N, T, s.cap_ctx, 1)):
            n = c1 - c0
            src = affT[:, c0:c1]
            for r in range(cap // 8):
                kb.emit("dve", lambda E, src=src: E.max(out=t8.h[:], in_=src.ap), [src], [t8])
                kb.emit("dve", lambda E, src=src, n=n: E.match_replace(out=work.h[:, 0:n], in_to_replace=t8.h[:],
                                                                        in_values=src.ap, imm_value=0.0),
                        [src, t8], [work])
                src = work[:, 0:n]
            kb.ts(mask[:, c0:c1], work[:, 0:n], 0.0, None, ALU.is_equal)
            kb.emit("dve", lambda E, n=n, c0=c0, c1=c1: E.tensor_tensor_scan(
                out=slotf.h[:, c0:c1], data0=ones.h[:, 0:1].to_broadcast([16, n]), data1=mask.h[:, c0:c1], initial=0.0,
                op0=ALU.mult, op1=ALU.add), [ones, mask], [slotf])
            kb.stt(slotf[:, c0:c1], slotf[:, c0:c1], escal[:, ecol:ecol + 1], mask[:, c0:c1], ALU.add, ALU.mult)
            kb.ts(slotf[:, c0:c1], slotf[:, c0:c1], BIG, None, ALU.add)
        kb.tt(gsel.v(), affT.v(), mask.v(), ALU.mult)
        psS = kb.pool(2, [128, 512], F32, "psS", space="psum")
        slt = kb.tile([128, NT, 16], F32, "slt")
        for (srcT, dst) in ((slotf, slt), (gsel, gateT)):
            for g0 in range(0, NT, 32):
                g1 = min(NT, g0 + 32)
                ps = psS.next()
                for ti in range(g0, g1):
                    kb.tr(ps[:, (ti - g0) * 16:(ti - g0 + 1) * 16], srcT[:, ti * 128:(ti + 1) * 128], s.ident_f[0:16, 0:16])
                kb.copy(dst[:, g0:g1, :], ps[:, 0:(g1 - g0) * 16].re("p (t e) -> p t e", e=16))
        kb.copy(slot_i.v(), slt.v())
        kb.pop()
        kb.push()
        hbp = kb.pool(3, [128, D], BF16, "hb")
        for ti in range(NT):
            t0 = ti * 128
            hb = hbp.next()
            kb.dma("sp", hb.v(), s.H[t0:t0 + 128, :])
            for e in range(16):
                kb.dma("pool", s.XG, hb.v(), indirect=dict(idx=slot_i[:, ti, e:e + 1], side="out", bound=16 * S - 1))
        kb.pop()
        kb.push()
        stage = kb.pool(1, [128, 8, 512], F32, "stage")
        wgu = kb.tile([128, 8, 2 * D], BF16, "wgu")
        wd = kb.tile([128, 8, D], BF16, "wd")
        nst = (S + 127) // 128
        stiles = [(i * 128, min(128, S - i * 128)) for i in range(nst)]
        nchunks = [(n0, min(512, S - n0)) for n0 in range(0, S, 512)]
        xgp = kb.pool(1, [128, nst, D], BF16, "xg")
        xgT = kb.tile([128, 8, S], BF16, "xgT")
        actT = kb.tile([128, 8, S], BF16, "actT")
        psX = kb.pool(2, [128, 512], F32, "psX", space="psum")
        psG = kb.pool(2, [128, 512], F32, "psG", space="psum")
        psU = kb.pool(2, [128, 512], F32, "psU", space="psum")
        psD = kb.pool(2, [128, 512], F32, "psD", space="psum")
        sgp = kb.pool(2, [128, 512], F32, "sg")
        ysp = kb.pool(2, [128, D], BF16, "ys")
        for e in range(16):
            s.load_w_bf16(wgu, s.w_gate_up[l, e], 2 * D, stage)
            s.load_w_bf16(wd, s.w_down[l, e], D, stage)
            xg = xgp.next()
            for i, (s0, sz) in enumerate(stiles):
                kb.dma("sp", xg[0:sz, i, :], s.XG[e * S + s0:e * S + s0 + sz, :])
            for i, (s0, sz) in enumerate(stiles):
                ps = psX.next()
                psb = ps.v().bitcast(BF16)
                for k in range(8):
                    kb.tr(psb[:, k * 128:k * 128 + sz], xg[0:sz, i, k * 128:(k + 1) * 128], s.ident_b[0:sz, 0:sz])
                kb.copy(xgT[:, :, s0:s0 + sz], psb.re("p (k t) -> p k t", t=128)[:, :, 0:sz], eng="act")
            for fc in range(8):
                for (n0, nsz) in nchunks:
                    pg = psG.next()
                    pu = psU.next()
                    for k in range(8):
                        kb.mm(pg[:, 0:nsz], wgu[:, k, fc * 128:(fc + 1) * 128], xgT[:, k, n0:n0 + nsz],
                              start=(k == 0), stop=(k == 7))
                    for k in range(8):
                        kb.mm(pu[:, 0:nsz], wgu[:, k, D + fc * 128:D + (fc + 1) * 128], xgT[:, k, n0:n0 + nsz],
                              start=(k == 0), stop=(k == 7))
                    sg = sgp.next()
                    kb.act(sg[:, 0:nsz], pg[:, 0:nsz], AF.Silu)
                    kb.tt(actT[:, fc, n0:n0 + nsz], sg[:, 0:nsz], pu[:, 0:nsz], ALU.mult)
            for i, (s0, sz) in enumerate(stiles):
                ys = ysp.next()
                for nb in range(2):
                    pd = psD.next()
                    for fk in range(8):
                        kb.mm(pd[0:sz, :], actT[:, fk, s0:s0 + sz], wd[:, fk, nb * 512:(nb + 1) * 512],
                              start=(fk == 0), stop=(fk == 7))
                    kb.copy(ys[0:sz, nb * 512:(nb + 1) * 512], pd[0:sz, :], eng="act")
                kb.dma("pool", s.Y[e * S + s0:e * S + s0 + sz, :], ys[0:sz, :])
        kb.pop()
        kb.push()
        gp = kb.pool(4, [128, D], BF16, "gath")
        for b in gp.bufs:
            kb.memset(b.v(), 0.0)
        accp = kb.pool(2, [128, D], F32, "acc")
        xp = kb.pool(2, [128, D], F32, "x")
        for ti in range(NT):
            w = 0 if ti < NLT else 1
            t0 = ti * 128
            acc = accp.next()
            kb.memset(acc.v(), 0.0)
            for e in range(16):
                g = gp.next()
                kb.dma("pool", g.v(), s.Y, indirect=dict(idx=slot_i[:, ti, e:e + 1], side="in", bound=16 * S - 1))
                kb.stt(acc.v(), g.v(), gateT[:, ti, e:e + 1], acc.v(), ALU.mult, ALU.add)
            xt = xp.next()
            kb.dma("sp", xt.v(), s.X[t0:t0 + 128, :])
            kb.tt(acc.v(), acc.v(), mod[(w, 5)].v(), ALU.mult)
            kb.tt(xt.v(), xt.v(), acc.v(), ALU.add)
            kb.dma("pool", s.X[t0:t0 + 128, :], xt.v())
        kb.pop()
        kb.pop()

    def finish(s):
        kb = s.kb
        for r0 in range(0, s.T, 1024):
            r1 = min(s.T, r0 + 1024)
            kb.dma("sp", s.out[r0:r1, :], s.X[r0:r1, :])
        kb.barrier()


def host_consts(N):
    T = N + NCTX
    S = 2 * N // 16 + 2 * NCTX // 16
    GRID_W = 64
    rows = N // GRID_W
    t_row = np.repeat(np.arange(rows), GRID_W).astype(np.float32)
    t_col = np.tile(np.arange(GRID_W), rows).astype(np.float32)
    n_freq = 16
    inv = (10000.0 ** (-np.arange(n_freq, dtype=np.float32) / n_freq)).astype(np.float32)
    ang = np.concatenate([t_row[:, None] * inv, t_col[:, None] * inv], -1).astype(np.float32)
    cos = np.ones((T, 32), np.float32)
    sin = np.zeros((T, 32), np.float32)
    cos[:N] = np.cos(ang)
    sin[:N] = np.sin(ang)
    a = np.arange(128)
    tril = (a[None, :] <= a[:, None]).astype(np.float32)
    triu = (a[:, None] <= a[None, :]).astype(np.float32)
    e = np.arange(16, dtype=np.float32)
    escal = np.stack([e * S - 1 - BIG, e * S - 1 - BIG + 2 * N // 16], 1).astype(np.float32)
    ii = a[:, None]
    jj = a[None, :]
    bd16 = ((ii // 16) == (jj // 16)).astype(np.float32)
    offs = []
    for b in (16, 32, 64):
        same2b = (ii // (2 * b)) == (jj // (2 * b))
        diffb = (ii // b) != (jj // b)
        offs.append((same2b & diffb).astype(np.float32))
    return dict(k_bd16=bd16, k_off=np.stack(offs, 0), k_ident=np.eye(128, dtype=np.float32), k_cos=cos, k_sin=sin, k_tril=tril, k_triu=triu, k_escal=escal)


from concourse.bass_utils import run_bass_kernel_spmd

N_FULL = 8192


def kernel(**inputs):
    N = N_FULL
    mk = MK(N)
    mk.setup()
    for l in range(4):
        kind, j = l % 3, l // 3
        getattr(mk, ["layer_delta", "layer_swa", "layer_diff"][kind])(l, j)
        mk.moe(l)
    mk.finish()
    consts = host_consts(N)
    in_maps = []
    B = inputs["x"].shape[0]
    for b in range(B):
        im = {}
        for k in mk.inp:
            if k.startswith("k_"):
                im[k] = consts[k]
            elif k in ("x", "ctx", "c"):
                im[k] = np.ascontiguousarray(np.asarray(inputs[k])[b])
            else:
                im[k] = np.ascontiguousarray(np.asarray(inputs[k]))
        in_maps.append(im)
    res = run_bass_kernel_spmd(mk.nc, in_maps, core_ids=list(range(B)))
    out = np.stack([np.asarray(r["out"])[:N] for r in res.results], 0).astype(np.float32)
    return out
```

```python
import numpy as np
from contextlib import ExitStack
import concourse.bass as bass
import concourse.mybir as mybir

F32 = mybir.dt.float32
BF16 = mybir.dt.bfloat16
I32 = mybir.dt.int32
AF = mybir.ActivationFunctionType
ALU = mybir.AluOpType
AX = mybir.AxisListType

SELF_SYNC = True
FOLD_WAITS = True


class V:
    __slots__ = ("buf", "ap")

    def __init__(s, buf, ap):
        s.buf = buf
        s.ap = ap

    def __getitem__(s, k):
        return V(s.buf, s.ap[k])

    def re(s, pat, **kw):
        return V(s.buf, s.ap.rearrange(pat, **kw))

    def bc(s, shape):
        return V(s.buf, s.ap.to_broadcast(shape))

    def bitcast(s, dt):
        return V(s.buf, s.ap.bitcast(dt))


class Buf:
    def __init__(s, kb, handle, name):
        s.kb = kb
        s.h = handle
        s.name = name
        s.w = None
        s.r = {}
        s.sem = None
        s.semcnt = 0

    def __getitem__(s, k):
        return V(s, s.h[k])

    def v(s):
        return V(s, s.h[:])


class Pool:
    def __init__(s, bufs):
        s.bufs = bufs
        s.i = 0

    def next(s):
        b = s.bufs[s.i % len(s.bufs)]
        s.i += 1
        return b


def _ap(x):
    return x.ap if isinstance(x, V) else x


class KB:
    def __init__(s, nc):
        s.nc = nc
        s.E = {"pe": nc.tensor, "act": nc.scalar, "dve": nc.vector, "pool": nc.gpsimd, "sp": nc.sync}
        s.sem = {}
        s.cnt = {}
        s.waited = {e: {} for e in s.E}
        s.semh = {}
        for e in s.E:
            h = nc.alloc_semaphore("sem_" + e)
            s.sem[e] = h
            s.semh[e] = h
            s.cnt[e] = 0
        s.bar = nc.alloc_semaphore("sem_bar")
        s.barcnt = 0
        s.nbuf = 0
        s.dmabufs = []
        s.stack = [ExitStack()]
        s.phase_bufs = [[]]
        s.free_sems = []
        s.bar_t = s.tile([128, 8], F32, name="bar_t")
        s.n_ins = 0

    def tile(s, shape, dtype, name=None, space="sbuf"):
        s.nbuf += 1
        name = (name or "t") + "_%d" % s.nbuf
        if space == "sbuf":
            h = s.stack[-1].enter_context(s.nc.sbuf_tensor(name, list(shape), dtype))
        else:
            h = s.stack[-1].enter_context(s.nc.psum_tensor(name, list(shape), dtype))
        b = Buf(s, h, name)
        s.phase_bufs[-1].append(b)
        return b

    def pool(s, n, shape, dtype, name=None, space="sbuf"):
        return Pool([s.tile(shape, dtype, name=name, space=space) for _ in range(n)])

    def push(s):
        s.stack.append(ExitStack())
        s.phase_bufs.append([])

    def pop(s):
        s.barrier()
        for b in s.phase_bufs.pop():
            if b.sem is not None:
                s.free_sems.append((b.sem, b.semcnt, ("d", b.name)))
                b.sem = None
        s.stack.pop().close()

    def _getsem(s, b):
        if b.sem is None:
            if s.free_sems:
                h, c, oldkey = s.free_sems.pop()
                b.sem = h
                b.semcnt = c
                for e in s.E:
                    s.waited[e][("d", b.name)] = c
            else:
                b.sem = s.nc.alloc_semaphore("sd_" + b.name)
                b.semcnt = 0
            s.semh[("d", b.name)] = b.sem
            s.dmabufs.append(b)
        return b.sem

    def _wait(s, eng, ev):
        key, val = ev
        if s.waited[eng].get(key, 0) >= val:
            return
        if key == eng and (eng == "pe" or eng == "sp" or not SELF_SYNC):
            return
        s.E[eng].wait_ge(s.semh[key], val)
        s.waited[eng][key] = val

    def _need(s, eng, ev, lst):
        key, val = ev
        if s.waited[eng].get(key, 0) >= val:
            return
        if key == eng and (eng == "pe" or eng == "sp" or not SELF_SYNC):
            return
        for i, (k2, v2) in enumerate(lst):
            if k2 == key:
                if v2 < val:
                    lst[i] = (key, val)
                return
        lst.append((key, val))

    def _deps(s, eng, reads, writes, acc=False):
        lst = []
        for b in reads:
            if b.w is not None:
                s._need(eng, b.w, lst)
        for b in writes:
            if b.w is not None:
                if not (acc and b.w[0] == eng):
                    s._need(eng, b.w, lst)
            for k, v in b.r.items():
                s._need(eng, (k, v), lst)
        for ev in lst[:-1]:
            s._wait(eng, ev)
        return lst[-1] if lst else None

    def _fold(s, eng, ins, ev):
        if ev is None:
            return
        key, val = ev
        if FOLD_WAITS:
            ins._wait_ge(s.semh[key], val)
            s.waited[eng][key] = val
        else:
            raise RuntimeError("fold disabled")

    def emit(s, eng, fn, reads, writes, acc=False):
        reads = [x.buf if isinstance(x, V) else x for x in reads if isinstance(x, (V, Buf))]
        writes = [x.buf if isinstance(x, V) else x for x in writes if isinstance(x, (V, Buf))]
        last = s._deps(eng, reads, writes, acc)
        if last is not None and not FOLD_WAITS:
            s._wait(eng, last)
            last = None
        ins = fn(s.E[eng])
        s._fold(eng, ins, last)
        s.cnt[eng] += 1
        ins.then_inc(s.sem[eng], 1)
        ev = (eng, s.cnt[eng])
        for b in reads:
            b.r[eng] = s.cnt[eng]
        for b in writes:
            b.w = ev
            b.r = {}
        s.n_ins += 1
        return ins

    def dma(s, q, out, in_, indirect=None, **kw):
        obuf = out.buf if isinstance(out, V) else None
        ibuf = in_.buf if isinstance(in_, V) else None
        extra = []
        if indirect is not None:
            extra = [indirect["idx"].buf]
        carrier = obuf or ibuf or s.bar_t
        sem = s._getsem(carrier)
        reads = [b for b in [ibuf] + extra if b is not None]
        writes = [b for b in [obuf] if b is not None]
        last = s._deps(q, reads, writes)
        if last is not None and (not FOLD_WAITS or indirect is not None):
            s._wait(q, last)
            last = None
        E = s.E[q]
        if indirect is None:
            ins = E.dma_start(out=_ap(out), in_=_ap(in_), **kw)
        else:
            off = bass.IndirectOffsetOnAxis(ap=indirect["idx"].ap, axis=0)
            if not hasattr(s, "_bregs"):
                s._bregs = {}
            if indirect["bound"] not in s._bregs:
                s._bregs[indirect["bound"]] = E.to_reg(indirect["bound"])
            breg = s._bregs[indirect["bound"]]
            if indirect["side"] == "out":
                ins = E.indirect_dma_start(out=_ap(out), out_offset=off, in_=_ap(in_), in_offset=None,
                                           bounds_check=breg, oob_is_err=False)
            else:
                ins = E.indirect_dma_start(out=_ap(out), out_offset=None, in_=_ap(in_), in_offset=off,
                                           bounds_check=breg, oob_is_err=False)
        s._fold(q, ins, last)
        carrier.semcnt += 16
        ins.then_inc(sem, 16)
        key = ("d", carrier.name)
        ev = (key, carrier.semcnt)
        for b in reads:
            b.r[key] = carrier.semcnt
        for b in writes:
            b.w = ev
            b.r = {}
        s.n_ins += 1
        return ins

    def barrier(s):
        for e in s.E:
            if e != "pool" and s.cnt[e] > 0:
                s._wait("pool", (e, s.cnt[e]))
        if s.cnt["pool"] > 0:
            key, val = "pool", s.cnt["pool"]
            if s.waited["pool"].get(key, 0) < val:
                s.E["pool"].wait_ge(s.sem["pool"], val)
                s.waited["pool"][key] = val
        for b in s.dmabufs:
            if b.sem is not None and b.semcnt > 0:
                s._wait("pool", (("d", b.name), b.semcnt))
        s.barcnt += 1
        s.E["pool"].memset(s.bar_t.h[:, 0:1], 0.0).then_inc(s.bar, 1)
        for e in s.E:
            if e != "pool":
                s.E[e].wait_ge(s.bar, s.barcnt)
        s.E["pool"].wait_ge(s.bar, s.barcnt)
        for e in s.E:
            for e2 in s.E:
                s.waited[e][e2] = s.cnt[e2]
            for b in s.dmabufs:
                if b.sem is not None:
                    s.waited[e][("d", b.name)] = b.semcnt
        for lst in s.phase_bufs:
            for b in lst:
                b.w = None
                b.r = {}
        s.dmabufs = [b for b in s.dmabufs if b.sem is not None]

    def mm(s, out, lhsT, rhs, start=True, stop=True):
        return s.emit("pe", lambda E: E.matmul(_ap(out), _ap(lhsT), _ap(rhs), start=start, stop=stop),
                      [lhsT, rhs], [out], acc=not start)

    def tr(s, out, in_, ident):
        return s.emit("pe", lambda E: E.transpose(_ap(out), _ap(in_), _ap(ident)), [in_, ident], [out])

    def act(s, out, in_, func, bias=None, scale=None, accum_out=None, eng="act"):
        kw = {}
        rd = [in_]
        wr = [out]
        if bias is not None:
            kw["bias"] = _ap(bias)
            rd.append(bias)
        if scale is not None:
            kw["scale"] = _ap(scale)
            rd.append(scale)
        if accum_out is not None:
            kw["accum_out"] = _ap(accum_out)
            wr.append(accum_out)
        return s.emit(eng, lambda E: E.activation(out=_ap(out), in_=_ap(in_), func=func, **kw), rd, wr)

    def tt(s, out, a, b, op, eng="dve"):
        return s.emit(eng, lambda E: E.tensor_tensor(out=_ap(out), in0=_ap(a), in1=_ap(b), op=op), [a, b], [out])

    def ts(s, out, a, s1, s2, op0, op1=None, eng="dve"):
        kw = {}
        if op1 is not None:
            kw["op1"] = op1
        return s.emit(eng, lambda E: E.tensor_scalar(out=_ap(out), in0=_ap(a), scalar1=_ap(s1), scalar2=_ap(s2),
                                                     op0=op0, **kw), [a, s1, s2], [out])

    def stt(s, out, in0, scalar, in1, op0, op1, eng="dve"):
        return s.emit(eng, lambda E: E.scalar_tensor_tensor(out=_ap(out), in0=_ap(in0), scalar=_ap(scalar),
                                                            in1=_ap(in1), op0=op0, op1=op1),
                      [in0, scalar, in1], [out])

    def copy(s, out, in_, eng="dve"):
        if eng == "act":
            return s.emit(eng, lambda E: E.copy(out=_ap(out), in_=_ap(in_)), [in_], [out])
        return s.emit(eng, lambda E: E.tensor_copy(out=_ap(out), in_=_ap(in_)), [in_], [out])

    def red(s, out, in_, op, axis=AX.X, eng="dve"):
        return s.emit(eng, lambda E: E.tensor_reduce(out=_ap(out), in_=_ap(in_), axis=axis, op=op), [in_], [out])

    def memset(s, out, val, eng="dve"):
        return s.emit(eng, lambda E: E.memset(_ap(out), val), [], [out])

    def recip(s, out, in_):
        return s.emit("dve", lambda E: E.reciprocal(out=_ap(out), in_=_ap(in_)), [in_], [out])


import os as _os

D = 1024
NCTX = 256
EPS = 1e-6
BIG = 32768.0


class MK:
    def __init__(s, N):
        s.N = N
        s.T = N + NCTX
        s.NT = s.T // 128
        s.NLT = N // 128
        s.cap_lat = 2 * N // 16
        s.cap_ctx = 2 * NCTX // 16
        s.S = s.cap_lat + s.cap_ctx
        nc = bass.Bass("TRN2", target_bir_lowering=False)
        s.nc = nc
        s.inp = {}
        s.kb = KB(nc)

    def din(s, name, shape, dt=F32):
        t = s.nc.dram_tensor(name, list(shape), dt, kind="ExternalInput").ap()
        s.inp[name] = t
        return t

    def dscr(s, name, shape, dt):
        return s.nc.dram_tensor(name, list(shape), dt, kind="Internal").ap()

    def setup(s):
        kb = s.kb
        N, T = s.N, s.T
        s.x_in = s.din("x", [N, D])
        s.ctx_in = s.din("ctx", [NCTX, D])
        s.c_in = s.din("c", [D])
        s.cc_in = s.din("c_ctx", [D])
        s.w_ada = s.din("w_ada", [4, D, 6 * D])
        s.b_ada = s.din("b_ada", [4, 6 * D])
        s.g_mix = s.din("g_mix", [4, D])
        s.g_ffn = s.din("g_ffn", [4, D])
        s.a_w_in = s.din("a_w_in", [2, D, 4128])
        s.a_conv = s.din("a_conv", [2, 5, 3072])
        s.a_log = s.din("a_log", [2, 2, 8])
        s.a_dt_bias = s.din("a_dt_bias", [2, 2, 8])
        s.a_g_out = s.din("a_g_out", [2, 128])
        s.a_w_out = s.din("a_w_out", [2, D, D])
        s.b_w_in = s.din("b_w_in", [1, D, 1536])
        s.b_q_norm = s.din("b_q_norm", [1, 64])
        s.b_k_norm = s.din("b_k_norm", [1, 64])
        s.b_sink = s.din("b_sink", [1, 16])
        s.b_w_out = s.din("b_w_out", [1, D, D])
        s.c_w_in = s.din("c_w_in", [1, D, 3072])
        s.c_q_norm = s.din("c_q_norm", [1, 64])
        s.c_k_norm = s.din("c_k_norm", [1, 64])
        s.c_lambda = s.din("c_lambda", [1, 4, 64])
        s.c_g_sub = s.din("c_g_sub", [1, 128])
        s.c_w_out = s.din("c_w_out", [1, D, D])
        s.w_router = s.din("w_router", [4, D, 16])
        s.w_gate_up = s.din("w_gate_up", [4, 16, D, 2 * D])
        s.w_down = s.din("w_down", [4, 16, D, D])
        s.k_ident = s.din("k_ident", [128, 128])
        s.k_cos = s.din("k_cos", [T, 32])
        s.k_sin = s.din("k_sin", [T, 32])
        s.k_tril = s.din("k_tril", [128, 128])
        s.k_triu = s.din("k_triu", [128, 128])
        s.k_bd16 = s.din("k_bd16", [128, 128])
        s.k_off = s.din("k_off", [3, 128, 128])
        s.k_escal = s.din("k_escal", [16, 2])
        s.out = s.nc.dram_tensor("out", [T, D], F32, kind="ExternalOutput").ap()
        s.X = s.dscr("X", [T, D], F32)
        s.H = s.dscr("H", [T, D], BF16)
        s.XG = s.dscr("XG", [16 * s.S, D], BF16)
        s.Y = s.dscr("Y", [16 * s.S, D], BF16)
        s.QKT = s.dscr("QKT", [24, 128, T], BF16)
        s.VA = s.dscr("VA", [T, 4 * 65], BF16)
        s.PA = s.dscr("PA", [T, 3072], BF16)
        s.KVS = s.dscr("KVS", [T, 2048], F32)
        s.OF = s.dscr("OF", [T, D], F32)
        s.ident_f = kb.tile([128, 128], F32, "ident_f")
        s.ident_b = kb.tile([128, 128], BF16, "ident_b")
        kb.dma("sp", s.ident_f.v(), s.k_ident)
        kb.copy(s.ident_b.v(), s.ident_f.v())
        s.epst = kb.tile([128, 1], F32, "eps")
        kb.memset(s.epst.v(), EPS)
        s.scb = []
        for i, cin in enumerate((s.c_in, s.cc_in)):
            ct = kb.tile([128, 8], F32, "c%d" % i)
            kb.dma("sp", ct.v(), cin.rearrange("(k p) -> p k", p=128), allow_slow_non_contiguous=True)
            kb.act(ct.v(), ct.v(), AF.Silu)
            sb = kb.tile([128, 8, 128], F32, "scb%d" % i)
            kb.copy(sb.v(), ct.v().re("p (k o) -> p k o", o=1).bc([128, 8, 128]))
            s.scb.append(sb)
        for r0 in range(0, N, 1024):
            kb.dma("sp", s.X[r0:r0 + 1024, :], s.x_in[r0:r0 + 1024, :])
        kb.dma("sp", s.X[N:T, :], s.ctx_in)
        kb.barrier()

    def adaln(s, l, js, g_ap):
        kb = s.kb
        res = {}
        for w in (0, 1):
            for j in js:
                res[(w, j)] = kb.tile([128, D], F32, "mod%d_%d" % (w, j))
        kb.push()
        wpool = kb.pool(2, [128, 8, 512], F32, "wada")
        bpool = kb.pool(2, [128, 512], F32, "bada")
        pspool = kb.pool(2, [128, 512], F32, "ps_ada", space="psum")
        for j in js:
            for half in (0, 1):
                n0 = j * D + half * 512
                wt = wpool.next()
                kb.dma("sp", wt.v(), s.w_ada[l, :, n0:n0 + 512].rearrange("(k p) n -> p k n", p=128))
                bt = bpool.next()
                kb.dma("sp", bt.v(), s.b_ada[l:l + 1, n0:n0 + 512].to_broadcast([128, 512]))
                for w in (0, 1):
                    ps = pspool.next()
                    for k in range(8):
                        kb.mm(ps.v(), s.scb[w][:, k, :], wt[:, k, :], start=(k == 0), stop=(k == 7))
                    kb.tt(res[(w, j)][:, half * 512:(half + 1) * 512], ps.v(), bt.v(), ALU.add)
        gt = kb.tile([128, D], F32, "gt")
        kb.dma("sp", gt.v(), g_ap.to_broadcast([128, D]))
        for w in (0, 1):
            t = res[(w, js[1])]
            kb.stt(t.v(), t.v(), 1.0, gt.v(), ALU.add, ALU.mult)
        kb.pop()
        return res

    def norm_mod(s, xt, gs, sh, out, tmp_f, junk, ss):
        kb = s.kb
        kb.memset(ss.v(), 0.0)
        kb.act(junk.v(), xt.v(), AF.Square, accum_out=ss[:, 0:1])
        kb.act(ss[:, 1:2], ss[:, 0:1], AF.Sqrt, bias=s.epst[:, 0:1], scale=1.0 / D)
        kb.recip(ss[:, 1:2], ss[:, 1:2])
        kb.stt(tmp_f.v(), xt.v(), ss[:, 1:2], gs.v(), ALU.mult, ALU.mult)
        kb.tt(out.v(), tmp_f.v(), sh.v(), ALU.add)

    def transpose_chunks(s, dst, src, n, pspool, ident, evac="act"):
        kb = s.kb
        c = 0
        while c < n:
            m = min(8, n - c)
            ps = pspool.next()
            psb = ps.v().bitcast(BF16)
            for i in range(m):
                kb.tr(psb[:, i * 128:(i + 1) * 128], src[:, (c + i) * 128:(c + i + 1) * 128], ident.v())
            kb.copy(dst[:, c:c + m, :], psb[:, 0:m * 128].re("p (k t) -> p k t", t=128), eng=evac)
            c += m

    def load_w_bf16(s, dst, src_ap, ncols, stage_pool, chunk=512, engs=("dve", "act")):
        kb = s.kb
        i = 0
        for n0 in range(0, ncols, chunk):
            nsz = min(chunk, ncols - n0)
            st = stage_pool.next()
            kb.dma("sp", st[:, :, 0:nsz], src_ap[:, n0:n0 + nsz].rearrange("(k p) n -> p k n", p=128))
            kb.copy(dst[:, :, n0:n0 + nsz], st[:, :, 0:nsz], eng=engs[i % len(engs)])
            i += 1

    def qk_post(s, pqs, nh, G, cs, sn, qn, sq, ssh, t1, t2):
        kb = s.kb
        x3 = pqs.re("p (h d) -> p h d", d=64)
        kb.tt(sq.v().re("p (h d) -> p h d", d=64)[:, 0:nh, :], x3, x3, ALU.mult)
        kb.red(ssh[:, 0:nh], sq.v().re("p (h d) -> p h d", d=64)[:, 0:nh, :], ALU.add)
        kb.act(ssh[:, 0:nh], ssh[:, 0:nh], AF.Sqrt, bias=s.epst[:, 0:1], scale=1.0 / 64)
        kb.recip(ssh[:, 0:nh], ssh[:, 0:nh])
        q3 = qn.v().re("p (h d) -> p h d", d=64)[:, 0:nh, :]
        kb.tt(q3, x3, ssh[:, 0:nh].re("p (h o) -> p h o", o=1).bc([128, nh, 64]), ALU.mult)
        kb.tt(q3, q3, G.v().re("p (h d) -> p h d", d=64)[:, 0:nh, :], ALU.mult)
        x1 = q3[:, :, 0:32]
        x2 = q3[:, :, 32:64]
        cb = cs.v().re("p (o d) -> p o d", o=1).bc([128, nh, 32])
        sb = sn.v().re("p (o d) -> p o d", o=1).bc([128, nh, 32])
        t13 = t1.v().re("p (h d) -> p h d", d=64)[:, 0:nh, :]
        t23 = t2.v().re("p (h d) -> p h d", d=64)[:, 0:nh, :]
        kb.tt(t13[:, :, 0:32], x1, cb, ALU.mult)
        kb.tt(t13[:, :, 32:64], x2, cb, ALU.mult)
        kb.tt(t23[:, :, 0:32], x2, sb, ALU.mult)
        kb.tt(t23[:, :, 32:64], x1, sb, ALU.mult)
        return t13, t23

    def layer_swa(s, l, j):
        kb = s.kb
        N, T, NT, NLT = s.N, s.T, s.NT, s.NLT
        kb.push()
        mod = s.adaln(l, [0, 1, 2], s.g_mix[l:l + 1, :])
        kb.push()
        stage = kb.pool(2, [128, 8, 512], F32, "stage")
        w_in = kb.tile([128, 8, 1536], BF16, "w_in")
        s.load_w_bf16(w_in, s.b_w_in[j], 1536, stage)
        G = kb.tile([128, 20 * 64], F32, "G")
        kb.dma("sp", G[:, 0:64], s.b_q_norm[j:j + 1, :].to_broadcast([128, 64]))
        kb.dma("sp", G[:, 1024:1088], s.b_k_norm[j:j + 1, :].to_broadcast([128, 64]))
        kb.ts(G[:, 0:64], G[:, 0:64], 0.125, None, ALU.mult)
        for h in range(1, 16):
            kb.copy(G[:, h * 64:(h + 1) * 64], G[:, 0:64])
        for h in range(1, 4):
            kb.copy(G[:, 1024 + h * 64:1024 + (h + 1) * 64], G[:, 1024:1088])
        xp = kb.pool(2, [128, D], F32, "x")
        junk = kb.tile([128, D], F32, "junk")
        tmpf = kb.tile([128, D], F32, "tmpf")
        ssp = kb.pool(2, [128, 2], F32, "ss")
        hbp = kb.pool(2, [128, D], BF16, "hb")
        hTp = kb.pool(2, [128, 8, 128], BF16, "hT")
        psT = kb.pool(2, [128, 512], F32, "psT", space="psum")
        psP = kb.pool(3, [128, 512], F32, "psP", space="psum")
        pqs = kb.tile([128, 1536], F32, "pqs")
        sq = kb.tile([128, 1280], F32, "sq")
        ssh = kb.tile([128, 20], F32, "ssh")
        qn = kb.tile([128, 1280], F32, "qn")
        t1 = kb.tile([128, 1280], F32, "t1")
        t2 = kb.tile([128, 1280], F32, "t2")
        csp = kb.pool(2, [128, 32], F32, "cs")
        snp = kb.pool(2, [128, 32], F32, "sn")
        qkbp = kb.pool(2, [128, 1536], BF16, "qkb")
        qkTp = kb.pool(2, [128, 12, 128], BF16, "qkT")
        vap = kb.pool(2, [128, 4, 65], BF16, "va")
        for b in vap.bufs:
            kb.memset(b.v(), 1.0)
        for ti in range(NT):
            w = 0 if ti < NLT else 1
            t0 = ti * 128
            xt = xp.next()
            kb.dma("sp", xt.v(), s.X[t0:t0 + 128, :])
            hb = hbp.next()
            s.norm_mod(xt, mod[(w, 1)], mod[(w, 0)], hb, tmpf, junk, ssp.next())
            hT = hTp.next()
            s.transpose_chunks(hT.v(), hb.v(), 8, psT, s.ident_b)
            for nb in range(3):
                ps = psP.next()
                for k in range(8):
                    kb.mm(ps.v(), hT[:, k, :], w_in[:, k, nb * 512:(nb + 1) * 512], start=(k == 0), stop=(k == 7))
                kb.copy(pqs[:, nb * 512:(nb + 1) * 512], ps.v(), eng="act")
            cs = csp.next()
            sn = snp.next()
            kb.dma("sp", cs.v(), s.k_cos[t0:t0 + 128, :])
            kb.dma("sp", sn.v(), s.k_sin[t0:t0 + 128, :])
            t13, t23 = s.qk_post(pqs[:, 0:1280], 20, G, cs, sn, qn, sq, ssh, t1, t2)
            qkb = qkbp.next()
            q3 = qkb[:, 0:1024].re("p (h d) -> p h d", d=64)
            k3 = qkb[:, 1024:1536].re("p (h d) -> p h d", d=128)
            kb.tt(q3[:, :, 0:32], t13[:, 0:16, 0:32], t23[:, 0:16, 0:32], ALU.subtract)
            kb.tt(q3[:, :, 32:64], t13[:, 0:16, 32:64], t23[:, 0:16, 32:64], ALU.add)
            kb.tt(k3[:, :, 0:32], t13[:, 16:20, 0:32], t23[:, 16:20, 0:32], ALU.subtract)
            kb.tt(k3[:, :, 32:64], t13[:, 16:20, 32:64], t23[:, 16:20, 32:64], ALU.add)
            kb.copy(k3[:, :, 64:128], k3[:, :, 0:64])
            qkT = qkTp.next()
            s.transpose_chunks(qkT.v(), qkb.v(), 12, psT, s.ident_b)
            kb.dma("pool", s.QKT[0:12, :, t0:t0 + 128].rearrange("c p t -> p c t"), qkT.v())
            va = vap.next()
            kb.copy(va[:, :, 0:64], pqs[:, 1280:1536].re("p (h d) -> p h d", d=64), eng="act")
            kb.dma("pool", s.VA[t0:t0 + 128, :], va.v().re("p h d -> p (h d)"))
        kb.pop()
        kb.push()
        stage = kb.pool(2, [128, 8, 512], F32, "stage")
        w_out = kb.tile([128, 8, D], BF16, "w_out")
        s.load_w_bf16(w_out, s.b_w_out[j], D, stage)
        esink = kb.tile([128, 16], F32, "esink")
        kb.dma("sp", esink.v(), s.b_sink[j:j + 1, :].to_broadcast([128, 16]))
        kb.act(esink.v(), esink.v(), AF.Exp)
        mf = kb.tile([128, 128], F32, "mf")
        m_prev = kb.tile([128, 128], BF16, "m_prev")
        m_next = kb.tile([128, 128], BF16, "m_next")
        kb.dma("sp", mf.v(), s.k_tril)
        kb.copy(m_prev.v(), mf.v())
        kb.dma("sp", mf.v(), s.k_triu)
        kb.copy(m_next.v(), mf.v())
        kTc = kb.tile([128, 4, 256], BF16, "kTc")
        kb.dma("sp", kTc.v(), s.QKT[8:12, :, N:T].rearrange("c p t -> p c t"))
        Vc = kb.tile([128, 2, 260], BF16, "Vc")
        kb.dma("sp", Vc.v(), s.VA[N:T, :].rearrange("(j p) e -> p j e", p=128))
        qTp = kb.pool(2, [128, 8, 128], BF16, "qT")
        kTwp = kb.pool(2, [128, 4, 384], BF16, "kTw")
        Vwp = kb.pool(2, [128, 3, 260], BF16, "Vw")
        psA = kb.pool(2, [128, 512], F32, "psA", space="psum")
        psB = kb.pool(2, [128, 512], F32, "psB", space="psum")
        psO = kb.pool(2, [128, 512], F32, "psO", space="psum")
        psY = kb.pool(2, [128, 512], F32, "psY", space="psum")
        PTp = kb.pool(3, [128, 5, 128], BF16, "PT")
        osbp = kb.pool(2, [128, D], BF16, "osb")
        oTp = kb.pool(2, [128, 8, 128], BF16, "oT")
        denp = kb.pool(4, [128, 2], F32, "den")
        xp = kb.pool(2, [128, D], F32, "x")
        yt = kb.tile([128, D], F32, "yt")
        for ti in range(NT):
            lat = ti < NLT
            t0 = ti * 128
            qT = qTp.next()
            kb.dma("sp", qT.v(), s.QKT[0:8, :, t0:t0 + 128].rearrange("c p t -> p c t"))
            if lat:
                j0 = max(0, ti - 1)
                j1 = min(NLT, ti + 2)
                nw = j1 - j0
                kTw = kTwp.next()
                kb.dma("sp", kTw[:, :, 0:nw * 128], s.QKT[8:12, :, j0 * 128:j1 * 128].rearrange("c p t -> p c t"))
                Vw = Vwp.next()
                kb.dma("sp", Vw[:, 0:nw, :], s.VA[j0 * 128:j1 * 128, :].rearrange("(j p) e -> p j e", p=128))
            else:
                nw = 0
                j0 = 0
            osb = osbp.next()
            for hq in range(16):
                kv = hq // 4
                po = (hq % 2) * 64
                qh = qT[po:po + 64, hq // 2, :]
                pa = psA.next()
                pb = psB.next()
                PT = PTp.next()
                for jj in range(nw):
                    kb.mm(pa[:, jj * 128:(jj + 1) * 128], kTw[po:po + 64, kv, jj * 128:(jj + 1) * 128], qh)
                for jj in range(2):
                    kb.mm(pb[:, jj * 128:(jj + 1) * 128], kTc[po:po + 64, kv, jj * 128:(jj + 1) * 128], qh)
                if nw:
                    kb.act(PT[:, 0:nw, :], pa[:, 0:nw * 128].re("p (j t) -> p j t", t=128), AF.Exp)
                    if ti - 1 >= 0:
                        kb.tt(PT[:, 0, :], PT[:, 0, :], m_prev.v(), ALU.mult)
                    if ti + 1 < NLT:
                        kb.tt(PT[:, nw - 1, :], PT[:, nw - 1, :], m_next.v(), ALU.mult)
                kb.act(PT[:, 3:5, :], pb[:, 0:256].re("p (j t) -> p j t", t=128), AF.Exp)
                po_ = psO.next()
                nmm = nw + 2
                i = 0
                for jj in range(nw):
                    kb.mm(po_[:, 0:65], PT[:, jj, :], Vw[:, jj, kv * 65:(kv + 1) * 65], start=(i == 0), stop=(i == nmm - 1))
                    i += 1
                for jj in range(2):
                    kb.mm(po_[:, 0:65], PT[:, 3 + jj, :], Vc[:, jj, kv * 65:(kv + 1) * 65], start=(i == 0), stop=(i == nmm - 1))
                    i += 1
                den = denp.next()
                kb.tt(den[:, 0:1], po_[:, 64:65], esink[:, hq:hq + 1], ALU.add)
                kb.recip(den[:, 1:2], den[:, 0:1])
                kb.ts(osb[:, hq * 64:(hq + 1) * 64], po_[:, 0:64], den[:, 1:2], None, ALU.mult)
            oT = oTp.next()
            s.transpose_chunks(oT.v(), osb.v(), 8, psY, s.ident_b)
            xt = xp.next()
            kb.dma("sp", xt.v(), s.X[t0:t0 + 128, :])
            w = 0 if lat else 1
            for nb in range(2):
                ps = psY.next()
                for k in range(8):
                    kb.mm(ps.v(), oT[:, k, :], w_out[:, k, nb * 512:(nb + 1) * 512], start=(k == 0), stop=(k == 7))
                kb.tt(yt[:, nb * 512:(nb + 1) * 512], ps.v(), mod[(w, 2)][:, nb * 512:(nb + 1) * 512], ALU.mult)
            kb.tt(xt.v(), xt.v(), yt.v(), ALU.add)
            kb.dma("pool", s.X[t0:t0 + 128, :], xt.v())
        kb.pop()
        kb.pop()

    def out_proj_res(s, ti, oT, w_out, gate, psY, xp, yt):
        kb = s.kb
        t0 = ti * 128
        xt = xp.next()
        kb.dma("sp", xt.v(), s.X[t0:t0 + 128, :])
        for nb in range(2):
            ps = psY.next()
            for k in range(8):
                kb.mm(ps.v(), oT[:, k, :], w_out[:, k, nb * 512:(nb + 1) * 512], start=(k == 0), stop=(k == 7))
            kb.tt(yt[:, nb * 512:(nb + 1) * 512], ps.v(), gate[:, nb * 512:(nb + 1) * 512], ALU.mult)
        kb.tt(xt.v(), xt.v(), yt.v(), ALU.add)
        kb.dma("pool", s.X[t0:t0 + 128, :], xt.v())

    def layer_diff(s, l, j):
        import math
        kb = s.kb
        N, T, NT, NLT = s.N, s.T, s.NT, s.NLT
        lam_init = 0.8 - 0.6 * math.exp(-0.3 * l)
        kb.push()
        mod = s.adaln(l, [0, 1, 2], s.g_mix[l:l + 1, :])
        kb.push()
        stage = kb.pool(2, [128, 8, 512], F32, "stage")
        w_in = kb.tile([128, 8, 3072], BF16, "w_in")
        s.load_w_bf16(w_in, s.c_w_in[j], 3072, stage)
        G = kb.tile([128, 2048], F32, "G")
        kb.dma("sp", G[:, 0:64], s.c_q_norm[j:j + 1, :].to_broadcast([128, 64]))
        kb.dma("sp", G[:, 1024:1088], s.c_k_norm[j:j + 1, :].to_broadcast([128, 64]))
        kb.ts(G[:, 0:64], G[:, 0:64], 0.125, None, ALU.mult)
        for h in range(1, 16):
            kb.copy(G[:, h * 64:(h + 1) * 64], G[:, 0:64])
            kb.copy(G[:, 1024 + h * 64:1024 + (h + 1) * 64], G[:, 1024:1088])
        xp = kb.pool(2, [128, D], F32, "x")
        junk = kb.tile([128, D], F32, "junk")
        tmpf = kb.tile([128, D], F32, "tmpf")
        ssp = kb.pool(2, [128, 2], F32, "ss")
        hbp = kb.pool(2, [128, D], BF16, "hb")
        hTp = kb.pool(2, [128, 8, 128], BF16, "hT")
        psT = kb.pool(2, [128, 512], F32, "psT", space="psum")
        psP = kb.pool(4, [128, 512], F32, "psP", space="psum")
        pqs = kb.tile([128, 2048], F32, "pqs")
        sq = kb.tile([128, 2048], F32, "sq")
        ssh = kb.tile([128, 32], F32, "ssh")
        qn = kb.tile([128, 2048], F32, "qn")
        t1 = kb.tile([128, 2048], F32, "t1")
        t2 = kb.tile([128, 2048], F32, "t2")
        csp = kb.pool(2, [128, 32], F32, "cs")
        snp = kb.pool(2, [128, 32], F32, "sn")
        qkbp = kb.pool(2, [128, 2048], BF16, "qkb")
        qkTp = kb.pool(2, [128, 16, 128], BF16, "qkT")
        vbp = kb.pool(2, [128, D], BF16, "vb")
        for ti in range(NT):
            w = 0 if ti < NLT else 1
            t0 = ti * 128
            xt = xp.next()
            kb.dma("sp", xt.v(), s.X[t0:t0 + 128, :])
            hb = hbp.next()
            s.norm_mod(xt, mod[(w, 1)], mod[(w, 0)], hb, tmpf, junk, ssp.next())
            hT = hTp.next()
            s.transpose_chunks(hT.v(), hb.v(), 8, psT, s.ident_b)
            vb = vbp.next()
            for nb in range(6):
                ps = psP.next()
                for k in range(8):
                    kb.mm(ps.v(), hT[:, k, :], w_in[:, k, nb * 512:(nb + 1) * 512], start=(k == 0), stop=(k == 7))
                if nb < 4:
                    kb.copy(pqs[:, nb * 512:(nb + 1) * 512], ps.v(), eng="act")
                else:
                    kb.copy(vb[:, (nb - 4) * 512:(nb - 3) * 512], ps.v(), eng="act")
            kb.dma("pool", s.H[t0:t0 + 128, :], vb.v())
            cs = csp.next()
            sn = snp.next()
            kb.dma("sp", cs.v(), s.k_cos[t0:t0 + 128, :])
            kb.dma("sp", sn.v(), s.k_sin[t0:t0 + 128, :])
            t13, t23 = s.qk_post(pqs[:, 0:2048], 32, G, cs, sn, qn, sq, ssh, t1, t2)
            qkb = qkbp.next()
            q3 = qkb.v().re("p (h d) -> p h d", d=64)
            kb.tt(q3[:, :, 0:32], t13[:, :, 0:32], t23[:, :, 0:32], ALU.subtract)
            kb.tt(q3[:, :, 32:64], t13[:, :, 32:64], t23[:, :, 32:64], ALU.add)
            qkT = qkTp.next()
            s.transpose_chunks(qkT.v(), qkb.v(), 16, psT, s.ident_b)
            kb.dma("pool", s.QKT[0:16, :, t0:t0 + 128].rearrange("c p t -> p c t"), qkT.v())
        kb.pop()
        kb.push()
        lv = kb.tile([128, 256], F32, "lv")
        kb.dma("sp", lv.v(), s.c_lambda[j:j + 1].rearrange("o a d -> o (a d)").to_broadcast([128, 256]))
        lt = kb.tile([128, 128], F32, "lt")
        lam = kb.tile([128, 4], F32, "lam")
        kb.tt(lt[:, 0:64], lv[:, 0:64], lv[:, 64:128], ALU.mult)
        kb.tt(lt[:, 64:128], lv[:, 128:192], lv[:, 192:256], ALU.mult)
        kb.red(lam[:, 0:2], lt.v().re("p (a d) -> p a d", d=64), ALU.add)
        kb.act(lam[:, 0:2], lam[:, 0:2], AF.Exp)
        kb.tt(lam[:, 2:3], lam[:, 1:2], lam[:, 0:1], ALU.subtract)
        kb.ts(lam[:, 3:4], lam[:, 2:3], -lam_init, None, ALU.add)
        gsub = kb.tile([128, 1], F32, "gsub")
        kb.dma("sp", gsub.v(), s.c_g_sub[j].rearrange("(p o) -> p o", o=1))
        kb.ts(gsub.v(), gsub.v(), 1.0 - lam_init, None, ALU.mult)
        ones_b = kb.tile([128, 128], BF16, "ones_b")
        kb.memset(ones_b.v(), 1.0)
        ones_f = kb.tile([128, 128], F32, "ones_f")
        kb.memset(ones_f.v(), 1.0)
        kTp = kb.pool(2, [128, T], BF16, "kT")
        Vp = kb.pool(2, [128, NT, 128], BF16, "V")
        qTp = kb.pool(2, [128, 512], BF16, "qT")
        psS = kb.pool(2, [128, 512], F32, "psS", space="psum")
        psO = [kb.tile([128, 512], F32, "psO%d" % m, space="psum") for m in range(2)]
        psD = [kb.tile([128, 512], F32, "psD%d" % m, space="psum") for m in range(2)]
        psN = kb.pool(2, [128, 512], F32, "psN", space="psum")
        PTp = kb.pool(4, [128, 512], BF16, "PT")
        rp = kb.pool(2, [128, 512], F32, "r")
        o1p = kb.pool(2, [128, 512], F32, "o1")
        o2p = kb.pool(2, [128, 512], F32, "o2")
        sqp = kb.pool(2, [128, 512], F32, "sqo")
        oTp = kb.pool(2, [128, 512], BF16, "oT")
        groups = [(g0, 512, list(range(NT))) for g0 in range(0, N, 512)] + [(N, NCTX, [NLT, NLT + 1])]
        for h in range(8):
            kT = kTp.next()
            kb.dma("sp", kT.v(), s.QKT[8 + h, :, :])
            Vh = Vp.next()
            kb.dma("sp", Vh.v(), s.H[:, h * 128:(h + 1) * 128].rearrange("(j p) e -> p j e", p=128))
            for (g0, nq, kts) in groups:
                qT = qTp.next()
                kb.dma("sp", qT[:, 0:nq], s.QKT[h, :, g0:g0 + nq])
                for ki, kt in enumerate(kts):
                    for m in range(2):
                        ps = psS.next()
                        kb.mm(ps[:, 0:nq], kT[64 * m:64 * m + 64, kt * 128:(kt + 1) * 128], qT[64 * m:64 * m + 64, 0:nq])
                        PT = PTp.next()
                        kb.act(PT[:, 0:nq], ps[:, 0:nq], AF.Exp)
                        kb.mm(psO[m][:, 0:nq], Vh[:, kt, :], PT[:, 0:nq], start=(ki == 0), stop=(ki == len(kts) - 1))
                        kb.mm(psD[m][:, 0:nq], ones_b.v(), PT[:, 0:nq], start=(ki == 0), stop=(ki == len(kts) - 1))
                o1 = o1p.next()
                o2 = o2p.next()
                for m, o in ((0, o1), (1, o2)):
                    r = rp.next()
                    kb.recip(r[:, 0:nq], psD[m][:, 0:nq])
                    kb.tt(o[:, 0:nq], psO[m][:, 0:nq], r[:, 0:nq], ALU.mult)
                kb.stt(o1[:, 0:nq], o2[:, 0:nq], lam[:, 3:4], o1[:, 0:nq], ALU.mult, ALU.add)
                sqo = sqp.next()
                kb.tt(sqo[:, 0:nq], o1[:, 0:nq], o1[:, 0:nq], ALU.mult)
                pn = psN.next()
                kb.mm(pn[:, 0:nq], ones_f.v(), sqo[:, 0:nq])
                r = rp.next()
                kb.act(r[:, 0:nq], pn[:, 0:nq], AF.Sqrt, bias=s.epst[:, 0:1], scale=1.0 / 128)
                kb.recip(r[:, 0:nq], r[:, 0:nq])
                oT = oTp.next()
                kb.stt(oT[:, 0:nq], o1[:, 0:nq], gsub[:, 0:1], r[:, 0:nq], ALU.mult, ALU.mult)
                kb.dma("pool", s.QKT[16 + h, :, g0:g0 + nq], oT[:, 0:nq])
        kb.pop()
        kb.push()
        stage = kb.pool(2, [128, 8, 512], F32, "stage")
        w_out = kb.tile([128, 8, D], BF16, "w_out")
        s.load_w_bf16(w_out, s.c_w_out[j], D, stage)
        oTp = kb.pool(2, [128, 8, 128], BF16, "oT")
        psY = kb.pool(2, [128, 512], F32, "psY", space="psum")
        xp = kb.pool(2, [128, D], F32, "x")
        yt = kb.tile([128, D], F32, "yt")
        for ti in range(NT):
            t0 = ti * 128
            w = 0 if ti < NLT else 1
            oT = oTp.next()
            kb.dma("sp", oT.v(), s.QKT[16:24, :, t0:t0 + 128].rearrange("c p t -> p c t"))
            s.out_proj_res(ti, oT, w_out, mod[(w, 2)], psY, xp, yt)
        kb.pop()
        kb.pop()

    def layer_delta(s, l, j):
        kb = s.kb
        N, T, NT, NLT = s.N, s.T, s.NT, s.NLT
        kb.push()
        mod = s.adaln(l, [0, 1, 2], s.g_mix[l:l + 1, :])
        ab_all = kb.tile([128, NT, 32], F32, "ab_all")
        gb_all = kb.tile([128, NT, 32], F32, "gb_all")
        kb.push()
        stage = kb.pool(2, [128, 8, 512], F32, "stage")
        w_in = kb.tile([128, 8, 4128], BF16, "w_in")
        s.load_w_bf16(w_in, s.a_w_in[j], 4128, stage)
        xp = kb.pool(2, [128, D], F32, "x")
        junk = kb.tile([128, D], F32, "junk")
        tmpf = kb.tile([128, D], F32, "tmpf")
        ssp = kb.pool(2, [128, 2], F32, "ss")
        hbp = kb.pool(2, [128, D], BF16, "hb")
        hTp = kb.pool(2, [128, 8, 128], BF16, "hT")
        psT = kb.pool(2, [128, 512], F32, "psT", space="psum")
        psP = kb.pool(4, [128, 512], F32, "psP", space="psum")
        pap = kb.pool(2, [128, 3072], BF16, "pa")
        zbp = kb.pool(2, [128, D], BF16, "zb")
        chunks = [(n0, min(512, 4128 - n0)) for n0 in range(0, 4128, 512)]
        for ti in range(NT):
            w = 0 if ti < NLT else 1
            t0 = ti * 128
            xt = xp.next()
            kb.dma("sp", xt.v(), s.X[t0:t0 + 128, :])
            hb = hbp.next()
            s.norm_mod(xt, mod[(w, 1)], mod[(w, 0)], hb, tmpf, junk, ssp.next())
            hT = hTp.next()
            s.transpose_chunks(hT.v(), hb.v(), 8, psT, s.ident_b)
            pa = pap.next()
            zb = zbp.next()
            for ci, (n0, nsz) in enumerate(chunks):
                ps = psP.next()
                for k in range(8):
                    kb.mm(ps[:, 0:nsz], hT[:, k, :], w_in[:, k, n0:n0 + nsz], start=(k == 0), stop=(k == 7))
                eng = "act" if ci % 2 == 0 else "dve"
                if n0 < 3072:
                    kb.copy(pa[:, n0:n0 + nsz], ps[:, 0:nsz], eng=eng)
                elif n0 < 4096:
                    kb.copy(zb[:, n0 - 3072:n0 - 3072 + nsz], ps[:, 0:nsz], eng=eng)
                else:
                    kb.copy(ab_all[:, ti, :], ps[:, 0:32], eng=eng)
            kb.dma("pool", s.PA[t0:t0 + 128, :], pa.v())
            kb.dma("pool", s.H[t0:t0 + 128, :], zb.v())
        kb.pop()
        if getattr(s, "dbg_stop", 9) <= 1:
            kb.pop(); return
        kb.push()
        wc = []
        for k in range(5):
            t = kb.tile([128, 3072], F32, "wc%d" % k)
            kb.dma("sp", t.v(), s.a_conv[j, k:k + 1, :].to_broadcast([128, 3072]))
            wc.append(t)
        nA = kb.tile([128, 16], F32, "nA")
        kb.dma("sp", nA.v(), s.a_log[j:j + 1].rearrange("o d h -> o (d h)").to_broadcast([128, 16]))
        kb.act(nA.v(), nA.v(), AF.Exp)
        kb.ts(nA.v(), nA.v(), -1.0, None, ALU.mult)
        dtb = kb.tile([128, 16], F32, "dtb")
        kb.dma("sp", dtb.v(), s.a_dt_bias[j:j + 1].rearrange("o d h -> o (d h)").to_broadcast([128, 16]))
        onec = kb.tile([128, 1], F32, "onec")
        kb.memset(onec.v(), 1.0)
        shp = kb.pool(5, [128, 3072], BF16, "sh")
        acc = kb.tile([128, 3072], F32, "acc")
        tmp = kb.tile([128, 3072], F32, "tmp")
        qkv = kb.tile([128, 3072], F32, "qkv")
        sq = kb.tile([128, 2048], F32, "sq")
        ssh = kb.tile([128, 16], F32, "ssh")
        gt = kb.tile([128, 16], F32, "gt")
        qknp = kb.pool(1, [128, 2048], BF16, "qkn")
        kvsp = kb.pool(1, [128, 2048], F32, "kvs")
        qkTp = kb.pool(1, [128, 16, 128], BF16, "qkT")
        psT = kb.pool(2, [128, 512], F32, "psT", space="psum")
        for ti in range(NT):
            seg0, seg1 = (0, N) if ti < NLT else (N, T)
            t0 = ti * 128
            shs = []
            for k in range(5):
                r0 = t0 - 2 + k
                r1 = r0 + 128
                lo = max(r0, seg0)
                hi = min(r1, seg1)
                sh = shp.next()
                if lo > r0 or hi < r1:
                    kb.memset(sh.v(), 0.0)
                kb.dma("sp", sh[lo - r0:hi - r0, :], s.PA[lo:hi, :])
                shs.append(sh)
            kb.tt(acc.v(), shs[0].v(), wc[0].v(), ALU.mult)
            for k in range(1, 5):
                kb.tt(tmp.v(), shs[k].v(), wc[k].v(), ALU.mult, eng="pool")
                kb.tt(acc.v(), acc.v(), tmp.v(), ALU.add)
            kb.act(qkv.v(), acc.v(), AF.Silu)
            x3 = qkv[:, 0:2048].re("p (h d) -> p h d", d=128)
            kb.tt(sq.v().re("p (h d) -> p h d", d=128), x3, x3, ALU.mult)
            kb.red(ssh.v(), sq.v().re("p (h d) -> p h d", d=128), ALU.add)
            kb.act(ssh.v(), ssh.v(), AF.Sqrt, bias=s.epst[:, 0:1], scale=1.0)
            kb.recip(ssh.v(), ssh.v())
            kb.ts(ssh[:, 0:8], ssh[:, 0:8], 128.0 ** -0.5, None, ALU.mult)
            qkn = qknp.next()
            rb = ssh.v().re("p (h o) -> p h o", o=1).bc([128, 16, 128])
            kb.tt(qkn.v().re("p (h d) -> p h d", d=128), x3, rb, ALU.mult)
            kvs = kvsp.next()
            kb.tt(kvs[:, 0:1024].re("p (h d) -> p h d", d=128), qkv[:, 1024:2048].re("p (h d) -> p h d", d=128),
                  ssh[:, 8:16].re("p (h o) -> p h o", o=1).bc([128, 8, 128]), ALU.mult)
            kb.copy(kvs[:, 1024:2048], qkv[:, 2048:3072], eng="act")
            qkT = qkTp.next()
            s.transpose_chunks(qkT.v(), qkn.v(), 16, psT, s.ident_b)
            kb.dma("pool", s.QKT[0:16, :, t0:t0 + 128].rearrange("c p t -> p c t"), qkT.v())
            kb.dma("pool", s.KVS[t0:t0 + 128, :], kvs.v())
            kb.tt(gt.v(), ab_all[:, ti, 0:16], dtb.v(), ALU.add)
            kb.act(gt.v(), gt.v(), AF.Exp)
            kb.act(gt.v(), gt.v(), AF.Ln, bias=onec[:, 0:1])
            kb.tt(gb_all[:, ti, 0:16], gt.v(), nA.v(), ALU.mult)
            kb.act(gb_all[:, ti, 16:32], ab_all[:, ti, 16:32], AF.Sigmoid)
        kb.pop()
        if getattr(s, "dbg_stop", 9) <= 2:
            kb.pop(); return
        for d in (0, 1):
            if getattr(s, "dbg_stop", 9) <= 3 + d - 1 + 0 and d == 1:
                break
            kb.push()
            ones_f = kb.tile([128, 128], F32, "ones_f")
            kb.memset(ones_f.v(), 1.0)
            tril_i = kb.tile([128, 128], F32, "tril_i")
            triu_i = kb.tile([128, 128], F32, "triu_i")
            tril_s = kb.tile([128, 128], F32, "tril_s")
            triu_s = kb.tile([128, 128], F32, "triu_s")
            kb.dma("sp", tril_i.v(), s.k_tril)
            kb.dma("sp", triu_i.v(), s.k_triu)
            kb.tt(tril_s.v(), tril_i.v(), s.ident_f.v(), ALU.subtract)
            kb.tt(triu_s.v(), triu_i.v(), s.ident_f.v(), ALU.subtract)
            bd16 = kb.tile([128, 128], F32, "bd16")
            kb.dma("sp", bd16.v(), s.k_bd16)
            offm = []
            for li in range(3):
                t_ = kb.tile([128, 128], F32, "off%d" % li)
                kb.dma("sp", t_.v(), s.k_off[li])
                offm.append(t_)
            if d == 0:
                mA, mAT, mQK, cumM = tril_s, triu_s, triu_i, triu_i
                order = [NLT, NLT + 1] + list(range(NLT))
            else:
                mA, mAT, mQK, cumM = triu_s, tril_s, tril_i, tril_i
                order = [NLT + 1, NLT] + list(range(NLT - 1, -1, -1))
            S = kb.tile([128, 8, 128], F32, "S")
            kb.memset(S.v(), 0.0)
            psR = kb.pool(6, [128, 512], F32, "psR", space="psum")
            kvsp = kb.pool(1, [128, 2048], F32, "kvs")
            qkbp = kb.pool(2, [128, 16, 128], BF16, "qkb")
            qkfp = kb.pool(1, [128, 16, 128], F32, "qkf")
            sc = {nm: kb.pool(2, [128, 8], F32, nm) for nm in ("gcl2", "gam", "e_", "gamL", "bg", "lnb", "gcb", "nb", "tmp8")}
            gclp = kb.pool(2, [128, 16], F32, "gcl")
            m = {nm: kb.pool(6 if nm in ("P", "Q") else 3, [128, 128], F32, nm) for nm in
                 ("dg1", "dg2", "E1", "E2", "E3", "P", "Q", "X", "Xn", "PL", "QL", "I1", "I2", "Mqk", "bV", "bgK", "Kt", "nWT", "Vn", "o1s")}
            otp = kb.pool(2, [128, D], F32, "ot")
            if d == 1:
                stage = kb.pool(1, [128, 8, 512], F32, "stage")
                w_out = kb.tile([128, 8, D], BF16, "w_out")
                s.load_w_bf16(w_out, s.a_w_out[j], D, stage)
                gout = kb.tile([128, 128], F32, "gout")
                kb.dma("sp", gout.v(), s.a_g_out[j:j + 1, :].to_broadcast([128, 128]))
                ofp = kb.pool(1, [128, D], F32, "of")
                zp = kb.pool(2, [128, D], BF16, "z")
                zf = kb.tile([128, D], F32, "zf")
                sqo = kb.tile([128, D], F32, "sqo")
                rs8 = kb.pool(2, [128, 8], F32, "rs8")
                obp = kb.pool(2, [128, D], BF16, "ob")
                oTp = kb.pool(2, [128, 8, 128], BF16, "oT")
                psY = kb.pool(2, [128, 512], F32, "psY", space="psum")
                xp = kb.pool(2, [128, D], F32, "x")
                yt = kb.tile([128, D], F32, "yt")
            for c in order[:int(_os.environ.get('DBG_C', '999'))]:
                t0 = c * 128
                kvs = kvsp.next()
                kb.dma("sp", kvs.v(), s.KVS[t0:t0 + 128, :])
                qkb = qkbp.next()
                kb.dma("sp", qkb.v(), s.QKT[0:16, :, t0:t0 + 128].rearrange("c p t -> p c t"))
                qkf = qkfp.next()
                kb.copy(qkf.v(), qkb.v(), eng="act")
                g8 = gb_all[:, c, d * 8:(d + 1) * 8]
                b8 = gb_all[:, c, 16 + d * 8:16 + (d + 1) * 8]
                ps = psR.next()
                kb.mm(ps[:, 0:8], cumM.v(), g8)
                kb.mm(ps[:, 8:16], ones_f.v(), g8)
                gcl = gclp.next()
                kb.copy(gcl.v(), ps[:, 0:16])
                gc = gcl[:, 0:8]
                gl = gcl[:, 8:16]
                gam = sc["gam"].next(); e_ = sc["e_"].next(); gamL = sc["gamL"].next(); bg = sc["bg"].next()
                lnb = sc["lnb"].next(); gcb = sc["gcb"].next(); nb = sc["nb"].next(); tmp8 = sc["tmp8"].next()
                kb.act(gam.v(), gc, AF.Exp)
                kb.tt(tmp8.v(), gl, gc, ALU.subtract)
                kb.act(e_.v(), tmp8.v(), AF.Exp)
                kb.act(gamL.v(), gl, AF.Exp)
                kb.tt(bg.v(), b8, gam.v(), ALU.mult)
                kb.act(lnb.v(), b8, AF.Ln)
                kb.tt(gcb.v(), gc, lnb.v(), ALU.add)
                kb.ts(nb.v(), b8, -1.0, None, ALU.mult)
                ot = otp.next()
                if int(_os.environ.get("DBG_STEP", "9")) <= 0:
                    continue
                for h in range(int(_os.environ.get("DBG_H", "8"))):
                    Kh = kvs[:, h * 128:(h + 1) * 128]
                    Vh = kvs[:, 1024 + h * 128:1024 + (h + 1) * 128]
                    QT = qkf[:, h, :]
                    KT = qkf[:, 8 + h, :]
                    gch = gcl[:, h:h + 1]
                    pKK = psR.next()
                    kb.mm(pKK[:, 0:128], KT, KT)
                    kb.mm(pKK[:, 128:256], KT, QT)
                    dg1 = m["dg1"].next(); dg2 = m["dg2"].next()
                    kb.ts(dg1.v(), s.ident_f.v(), gch, None, ALU.mult)
                    kb.ts(dg2.v(), s.ident_f.v(), gcb[:, h:h + 1], None, ALU.mult)
                    pBC = psR.next()
                    kb.mm(pBC[:, 0:128], ones_f.v(), dg1.v())
                    kb.mm(pBC[:, 128:256], ones_f.v(), dg2.v())
                    E1 = m["E1"].next(); E2 = m["E2"].next(); E3 = m["E3"].next()
                    P = m["P"].next(); Q = m["Q"].next(); X = m["X"].next(); Mqk = m["Mqk"].next()
                    kb.ts(E1.v(), pBC[:, 0:128], gch, 0.0, ALU.subtract, ALU.max)
                    kb.act(E1.v(), E1.v(), AF.Exp, scale=-1.0)
                    kb.tt(E1.v(), E1.v(), pKK[:, 0:128], ALU.mult)
                    kb.stt(P.v(), E1.v(), nb[:, h:h + 1], mA.v(), ALU.mult, ALU.mult)
                    kb.ts(E2.v(), pBC[:, 128:256], gch, 0.0, ALU.subtract, ALU.min)
                    kb.act(E2.v(), E2.v(), AF.Exp)
                    kb.tt(E2.v(), E2.v(), pKK[:, 0:128], ALU.mult)
                    kb.stt(Q.v(), E2.v(), -1.0, mAT.v(), ALU.mult, ALU.mult)
                    kb.ts(E3.v(), pBC[:, 0:128], gch, 0.0, ALU.subtract, ALU.min)
                    kb.act(E3.v(), E3.v(), AF.Exp)
                    kb.tt(E3.v(), E3.v(), pKK[:, 128:256], ALU.mult)
                    kb.tt(Mqk.v(), E3.v(), mQK.v(), ALU.mult)
                    P0, Q0 = P, Q
                    Pb = m["P"].next(); Qb = m["Q"].next()
                    kb.tt(Pb.v(), P0.v(), bd16.v(), ALU.mult)
                    kb.tt(Qb.v(), Q0.v(), bd16.v(), ALU.mult)
                    Xt = m["X"].next(); Xn = m["Xn"].next()
                    kb.tt(Xt.v(), Qb.v(), s.ident_f.v(), ALU.add)
                    kb.tt(Xn.v(), Pb.v(), s.ident_f.v(), ALU.add)
                    Pk, Qk = Pb, Qb
                    for k in range(1, 4):
                        pp = psR.next()
                        kb.mm(pp[:, 0:128], Qk.v(), Pk.v())
                        kb.mm(pp[:, 128:256], Pk.v(), Qk.v())
                        Pn = m["P"].next(); Qn = m["Q"].next()
                        kb.copy(Pn.v(), pp[:, 0:128])
                        kb.copy(Qn.v(), pp[:, 128:256])
                        pa = psR.next()
                        kb.mm(pa[:, 0:128], Pn.v(), Xt.v())
                        kb.mm(pa[:, 128:256], Qn.v(), Xn.v())
                        Xt2 = m["X"].next(); Xn2 = m["Xn"].next()
                        kb.tt(Xt2.v(), Xt.v(), pa[:, 0:128], ALU.add)
                        kb.tt(Xn2.v(), Xn.v(), pa[:, 128:256], ALU.add)
                        Pk, Qk, Xt, Xn = Pn, Qn, Xt2, Xn2
                    for li in range(3):
                        last = li == 2
                        PL = m["PL"].next()
                        kb.tt(PL.v(), P0.v(), offm[li].v(), ALU.mult)
                        if not last:
                            QL = m["QL"].next()
                            kb.tt(QL.v(), Q0.v(), offm[li].v(), ALU.mult)
                        pi = psR.next()
                        kb.mm(pi[:, 0:128], PL.v(), Xt.v())
                        if not last:
                            kb.mm(pi[:, 128:256], QL.v(), Xn.v())
                        I1 = m["I1"].next()
                        kb.copy(I1.v(), pi[:, 0:128])
                        if not last:
                            I2 = m["I2"].next()
                            kb.copy(I2.v(), pi[:, 128:256])
                        po2 = psR.next()
                        kb.mm(po2[:, 0:128], Xn.v(), I1.v())
                        if not last:
                            kb.mm(po2[:, 128:256], Xt.v(), I2.v())
                        Xt2 = m["X"].next()
                        kb.tt(Xt2.v(), Xt.v(), po2[:, 0:128], ALU.add)
                        if not last:
                            Xn2 = m["Xn"].next()
                            kb.tt(Xn2.v(), Xn.v(), po2[:, 128:256], ALU.add)
                            Xn = Xn2
                        Xt = Xt2
                    X = Xt
                    bV = m["bV"].next(); bgK = m["bgK"].next(); Kt = m["Kt"].next()
                    kb.ts(bV.v(), Vh, b8[:, h:h + 1], None, ALU.mult)
                    kb.ts(bgK.v(), Kh, bg[:, h:h + 1], None, ALU.mult)
                    kb.ts(Kt.v(), Kh, e_[:, h:h + 1], None, ALU.mult)
                    pW = psR.next()
                    kb.mm(pW[:, 0:128], bgK.v(), X.v())
                    nWT = m["nWT"].next()
                    kb.ts(nWT.v(), pW[:, 0:128], -1.0, None, ALU.mult)
                    pV = psR.next()
                    kb.mm(pV[:, 0:128], X.v(), bV.v(), start=True, stop=False)
                    kb.mm(pV[:, 0:128], nWT.v(), S[:, h, :], start=False, stop=True)
                    Vn = m["Vn"].next()
                    kb.copy(Vn.v(), pV[:, 0:128], eng="act")
                    pO = psR.next()
                    kb.mm(pO[:, 0:128], QT, S[:, h, :])
                    kb.mm(pO[:, 128:256], Mqk.v(), Vn.v())
                    o1s = m["o1s"].next()
                    kb.ts(o1s.v(), pO[:, 0:128], gam[:, h:h + 1], None, ALU.mult)
                    kb.tt(ot[:, h * 128:(h + 1) * 128], o1s.v(), pO[:, 128:256], ALU.add)
                    pS = psR.next()
                    kb.mm(pS[:, 0:128], Kt.v(), Vn.v())
                    kb.stt(S[:, h, :], S[:, h, :], gamL[:, h:h + 1], pS[:, 0:128], ALU.mult, ALU.add)
                if d == 0:
                    kb.dma("pool", s.OF[t0:t0 + 128, :], ot.v())
                else:
                    w = 0 if c < NLT else 1
                    of = ofp.next()
                    kb.dma("sp", of.v(), s.OF[t0:t0 + 128, :])
                    kb.tt(ot.v(), ot.v(), of.v(), ALU.add)
                    o3 = ot.v().re("p (h d) -> p h d", d=128)
                    kb.tt(sqo.v(), ot.v(), ot.v(), ALU.mult)
                    r8 = rs8.next()
                    kb.red(r8.v(), sqo.v().re("p (h d) -> p h d", d=128), ALU.add)
                    kb.act(r8.v(), r8.v(), AF.Sqrt, bias=s.epst[:, 0:1], scale=1.0 / 128)
                    kb.recip(r8.v(), r8.v())
                    kb.tt(o3, o3, r8.v().re("p (h o) -> p h o", o=1).bc([128, 8, 128]), ALU.mult)
                    kb.tt(o3, o3, gout.v().re("p (o d) -> p o d", o=1).bc([128, 8, 128]), ALU.mult)
                    z = zp.next()
                    kb.dma("sp", z.v(), s.H[t0:t0 + 128, :])
                    kb.act(zf.v(), z.v(), AF.Silu)
                    ob = obp.next()
                    kb.tt(ob.v(), ot.v(), zf.v(), ALU.mult)
                    oT = oTp.next()
                    s.transpose_chunks(oT.v(), ob.v(), 8, psY, s.ident_b)
                    s.out_proj_res(c, oT, w_out, mod[(w, 2)], psY, xp, yt)
            kb.pop()
        kb.pop()

    def moe(s, l):
        kb = s.kb
        N, T, NT, NLT, S = s.N, s.T, s.NT, s.NLT, s.S
        kb.push()
        mod = s.adaln(l, [3, 4, 5], s.g_ffn[l:l + 1, :])
        affT = kb.tile([16, T], F32, "affT")
        slot_i = kb.tile([128, NT, 16], I32, "slot_i")
        gateT = kb.tile([128, NT, 16], F32, "gateT")
        kb.push()
        wr = kb.tile([128, 8, 16], F32, "wr")
        kb.dma("sp", wr.v(), s.w_router[l].rearrange("(k p) e -> p k e", p=128))
        xp = kb.pool(2, [128, D], F32, "x")
        junk = kb.tile([128, D], F32, "junk")
        tmpf = kb.tile([128, D], F32, "tmpf")
        ssp = kb.pool(2, [128, 2], F32, "ss")
        hfp = kb.pool(2, [128, D], F32, "hf")
        hbp = kb.pool(2, [128, D], BF16, "hb")
        hTf = kb.pool(2, [128, 8, 128], F32, "hTf")
        psT = kb.pool(4, [128, 512], F32, "psT", space="psum")
        psL = kb.pool(2, [128, 512], F32, "psL", space="psum")
        smp = kb.pool(2, [128, 4], F32, "sm")
        ep = kb.pool(2, [128, 16], F32, "e")
        for ti in range(NT):
            w = 0 if ti < NLT else 1
            t0 = ti * 128
            xt = xp.next()
            kb.dma("sp", xt.v(), s.X[t0:t0 + 128, :])
            hf = hfp.next()
            s.norm_mod(xt, mod[(w, 4)], mod[(w, 3)], hf, tmpf, junk, ssp.next())
            hb = hbp.next()
            kb.copy(hb.v(), hf.v(), eng="act")
            kb.dma("pool", s.H[t0:t0 + 128, :], hb.v())
            hT = hTf.next()
            for half in range(2):
                ps = psT.next()
                for i in range(4):
                    k = half * 4 + i
                    kb.tr(ps[:, i * 128:(i + 1) * 128], hf[:, k * 128:(k + 1) * 128], s.ident_f.v())
                kb.copy(hT[:, half * 4:half * 4 + 4, :], ps.v().re("p (k t) -> p k t", t=128), eng="act")
            pl = psL.next()
            for k in range(8):
                kb.mm(pl[:, 0:16], hT[:, k, :], wr[:, k, :], start=(k == 0), stop=(k == 7))
            sm = smp.next()
            e = ep.next()
            kb.red(sm[:, 0:1], pl[:, 0:16], ALU.max)
            kb.ts(sm[:, 1:2], sm[:, 0:1], -1.0, None, ALU.mult)
            kb.memset(sm[:, 2:3], 0.0)
            kb.act(e.v(), pl[:, 0:16], AF.Exp, bias=sm[:, 1:2], accum_out=sm[:, 2:3])
            kb.recip(sm[:, 3:4], sm[:, 2:3])
            kb.ts(e.v(), e.v(), sm[:, 3:4], None, ALU.mult)
            pt = psL.next()
            kb.tr(pt[0:16, 0:128], e.v(), s.ident_f.v())
            kb.copy(affT[:, t0:t0 + 128], pt[0:16, 0:128])
        kb.pop()
        kb.push()
        escal = kb.tile([16, 2], F32, "escal")
        kb.dma("sp", escal.v(), s.k_escal)
        Wmax = max(N, NCTX)
        work = kb.tile([16, Wmax], F32, "work")
        ones = kb.tile([16, 1], F32, "ones")
        kb.memset(ones.v(), 1.0)
        t8 = kb.tile([16, 8], F32, "t8")
        mask = kb.tile([16, T], F32, "mask")
        slotf = kb.tile([16, T], F32, "slotf")
        gsel = affT
        for (c0, c1, cap, ecol) in ((0, N, s.cap_lat, 0), (N, T, s.cap_ctx, 1)):
            n = c1 - c0
            src = affT[:, c0:c1]
            for r in range(cap // 8):
                kb.emit("dve", lambda E, src=src: E.max(out=t8.h[:], in_=src.ap), [src], [t8])
                kb.emit("dve", lambda E, src=src, n=n: E.match_replace(out=work.h[:, 0:n], in_to_replace=t8.h[:],
                                                                        in_values=src.ap, imm_value=0.0),
                        [src, t8], [work])
                src = work[:, 0:n]
            kb.ts(mask[:, c0:c1], work[:, 0:n], 0.0, None, ALU.is_equal)
            kb.emit("dve", lambda E, n=n, c0=c0, c1=c1: E.tensor_tensor_scan(
                out=slotf.h[:, c0:c1], data0=ones.h[:, 0:1].to_broadcast([16, n]), data1=mask.h[:, c0:c1], initial=0.0,
                op0=ALU.mult, op1=ALU.add), [ones, mask], [slotf])
            kb.stt(slotf[:, c0:c1], slotf[:, c0:c1], escal[:, ecol:ecol + 1], mask[:, c0:c1], ALU.add, ALU.mult)
            kb.ts(slotf[:, c0:c1], slotf[:, c0:c1], BIG, None, ALU.add)
        kb.tt(gsel.v(), affT.v(), mask.v(), ALU.mult)
        psS = kb.pool(2, [128, 512], F32, "psS", space="psum")
        slt = kb.tile([128, NT, 16], F32, "slt")
        for (srcT, dst) in ((slotf, slt), (gsel, gateT)):
            for g0 in range(0, NT, 32):
                g1 = min(NT, g0 + 32)
                ps = psS.next()
                for ti in range(g0, g1):
                    kb.tr(ps[:, (ti - g0) * 16:(ti - g0 + 1) * 16], srcT[:, ti * 128:(ti + 1) * 128], s.ident_f[0:16, 0:16])
                kb.copy(dst[:, g0:g1, :], ps[:, 0:(g1 - g0) * 16].re("p (t e) -> p t e", e=16))
        kb.copy(slot_i.v(), slt.v())
        kb.pop()
        kb.push()
        hbp = kb.pool(3, [128, D], BF16, "hb")
        for ti in range(NT):
            t0 = ti * 128
            hb = hbp.next()
            kb.dma("sp", hb.v(), s.H[t0:t0 + 128, :])
            for e in range(16):
                kb.dma("pool", s.XG, hb.v(), indirect=dict(idx=slot_i[:, ti, e:e + 1], side="out", bound=16 * S - 1))
        kb.pop()
        kb.push()
        stage = kb.pool(1, [128, 8, 512], F32, "stage")
        wgu = kb.tile([128, 8, 2 * D], BF16, "wgu")
        wd = kb.tile([128, 8, D], BF16, "wd")
        nst = (S + 127) // 128
        stiles = [(i * 128, min(128, S - i * 128)) for i in range(nst)]
        nchunks = [(n0, min(512, S - n0)) for n0 in range(0, S, 512)]
        xgp = kb.pool(1, [128, nst, D], BF16, "xg")
        xgT = kb.tile([128, 8, S], BF16, "xgT")
        actT = kb.tile([128, 8, S], BF16, "actT")
        psX = kb.pool(2, [128, 512], F32, "psX", space="psum")
        psG = kb.pool(2, [128, 512], F32, "psG", space="psum")
        psU = kb.pool(2, [128, 512], F32, "psU", space="psum")
        psD = kb.pool(2, [128, 512], F32, "psD", space="psum")
        sgp = kb.pool(2, [128, 512], F32, "sg")
        ysp = kb.pool(2, [128, D], BF16, "ys")
        for e in range(16):
            s.load_w_bf16(wgu, s.w_gate_up[l, e], 2 * D, stage)
            s.load_w_bf16(wd, s.w_down[l, e], D, stage)
            xg = xgp.next()
            for i, (s0, sz) in enumerate(stiles):
                kb.dma("sp", xg[0:sz, i, :], s.XG[e * S + s0:e * S + s0 + sz, :])
            for i, (s0, sz) in enumerate(stiles):
                ps = psX.next()
                psb = ps.v().bitcast(BF16)
                for k in range(8):
                    kb.tr(psb[:, k * 128:k * 128 + sz], xg[0:sz, i, k * 128:(k + 1) * 128], s.ident_b[0:sz, 0:sz])
                kb.copy(xgT[:, :, s0:s0 + sz], psb.re("p (k t) -> p k t", t=128)[:, :, 0:sz], eng="act")
            for fc in range(8):
                for (n0, nsz) in nchunks:
                    pg = psG.next()
                    pu = psU.next()
                    for k in range(8):
                        kb.mm(pg[:, 0:nsz], wgu[:, k, fc * 128:(fc + 1) * 128], xgT[:, k, n0:n0 + nsz],
                              start=(k == 0), stop=(k == 7))
                    for k in range(8):
                        kb.mm(pu[:, 0:nsz], wgu[:, k, D + fc * 128:D + (fc + 1) * 128], xgT[:, k, n0:n0 + nsz],
                              start=(k == 0), stop=(k == 7))
                    sg = sgp.next()
                    kb.act(sg[:, 0:nsz], pg[:, 0:nsz], AF.Silu)
                    kb.tt(actT[:, fc, n0:n0 + nsz], sg[:, 0:nsz], pu[:, 0:nsz], ALU.mult)
            for i, (s0, sz) in enumerate(stiles):
                ys = ysp.next()
                for nb in range(2):
                    pd = psD.next()
                    for fk in range(8):
                        kb.mm(pd[0:sz, :], actT[:, fk, s0:s0 + sz], wd[:, fk, nb * 512:(nb + 1) * 512],
                              start=(fk == 0), stop=(fk == 7))
                    kb.copy(ys[0:sz, nb * 512:(nb + 1) * 512], pd[0:sz, :], eng="act")
                kb.dma("pool", s.Y[e * S + s0:e * S + s0 + sz, :], ys[0:sz, :])
        kb.pop()
        kb.push()
        gp = kb.pool(4, [128, D], BF16, "gath")
        for b in gp.bufs:
            kb.memset(b.v(), 0.0)
        accp = kb.pool(2, [128, D], F32, "acc")
        xp = kb.pool(2, [128, D], F32, "x")
        for ti in range(NT):
            w = 0 if ti < NLT else 1
            t0 = ti * 128
            acc = accp.next()
            kb.memset(acc.v(), 0.0)
            for e in range(16):
                g = gp.next()
                kb.dma("pool", g.v(), s.Y, indirect=dict(idx=slot_i[:, ti, e:e + 1], side="in", bound=16 * S - 1))
                kb.stt(acc.v(), g.v(), gateT[:, ti, e:e + 1], acc.v(), ALU.mult, ALU.add)
            xt = xp.next()
            kb.dma("sp", xt.v(), s.X[t0:t0 + 128, :])
            kb.tt(acc.v(), acc.v(), mod[(w, 5)].v(), ALU.mult)
            kb.tt(xt.v(), xt.v(), acc.v(), ALU.add)
            kb.dma("pool", s.X[t0:t0 + 128, :], xt.v())
        kb.pop()
        kb.pop()

    def finish(s):
        kb = s.kb
        for r0 in range(0, s.T, 1024):
            r1 = min(s.T, r0 + 1024)
            kb.dma("sp", s.out[r0:r1, :], s.X[r0:r1, :])
        kb.barrier()


def host_consts(N):
    T = N + NCTX
    S = 2 * N // 16 + 2 * NCTX // 16
    GRID_W = 64
    rows = N // GRID_W
    t_row = np.repeat(np.arange(rows), GRID_W).astype(np.float32)
    t_col = np.tile(np.arange(GRID_W), rows).astype(np.float32)
    n_freq = 16
    inv = (10000.0 ** (-np.arange(n_freq, dtype=np.float32) / n_freq)).astype(np.float32)
    ang = np.concatenate([t_row[:, None] * inv, t_col[:, None] * inv], -1).astype(np.float32)
    cos = np.ones((T, 32), np.float32)
    sin = np.zeros((T, 32), np.float32)
    cos[:N] = np.cos(ang)
    sin[:N] = np.sin(ang)
    a = np.arange(128)
    tril = (a[None, :] <= a[:, None]).astype(np.float32)
    triu = (a[:, None] <= a[None, :]).astype(np.float32)
    e = np.arange(16, dtype=np.float32)
    escal = np.stack([e * S - 1 - BIG, e * S - 1 - BIG + 2 * N // 16], 1).astype(np.float32)
    ii = a[:, None]
    jj = a[None, :]
    bd16 = ((ii // 16) == (jj // 16)).astype(np.float32)
    offs = []
    for b in (16, 32, 64):
        same2b = (ii // (2 * b)) == (jj // (2 * b))
        diffb = (ii // b) != (jj // b)
        offs.append((same2b & diffb).astype(np.float32))
    return dict(k_bd16=bd16, k_off=np.stack(offs, 0), k_ident=np.eye(128, dtype=np.float32), k_cos=cos, k_sin=sin, k_tril=tril, k_triu=triu, k_escal=escal)


from concourse.bass_utils import run_bass_kernel_spmd

N_FULL = 8192


def kernel(**inputs):
    N = N_FULL
    mk = MK(N)
    mk.setup()
    for l in range(4):
        kind, j = l % 3, l // 3
        getattr(mk, ["layer_delta", "layer_swa", "layer_diff"][kind])(l, j)
        mk.moe(l)
    mk.finish()
    consts = host_consts(N)
    in_maps = []
    B = inputs["x"].shape[0]
    for b in range(B):
        im = {}
        for k in mk.inp:
            if k.startswith("k_"):
                im[k] = consts[k]
            elif k in ("x", "ctx", "c"):
                im[k] = np.ascontiguousarray(np.asarray(inputs[k])[b])
            else:
                im[k] = np.ascontiguousarray(np.asarray(inputs[k]))
        in_maps.append(im)
    res = run_bass_kernel_spmd(mk.nc, in_maps, core_ids=list(range(B)))
    out = np.stack([np.asarray(r["out"])[:N] for r in res.results], 0).astype(np.float32)
    return out
```

```python
import numpy as np
from contextlib import ExitStack
import concourse.bass as bass
import concourse.mybir as mybir

F32 = mybir.dt.float32
BF16 = mybir.dt.bfloat16
I32 = mybir.dt.int32
AF = mybir.ActivationFunctionType
ALU = mybir.AluOpType
AX = mybir.AxisListType

SELF_SYNC = True
FOLD_WAITS = True


class V:
    __slots__ = ("buf", "ap")

    def __init__(s, buf, ap):
        s.buf = buf
        s.ap = ap

    def __getitem__(s, k):
        return V(s.buf, s.ap[k])

    def re(s, pat, **kw):
        return V(s.buf, s.ap.rearrange(pat, **kw))

    def bc(s, shape):
        return V(s.buf, s.ap.to_broadcast(shape))

    def bitcast(s, dt):
        return V(s.buf, s.ap.bitcast(dt))


class Buf:
    def __init__(s, kb, handle, name):
        s.kb = kb
        s.h = handle
        s.name = name
        s.w = None
        s.r = {}
        s.sem = None
        s.semcnt = 0

    def __getitem__(s, k):
        return V(s, s.h[k])

    def v(s):
        return V(s, s.h[:])


class Pool:
    def __init__(s, bufs):
        s.bufs = bufs
        s.i = 0

    def next(s):
        b = s.bufs[s.i % len(s.bufs)]
        s.i += 1
        return b


def _ap(x):
    return x.ap if isinstance(x, V) else x


class KB:
    def __init__(s, nc):
        s.nc = nc
        s.E = {"pe": nc.tensor, "act": nc.scalar, "dve": nc.vector, "pool": nc.gpsimd, "sp": nc.sync}
        s.sem = {}
        s.cnt = {}
        s.waited = {e: {} for e in s.E}
        s.semh = {}
        for e in s.E:
            h = nc.alloc_semaphore("sem_" + e)
            s.sem[e] = h
            s.semh[e] = h
            s.cnt[e] = 0
        s.bar = nc.alloc_semaphore("sem_bar")
        s.barcnt = 0
        s.nbuf = 0
        s.dmabufs = []
        s.stack = [ExitStack()]
        s.phase_bufs = [[]]
        s.free_sems = []
        s.bar_t = s.tile([128, 8], F32, name="bar_t")
        s.n_ins = 0

    def tile(s, shape, dtype, name=None, space="sbuf"):
        s.nbuf += 1
        name = (name or "t") + "_%d" % s.nbuf
        if space == "sbuf":
            h = s.stack[-1].enter_context(s.nc.sbuf_tensor(name, list(shape), dtype))
        else:
            h = s.stack[-1].enter_context(s.nc.psum_tensor(name, list(shape), dtype))
        b = Buf(s, h, name)
        s.phase_bufs[-1].append(b)
        return b

    def pool(s, n, shape, dtype, name=None, space="sbuf"):
        return Pool([s.tile(shape, dtype, name=name, space=space) for _ in range(n)])

    def push(s):
        s.stack.append(ExitStack())
        s.phase_bufs.append([])

    def pop(s):
        s.barrier()
        for b in s.phase_bufs.pop():
            if b.sem is not None:
                s.free_sems.append((b.sem, b.semcnt, ("d", b.name)))
                b.sem = None
        s.stack.pop().close()

    def _getsem(s, b):
        if b.sem is None:
            if s.free_sems:
                h, c, oldkey = s.free_sems.pop()
                b.sem = h
                b.semcnt = c
                for e in s.E:
                    s.waited[e][("d", b.name)] = c
            else:
                b.sem = s.nc.alloc_semaphore("sd_" + b.name)
                b.semcnt = 0
            s.semh[("d", b.name)] = b.sem
            s.dmabufs.append(b)
        return b.sem

    def _wait(s, eng, ev):
        key, val = ev
        if s.waited[eng].get(key, 0) >= val:
            return
        if key == eng and (eng == "pe" or eng == "sp" or not SELF_SYNC):
            return
        s.E[eng].wait_ge(s.semh[key], val)
        s.waited[eng][key] = val

    def _need(s, eng, ev, lst):
        key, val = ev
        if s.waited[eng].get(key, 0) >= val:
            return
        if key == eng and (eng == "pe" or eng == "sp" or not SELF_SYNC):
            return
        for i, (k2, v2) in enumerate(lst):
            if k2 == key:
                if v2 < val:
                    lst[i] = (key, val)
                return
        lst.append((key, val))

    def _deps(s, eng, reads, writes, acc=False):
        lst = []
        for b in reads:
            if b.w is not None:
                s._need(eng, b.w, lst)
        for b in writes:
            if b.w is not None:
                if not (acc and b.w[0] == eng):
                    s._need(eng, b.w, lst)
            for k, v in b.r.items():
                s._need(eng, (k, v), lst)
        for ev in lst[:-1]:
            s._wait(eng, ev)
        return lst[-1] if lst else None

    def _fold(s, eng, ins, ev):
        if ev is None:
            return
        key, val = ev
        if FOLD_WAITS:
            ins._wait_ge(s.semh[key], val)
            s.waited[eng][key] = val
        else:
            raise RuntimeError("fold disabled")

    def emit(s, eng, fn, reads, writes, acc=False):
        reads = [x.buf if isinstance(x, V) else x for x in reads if isinstance(x, (V, Buf))]
        writes = [x.buf if isinstance(x, V) else x for x in writes if isinstance(x, (V, Buf))]
        last = s._deps(eng, reads, writes, acc)
        if last is not None and not FOLD_WAITS:
            s._wait(eng, last)
            last = None
        ins = fn(s.E[eng])
        s._fold(eng, ins, last)
        s.cnt[eng] += 1
        ins.then_inc(s.sem[eng], 1)
        ev = (eng, s.cnt[eng])
        for b in reads:
            b.r[eng] = s.cnt[eng]
        for b in writes:
            b.w = ev
            b.r = {}
        s.n_ins += 1
        return ins

    def dma(s, q, out, in_, indirect=None, **kw):
        obuf = out.buf if isinstance(out, V) else None
        ibuf = in_.buf if isinstance(in_, V) else None
        extra = []
        if indirect is not None:
            extra = [indirect["idx"].buf]
        carrier = obuf or ibuf or s.bar_t
        sem = s._getsem(carrier)
        reads = [b for b in [ibuf] + extra if b is not None]
        writes = [b for b in [obuf] if b is not None]
        last = s._deps(q, reads, writes)
        if last is not None and (not FOLD_WAITS or indirect is not None):
            s._wait(q, last)
            last = None
        E = s.E[q]
        if indirect is None:
            ins = E.dma_start(out=_ap(out), in_=_ap(in_), **kw)
        else:
            off = bass.IndirectOffsetOnAxis(ap=indirect["idx"].ap, axis=0)
            if not hasattr(s, "_bregs"):
                s._bregs = {}
            if indirect["bound"] not in s._bregs:
                s._bregs[indirect["bound"]] = E.to_reg(indirect["bound"])
            breg = s._bregs[indirect["bound"]]
            if indirect["side"] == "out":
                ins = E.indirect_dma_start(out=_ap(out), out_offset=off, in_=_ap(in_), in_offset=None,
                                           bounds_check=breg, oob_is_err=False)
            else:
                ins = E.indirect_dma_start(out=_ap(out), out_offset=None, in_=_ap(in_), in_offset=off,
                                           bounds_check=breg, oob_is_err=False)
        s._fold(q, ins, last)
        carrier.semcnt += 16
        ins.then_inc(sem, 16)
        key = ("d", carrier.name)
        ev = (key, carrier.semcnt)
        for b in reads:
            b.r[key] = carrier.semcnt
        for b in writes:
            b.w = ev
            b.r = {}
        s.n_ins += 1
        return ins

    def barrier(s):
        for e in s.E:
            if e != "pool" and s.cnt[e] > 0:
                s._wait("pool", (e, s.cnt[e]))
        if s.cnt["pool"] > 0:
            key, val = "pool", s.cnt["pool"]
            if s.waited["pool"].get(key, 0) < val:
                s.E["pool"].wait_ge(s.sem["pool"], val)
                s.waited["pool"][key] = val
        for b in s.dmabufs:
            if b.sem is not None and b.semcnt > 0:
                s._wait("pool", (("d", b.name), b.semcnt))
        s.barcnt += 1
        s.E["pool"].memset(s.bar_t.h[:, 0:1], 0.0).then_inc(s.bar, 1)
        for e in s.E:
            if e != "pool":
                s.E[e].wait_ge(s.bar, s.barcnt)
        s.E["pool"].wait_ge(s.bar, s.barcnt)
        for e in s.E:
            for e2 in s.E:
                s.waited[e][e2] = s.cnt[e2]
            for b in s.dmabufs:
                if b.sem is not None:
                    s.waited[e][("d", b.name)] = b.semcnt
        for lst in s.phase_bufs:
            for b in lst:
                b.w = None
                b.r = {}
        s.dmabufs = [b for b in s.dmabufs if b.sem is not None]

    def mm(s, out, lhsT, rhs, start=True, stop=True):
        return s.emit("pe", lambda E: E.matmul(_ap(out), _ap(lhsT), _ap(rhs), start=start, stop=stop),
                      [lhsT, rhs], [out], acc=not start)

    def tr(s, out, in_, ident):
        return s.emit("pe", lambda E: E.transpose(_ap(out), _ap(in_), _ap(ident)), [in_, ident], [out])

    def act(s, out, in_, func, bias=None, scale=None, accum_out=None, eng="act"):
        kw = {}
        rd = [in_]
        wr = [out]
        if bias is not None:
            kw["bias"] = _ap(bias)
            rd.append(bias)
        if scale is not None:
            kw["scale"] = _ap(scale)
            rd.append(scale)
        if accum_out is not None:
            kw["accum_out"] = _ap(accum_out)
            wr.append(accum_out)
        return s.emit(eng, lambda E: E.activation(out=_ap(out), in_=_ap(in_), func=func, **kw), rd, wr)

    def tt(s, out, a, b, op, eng="dve"):
        return s.emit(eng, lambda E: E.tensor_tensor(out=_ap(out), in0=_ap(a), in1=_ap(b), op=op), [a, b], [out])

    def ts(s, out, a, s1, s2, op0, op1=None, eng="dve"):
        kw = {}
        if op1 is not None:
            kw["op1"] = op1
        return s.emit(eng, lambda E: E.tensor_scalar(out=_ap(out), in0=_ap(a), scalar1=_ap(s1), scalar2=_ap(s2),
                                                     op0=op0, **kw), [a, s1, s2], [out])

    def stt(s, out, in0, scalar, in1, op0, op1, eng="dve"):
        return s.emit(eng, lambda E: E.scalar_tensor_tensor(out=_ap(out), in0=_ap(in0), scalar=_ap(scalar),
                                                            in1=_ap(in1), op0=op0, op1=op1),
                      [in0, scalar, in1], [out])

    def copy(s, out, in_, eng="dve"):
        if eng == "act":
            return s.emit(eng, lambda E: E.copy(out=_ap(out), in_=_ap(in_)), [in_], [out])
        return s.emit(eng, lambda E: E.tensor_copy(out=_ap(out), in_=_ap(in_)), [in_], [out])

    def red(s, out, in_, op, axis=AX.X, eng="dve"):
        return s.emit(eng, lambda E: E.tensor_reduce(out=_ap(out), in_=_ap(in_), axis=axis, op=op), [in_], [out])

    def memset(s, out, val, eng="dve"):
        return s.emit(eng, lambda E: E.memset(_ap(out), val), [], [out])

    def recip(s, out, in_):
        return s.emit("dve", lambda E: E.reciprocal(out=_ap(out), in_=_ap(in_)), [in_], [out])


import os as _os

D = 1024
NCTX = 256
EPS = 1e-6
BIG = 32768.0


class MK:
    def __init__(s, N):
        s.N = N
        s.T = N + NCTX
        s.NT = s.T // 128
        s.NLT = N // 128
        s.cap_lat = 2 * N // 16
        s.cap_ctx = 2 * NCTX // 16
        s.S = s.cap_lat + s.cap_ctx
        nc = bass.Bass("TRN2", target_bir_lowering=False)
        s.nc = nc
        s.inp = {}
        s.kb = KB(nc)

    def din(s, name, shape, dt=F32):
        t = s.nc.dram_tensor(name, list(shape), dt, kind="ExternalInput").ap()
        s.inp[name] = t
        return t

    def dscr(s, name, shape, dt):
        return s.nc.dram_tensor(name, list(shape), dt, kind="Internal").ap()

    def setup(s):
        kb = s.kb
        N, T = s.N, s.T
        s.x_in = s.din("x", [N, D])
        s.ctx_in = s.din("ctx", [NCTX, D])
        s.c_in = s.din("c", [D])
        s.cc_in = s.din("c_ctx", [D])
        s.w_ada = s.din("w_ada", [4, D, 6 * D])
        s.b_ada = s.din("b_ada", [4, 6 * D])
        s.g_mix = s.din("g_mix", [4, D])
        s.g_ffn = s.din("g_ffn", [4, D])
        s.a_w_in = s.din("a_w_in", [2, D, 4128])
        s.a_conv = s.din("a_conv", [2, 5, 3072])
        s.a_log = s.din("a_log", [2, 2, 8])
        s.a_dt_bias = s.din("a_dt_bias", [2, 2, 8])
        s.a_g_out = s.din("a_g_out", [2, 128])
        s.a_w_out = s.din("a_w_out", [2, D, D])
        s.b_w_in = s.din("b_w_in", [1, D, 1536])
        s.b_q_norm = s.din("b_q_norm", [1, 64])
        s.b_k_norm = s.din("b_k_norm", [1, 64])
        s.b_sink = s.din("b_sink", [1, 16])
        s.b_w_out = s.din("b_w_out", [1, D, D])
        s.c_w_in = s.din("c_w_in", [1, D, 3072])
        s.c_q_norm = s.din("c_q_norm", [1, 64])
        s.c_k_norm = s.din("c_k_norm", [1, 64])
        s.c_lambda = s.din("c_lambda", [1, 4, 64])
        s.c_g_sub = s.din("c_g_sub", [1, 128])
        s.c_w_out = s.din("c_w_out", [1, D, D])
        s.w_router = s.din("w_router", [4, D, 16])
        s.w_gate_up = s.din("w_gate_up", [4, 16, D, 2 * D])
        s.w_down = s.din("w_down", [4, 16, D, D])
        s.k_ident = s.din("k_ident", [128, 128])
        s.k_cos = s.din("k_cos", [T, 32])
        s.k_sin = s.din("k_sin", [T, 32])
        s.k_tril = s.din("k_tril", [128, 128])
        s.k_triu = s.din("k_triu", [128, 128])
        s.k_bd16 = s.din("k_bd16", [128, 128])
        s.k_off = s.din("k_off", [3, 128, 128])
        s.k_escal = s.din("k_escal", [16, 2])
        s.out = s.nc.dram_tensor("out", [T, D], F32, kind="ExternalOutput").ap()
        s.X = s.dscr("X", [T, D], F32)
        s.H = s.dscr("H", [T, D], BF16)
        s.XG = s.dscr("XG", [16 * s.S, D], BF16)
        s.Y = s.dscr("Y", [16 * s.S, D], BF16)
        s.QKT = s.dscr("QKT", [24, 128, T], BF16)
        s.VA = s.dscr("VA", [T, 4 * 65], BF16)
        s.PA = s.dscr("PA", [T, 3072], BF16)
        s.KVS = s.dscr("KVS", [T, 2048], F32)
        s.OF = s.dscr("OF", [T, D], F32)
        s.ident_f = kb.tile([128, 128], F32, "ident_f")
        s.ident_b = kb.tile([128, 128], BF16, "ident_b")
        kb.dma("sp", s.ident_f.v(), s.k_ident)
        kb.copy(s.ident_b.v(), s.ident_f.v())
        s.epst = kb.tile([128, 1], F32, "eps")
        kb.memset(s.epst.v(), EPS)
        s.scb = []
        for i, cin in enumerate((s.c_in, s.cc_in)):
            ct = kb.tile([128, 8], F32, "c%d" % i)
            kb.dma("sp", ct.v(), cin.rearrange("(k p) -> p k", p=128), allow_slow_non_contiguous=True)
            kb.act(ct.v(), ct.v(), AF.Silu)
            sb = kb.tile([128, 8, 128], F32, "scb%d" % i)
            kb.copy(sb.v(), ct.v().re("p (k o) -> p k o", o=1).bc([128, 8, 128]))
            s.scb.append(sb)
        for r0 in range(0, N, 1024):
            kb.dma("sp", s.X[r0:r0 + 1024, :], s.x_in[r0:r0 + 1024, :])
        kb.dma("sp", s.X[N:T, :], s.ctx_in)
        kb.barrier()

    def adaln(s, l, js, g_ap):
        kb = s.kb
        res = {}
        for w in (0, 1):
            for j in js:
                res[(w, j)] = kb.tile([128, D], F32, "mod%d_%d" % (w, j))
        kb.push()
        wpool = kb.pool(2, [128, 8, 512], F32, "wada")
        bpool = kb.pool(2, [128, 512], F32, "bada")
        pspool = kb.pool(2, [128, 512], F32, "ps_ada", space="psum")
        for j in js:
            for half in (0, 1):
                n0 = j * D + half * 512
                wt = wpool.next()
                kb.dma("sp", wt.v(), s.w_ada[l, :, n0:n0 + 512].rearrange("(k p) n -> p k n", p=128))
                bt = bpool.next()
                kb.dma("sp", bt.v(), s.b_ada[l:l + 1, n0:n0 + 512].to_broadcast([128, 512]))
                for w in (0, 1):
                    ps = pspool.next()
                    for k in range(8):
                        kb.mm(ps.v(), s.scb[w][:, k, :], wt[:, k, :], start=(k == 0), stop=(k == 7))
                    kb.tt(res[(w, j)][:, half * 512:(half + 1) * 512], ps.v(), bt.v(), ALU.add)
        gt = kb.tile([128, D], F32, "gt")
        kb.dma("sp", gt.v(), g_ap.to_broadcast([128, D]))
        for w in (0, 1):
            t = res[(w, js[1])]
            kb.stt(t.v(), t.v(), 1.0, gt.v(), ALU.add, ALU.mult)
        kb.pop()
        return res

    def norm_mod(s, xt, gs, sh, out, tmp_f, junk, ss):
        kb = s.kb
        kb.memset(ss.v(), 0.0)
        kb.act(junk.v(), xt.v(), AF.Square, accum_out=ss[:, 0:1])
        kb.act(ss[:, 1:2], ss[:, 0:1], AF.Sqrt, bias=s.epst[:, 0:1], scale=1.0 / D)
        kb.recip(ss[:, 1:2], ss[:, 1:2])
        kb.stt(tmp_f.v(), xt.v(), ss[:, 1:2], gs.v(), ALU.mult, ALU.mult)
        kb.tt(out.v(), tmp_f.v(), sh.v(), ALU.add)

    def transpose_chunks(s, dst, src, n, pspool, ident, evac="act"):
        kb = s.kb
        c = 0
        while c < n:
            m = min(8, n - c)
            ps = pspool.next()
            psb = ps.v().bitcast(BF16)
            for i in range(m):
                kb.tr(psb[:, i * 128:(i + 1) * 128], src[:, (c + i) * 128:(c + i + 1) * 128], ident.v())
            kb.copy(dst[:, c:c + m, :], psb[:, 0:m * 128].re("p (k t) -> p k t", t=128), eng=evac)
            c += m

    def load_w_bf16(s, dst, src_ap, ncols, stage_pool, chunk=512, engs=("dve", "act")):
        kb = s.kb
        i = 0
        for n0 in range(0, ncols, chunk):
            nsz = min(chunk, ncols - n0)
            st = stage_pool.next()
            kb.dma("sp", st[:, :, 0:nsz], src_ap[:, n0:n0 + nsz].rearrange("(k p) n -> p k n", p=128))
            kb.copy(dst[:, :, n0:n0 + nsz], st[:, :, 0:nsz], eng=engs[i % len(engs)])
            i += 1

    def qk_post(s, pqs, nh, G, cs, sn, qn, sq, ssh, t1, t2):
        kb = s.kb
        x3 = pqs.re("p (h d) -> p h d", d=64)
        kb.tt(sq.v().re("p (h d) -> p h d", d=64)[:, 0:nh, :], x3, x3, ALU.mult)
        kb.red(ssh[:, 0:nh], sq.v().re("p (h d) -> p h d", d=64)[:, 0:nh, :], ALU.add)
        kb.act(ssh[:, 0:nh], ssh[:, 0:nh], AF.Sqrt, bias=s.epst[:, 0:1], scale=1.0 / 64)
        kb.recip(ssh[:, 0:nh], ssh[:, 0:nh])
        q3 = qn.v().re("p (h d) -> p h d", d=64)[:, 0:nh, :]
        kb.tt(q3, x3, ssh[:, 0:nh].re("p (h o) -> p h o", o=1).bc([128, nh, 64]), ALU.mult)
        kb.tt(q3, q3, G.v().re("p (h d) -> p h d", d=64)[:, 0:nh, :], ALU.mult)
        x1 = q3[:, :, 0:32]
        x2 = q3[:, :, 32:64]
        cb = cs.v().re("p (o d) -> p o d", o=1).bc([128, nh, 32])
        sb = sn.v().re("p (o d) -> p o d", o=1).bc([128, nh, 32])
        t13 = t1.v().re("p (h d) -> p h d", d=64)[:, 0:nh, :]
        t23 = t2.v().re("p (h d) -> p h d", d=64)[:, 0:nh, :]
        kb.tt(t13[:, :, 0:32], x1, cb, ALU.mult)
        kb.tt(t13[:, :, 32:64], x2, cb, ALU.mult)
        kb.tt(t23[:, :, 0:32], x2, sb, ALU.mult)
        kb.tt(t23[:, :, 32:64], x1, sb, ALU.mult)
        return t13, t23

    def layer_swa(s, l, j):
        kb = s.kb
        N, T, NT, NLT = s.N, s.T, s.NT, s.NLT
        kb.push()
        mod = s.adaln(l, [0, 1, 2], s.g_mix[l:l + 1, :])
        kb.push()
        stage = kb.pool(2, [128, 8, 512], F32, "stage")
        w_in = kb.tile([128, 8, 1536], BF16, "w_in")
        s.load_w_bf16(w_in, s.b_w_in[j], 1536, stage)
        G = kb.tile([128, 20 * 64], F32, "G")
        kb.dma("sp", G[:, 0:64], s.b_q_norm[j:j + 1, :].to_broadcast([128, 64]))
        kb.dma("sp", G[:, 1024:1088], s.b_k_norm[j:j + 1, :].to_broadcast([128, 64]))
        kb.ts(G[:, 0:64], G[:, 0:64], 0.125, None, ALU.mult)
        for h in range(1, 16):
            kb.copy(G[:, h * 64:(h + 1) * 64], G[:, 0:64])
        for h in range(1, 4):
            kb.copy(G[:, 1024 + h * 64:1024 + (h + 1) * 64], G[:, 1024:1088])
        xp = kb.pool(2, [128, D], F32, "x")
        junk = kb.tile([128, D], F32, "junk")
        tmpf = kb.tile([128, D], F32, "tmpf")
        ssp = kb.pool(2, [128, 2], F32, "ss")
        hbp = kb.pool(2, [128, D], BF16, "hb")
        hTp = kb.pool(2, [128, 8, 128], BF16, "hT")
        psT = kb.pool(2, [128, 512], F32, "psT", space="psum")
        psP = kb.pool(3, [128, 512], F32, "psP", space="psum")
        pqs = kb.tile([128, 1536], F32, "pqs")
        sq = kb.tile([128, 1280], F32, "sq")
        ssh = kb.tile([128, 20], F32, "ssh")
        qn = kb.tile([128, 1280], F32, "qn")
        t1 = kb.tile([128, 1280], F32, "t1")
        t2 = kb.tile([128, 1280], F32, "t2")
        csp = kb.pool(2, [128, 32], F32, "cs")
        snp = kb.pool(2, [128, 32], F32, "sn")
        qkbp = kb.pool(2, [128, 1536], BF16, "qkb")
        qkTp = kb.pool(2, [128, 12, 128], BF16, "qkT")
        vap = kb.pool(2, [128, 4, 65], BF16, "va")
        for b in vap.bufs:
            kb.memset(b.v(), 1.0)
        for ti in range(NT):
            w = 0 if ti < NLT else 1
            t0 = ti * 128
            xt = xp.next()
            kb.dma("sp", xt.v(), s.X[t0:t0 + 128, :])
            hb = hbp.next()
            s.norm_mod(xt, mod[(w, 1)], mod[(w, 0)], hb, tmpf, junk, ssp.next())
            hT = hTp.next()
            s.transpose_chunks(hT.v(), hb.v(), 8, psT, s.ident_b)
            for nb in range(3):
                ps = psP.next()
                for k in range(8):
                    kb.mm(ps.v(), hT[:, k, :], w_in[:, k, nb * 512:(nb + 1) * 512], start=(k == 0), stop=(k == 7))
                kb.copy(pqs[:, nb * 512:(nb + 1) * 512], ps.v(), eng="act")
            cs = csp.next()
            sn = snp.next()
            kb.dma("sp", cs.v(), s.k_cos[t0:t0 + 128, :])
            kb.dma("sp", sn.v(), s.k_sin[t0:t0 + 128, :])
            t13, t23 = s.qk_post(pqs[:, 0:1280], 20, G, cs, sn, qn, sq, ssh, t1, t2)
            qkb = qkbp.next()
            q3 = qkb[:, 0:1024].re("p (h d) -> p h d", d=64)
            k3 = qkb[:, 1024:1536].re("p (h d) -> p h d", d=128)
            kb.tt(q3[:, :, 0:32], t13[:, 0:16, 0:32], t23[:, 0:16, 0:32], ALU.subtract)
            kb.tt(q3[:, :, 32:64], t13[:, 0:16, 32:64], t23[:, 0:16, 32:64], ALU.add)
            kb.tt(k3[:, :, 0:32], t13[:, 16:20, 0:32], t23[:, 16:20, 0:32], ALU.subtract)
            kb.tt(k3[:, :, 32:64], t13[:, 16:20, 32:64], t23[:, 16:20, 32:64], ALU.add)
            kb.copy(k3[:, :, 64:128], k3[:, :, 0:64])
            qkT = qkTp.next()
            s.transpose_chunks(qkT.v(), qkb.v(), 12, psT, s.ident_b)
            kb.dma("pool", s.QKT[0:12, :, t0:t0 + 128].rearrange("c p t -> p c t"), qkT.v())
            va = vap.next()
            kb.copy(va[:, :, 0:64], pqs[:, 1280:1536].re("p (h d) -> p h d", d=64), eng="act")
            kb.dma("pool", s.VA[t0:t0 + 128, :], va.v().re("p h d -> p (h d)"))
        kb.pop()
        kb.push()
        stage = kb.pool(2, [128, 8, 512], F32, "stage")
        w_out = kb.tile([128, 8, D], BF16, "w_out")
        s.load_w_bf16(w_out, s.b_w_out[j], D, stage)
        esink = kb.tile([128, 16], F32, "esink")
        kb.dma("sp", esink.v(), s.b_sink[j:j + 1, :].to_broadcast([128, 16]))
        kb.act(esink.v(), esink.v(), AF.Exp)
        mf = kb.tile([128, 128], F32, "mf")
        m_prev = kb.tile([128, 128], BF16, "m_prev")
        m_next = kb.tile([128, 128], BF16, "m_next")
        kb.dma("sp", mf.v(), s.k_tril)
        kb.copy(m_prev.v(), mf.v())
        kb.dma("sp", mf.v(), s.k_triu)
        kb.copy(m_next.v(), mf.v())
        kTc = kb.tile([128, 4, 256], BF16, "kTc")
        kb.dma("sp", kTc.v(), s.QKT[8:12, :, N:T].rearrange("c p t -> p c t"))
        Vc = kb.tile([128, 2, 260], BF16, "Vc")
        kb.dma("sp", Vc.v(), s.VA[N:T, :].rearrange("(j p) e -> p j e", p=128))
        qTp = kb.pool(2, [128, 8, 128], BF16, "qT")
        kTwp = kb.pool(2, [128, 4, 384], BF16, "kTw")
        Vwp = kb.pool(2, [128, 3, 260], BF16, "Vw")
        psA = kb.pool(2, [128, 512], F32, "psA", space="psum")
        psB = kb.pool(2, [128, 512], F32, "psB", space="psum")
        psO = kb.pool(2, [128, 512], F32, "psO", space="psum")
        psY = kb.pool(2, [128, 512], F32, "psY", space="psum")
        PTp = kb.pool(3, [128, 5, 128], BF16, "PT")
        osbp = kb.pool(2, [128, D], BF16, "osb")
        oTp = kb.pool(2, [128, 8, 128], BF16, "oT")
        denp = kb.pool(4, [128, 2], F32, "den")
        xp = kb.pool(2, [128, D], F32, "x")
        yt = kb.tile([128, D], F32, "yt")
        for ti in range(NT):
            lat = ti < NLT
            t0 = ti * 128
            qT = qTp.next()
            kb.dma("sp", qT.v(), s.QKT[0:8, :, t0:t0 + 128].rearrange("c p t -> p c t"))
            if lat:
                j0 = max(0, ti - 1)
                j1 = min(NLT, ti + 2)
                nw = j1 - j0
                kTw = kTwp.next()
                kb.dma("sp", kTw[:, :, 0:nw * 128], s.QKT[8:12, :, j0 * 128:j1 * 128].rearrange("c p t -> p c t"))
                Vw = Vwp.next()
                kb.dma("sp", Vw[:, 0:nw, :], s.VA[j0 * 128:j1 * 128, :].rearrange("(j p) e -> p j e", p=128))
            else:
                nw = 0
                j0 = 0
            osb = osbp.next()
            for hq in range(16):
                kv = hq // 4
                po = (hq % 2) * 64
                qh = qT[po:po + 64, hq // 2, :]
                pa = psA.next()
                pb = psB.next()
                PT = PTp.next()
                for jj in range(nw):
                    kb.mm(pa[:, jj * 128:(jj + 1) * 128], kTw[po:po + 64, kv, jj * 128:(jj + 1) * 128], qh)
                for jj in range(2):
                    kb.mm(pb[:, jj * 128:(jj + 1) * 128], kTc[po:po + 64, kv, jj * 128:(jj + 1) * 128], qh)
                if nw:
                    kb.act(PT[:, 0:nw, :], pa[:, 0:nw * 128].re("p (j t) -> p j t", t=128), AF.Exp)
                    if ti - 1 >= 0:
                        kb.tt(PT[:, 0, :], PT[:, 0, :], m_prev.v(), ALU.mult)
                    if ti + 1 < NLT:
                        kb.tt(PT[:, nw - 1, :], PT[:, nw - 1, :], m_next.v(), ALU.mult)
                kb.act(PT[:, 3:5, :], pb[:, 0:256].re("p (j t) -> p j t", t=128), AF.Exp)
                po_ = psO.next()
                nmm = nw + 2
                i = 0
                for jj in range(nw):
                    kb.mm(po_[:, 0:65], PT[:, jj, :], Vw[:, jj, kv * 65:(kv + 1) * 65], start=(i == 0), stop=(i == nmm - 1))
                    i += 1
                for jj in range(2):
                    kb.mm(po_[:, 0:65], PT[:, 3 + jj, :], Vc[:, jj, kv * 65:(kv + 1) * 65], start=(i == 0), stop=(i == nmm - 1))
                    i += 1
                den = denp.next()
                kb.tt(den[:, 0:1], po_[:, 64:65], esink[:, hq:hq + 1], ALU.add)
                kb.recip(den[:, 1:2], den[:, 0:1])
                kb.ts(osb[:, hq * 64:(hq + 1) * 64], po_[:, 0:64], den[:, 1:2], None, ALU.mult)
            oT = oTp.next()
            s.transpose_chunks(oT.v(), osb.v(), 8, psY, s.ident_b)
            xt = xp.next()
            kb.dma("sp", xt.v(), s.X[t0:t0 + 128, :])
            w = 0 if lat else 1
            for nb in range(2):
                ps = psY.next()
                for k in range(8):
                    kb.mm(ps.v(), oT[:, k, :], w_out[:, k, nb * 512:(nb + 1) * 512], start=(k == 0), stop=(k == 7))
                kb.tt(yt[:, nb * 512:(nb + 1) * 512], ps.v(), mod[(w, 2)][:, nb * 512:(nb + 1) * 512], ALU.mult)
            kb.tt(xt.v(), xt.v(), yt.v(), ALU.add)
            kb.dma("pool", s.X[t0:t0 + 128, :], xt.v())
        kb.pop()
        kb.pop()

    def out_proj_res(s, ti, oT, w_out, gate, psY, xp, yt):
        kb = s.kb
        t0 = ti * 128
        xt = xp.next()
        kb.dma("sp", xt.v(), s.X[t0:t0 + 128, :])
        for nb in range(2):
            ps = psY.next()
            for k in range(8):
                kb.mm(ps.v(), oT[:, k, :], w_out[:, k, nb * 512:(nb + 1) * 512], start=(k == 0), stop=(k == 7))
            kb.tt(yt[:, nb * 512:(nb + 1) * 512], ps.v(), gate[:, nb * 512:(nb + 1) * 512], ALU.mult)
        kb.tt(xt.v(), xt.v(), yt.v(), ALU.add)
        kb.dma("pool", s.X[t0:t0 + 128, :], xt.v())

    def layer_diff(s, l, j):
        import math
        kb = s.kb
        N, T, NT, NLT = s.N, s.T, s.NT, s.NLT
        lam_init = 0.8 - 0.6 * math.exp(-0.3 * l)
        kb.push()
        mod = s.adaln(l, [0, 1, 2], s.g_mix[l:l + 1, :])
        kb.push()
        stage = kb.pool(2, [128, 8, 512], F32, "stage")
        w_in = kb.tile([128, 8, 3072], BF16, "w_in")
        s.load_w_bf16(w_in, s.c_w_in[j], 3072, stage)
        G = kb.tile([128, 2048], F32, "G")
        kb.dma("sp", G[:, 0:64], s.c_q_norm[j:j + 1, :].to_broadcast([128, 64]))
        kb.dma("sp", G[:, 1024:1088], s.c_k_norm[j:j + 1, :].to_broadcast([128, 64]))
        kb.ts(G[:, 0:64], G[:, 0:64], 0.125, None, ALU.mult)
        for h in range(1, 16):
            kb.copy(G[:, h * 64:(h + 1) * 64], G[:, 0:64])
            kb.copy(G[:, 1024 + h * 64:1024 + (h + 1) * 64], G[:, 1024:1088])
        xp = kb.pool(2, [128, D], F32, "x")
        junk = kb.tile([128, D], F32, "junk")
        tmpf = kb.tile([128, D], F32, "tmpf")
        ssp = kb.pool(2, [128, 2], F32, "ss")
        hbp = kb.pool(2, [128, D], BF16, "hb")
        hTp = kb.pool(2, [128, 8, 128], BF16, "hT")
        psT = kb.pool(2, [128, 512], F32, "psT", space="psum")
        psP = kb.pool(4, [128, 512], F32, "psP", space="psum")
        pqs = kb.tile([128, 2048], F32, "pqs")
        sq = kb.tile([128, 2048], F32, "sq")
        ssh = kb.tile([128, 32], F32, "ssh")
        qn = kb.tile([128, 2048], F32, "qn")
        t1 = kb.tile([128, 2048], F32, "t1")
        t2 = kb.tile([128, 2048], F32, "t2")
        csp = kb.pool(2, [128, 32], F32, "cs")
        snp = kb.pool(2, [128, 32], F32, "sn")
        qkbp = kb.pool(2, [128, 2048], BF16, "qkb")
        qkTp = kb.pool(2, [128, 16, 128], BF16, "qkT")
        vbp = kb.pool(2, [128, D], BF16, "vb")
        for ti in range(NT):
            w = 0 if ti < NLT else 1
            t0 = ti * 128
            xt = xp.next()
            kb.dma("sp", xt.v(), s.X[t0:t0 + 128, :])
            hb = hbp.next()
            s.norm_mod(xt, mod[(w, 1)], mod[(w, 0)], hb, tmpf, junk, ssp.next())
            hT = hTp.next()
            s.transpose_chunks(hT.v(), hb.v(), 8, psT, s.ident_b)
            vb = vbp.next()
            for nb in range(6):
                ps = psP.next()
                for k in range(8):
                    kb.mm(ps.v(), hT[:, k, :], w_in[:, k, nb * 512:(nb + 1) * 512], start=(k == 0), stop=(k == 7))
                if nb < 4:
                    kb.copy(pqs[:, nb * 512:(nb + 1) * 512], ps.v(), eng="act")
                else:
                    kb.copy(vb[:, (nb - 4) * 512:(nb - 3) * 512], ps.v(), eng="act")
            kb.dma("pool", s.H[t0:t0 + 128, :], vb.v())
            cs = csp.next()
            sn = snp.next()
            kb.dma("sp", cs.v(), s.k_cos[t0:t0 + 128, :])
            kb.dma("sp", sn.v(), s.k_sin[t0:t0 + 128, :])
            t13, t23 = s.qk_post(pqs[:, 0:2048], 32, G, cs, sn, qn, sq, ssh, t1, t2)
            qkb = qkbp.next()
            q3 = qkb.v().re("p (h d) -> p h d", d=64)
            kb.tt(q3[:, :, 0:32], t13[:, :, 0:32], t23[:, :, 0:32], ALU.subtract)
            kb.tt(q3[:, :, 32:64], t13[:, :, 32:64], t23[:, :, 32:64], ALU.add)
            qkT = qkTp.next()
            s.transpose_chunks(qkT.v(), qkb.v(), 16, psT, s.ident_b)
            kb.dma("pool", s.QKT[0:16, :, t0:t0 + 128].rearrange("c p t -> p c t"), qkT.v())
        kb.pop()
        kb.push()
        lv = kb.tile([128, 256], F32, "lv")
        kb.dma("sp", lv.v(), s.c_lambda[j:j + 1].rearrange("o a d -> o (a d)").to_broadcast([128, 256]))
        lt = kb.tile([128, 128], F32, "lt")
        lam = kb.tile([128, 4], F32, "lam")
        kb.tt(lt[:, 0:64], lv[:, 0:64], lv[:, 64:128], ALU.mult)
        kb.tt(lt[:, 64:128], lv[:, 128:192], lv[:, 192:256], ALU.mult)
        kb.red(lam[:, 0:2], lt.v().re("p (a d) -> p a d", d=64), ALU.add)
        kb.act(lam[:, 0:2], lam[:, 0:2], AF.Exp)
        kb.tt(lam[:, 2:3], lam[:, 1:2], lam[:, 0:1], ALU.subtract)
        kb.ts(lam[:, 3:4], lam[:, 2:3], -lam_init, None, ALU.add)
        gsub = kb.tile([128, 1], F32, "gsub")
        kb.dma("sp", gsub.v(), s.c_g_sub[j].rearrange("(p o) -> p o", o=1))
        kb.ts(gsub.v(), gsub.v(), 1.0 - lam_init, None, ALU.mult)
        ones_b = kb.tile([128, 128], BF16, "ones_b")
        kb.memset(ones_b.v(), 1.0)
        ones_f = kb.tile([128, 128], F32, "ones_f")
        kb.memset(ones_f.v(), 1.0)
        kTp = kb.pool(2, [128, T], BF16, "kT")
        Vp = kb.pool(2, [128, NT, 128], BF16, "V")
        qTp = kb.pool(2, [128, 512], BF16, "qT")
        psS = kb.pool(2, [128, 512], F32, "psS", space="psum")
        psO = [kb.tile([128, 512], F32, "psO%d" % m, space="psum") for m in range(2)]
        psD = [kb.tile([128, 512], F32, "psD%d" % m, space="psum") for m in range(2)]
        psN = kb.pool(2, [128, 512], F32, "psN", space="psum")
        PTp = kb.pool(4, [128, 512], BF16, "PT")
        rp = kb.pool(2, [128, 512], F32, "r")
        o1p = kb.pool(2, [128, 512], F32, "o1")
        o2p = kb.pool(2, [128, 512], F32, "o2")
        sqp = kb.pool(2, [128, 512], F32, "sqo")
        oTp = kb.pool(2, [128, 512], BF16, "oT")
        groups = [(g0, 512, list(range(NT))) for g0 in range(0, N, 512)] + [(N, NCTX, [NLT, NLT + 1])]
        for h in range(8):
            kT = kTp.next()
            kb.dma("sp", kT.v(), s.QKT[8 + h, :, :])
            Vh = Vp.next()
            kb.dma("sp", Vh.v(), s.H[:, h * 128:(h + 1) * 128].rearrange("(j p) e -> p j e", p=128))
            for (g0, nq, kts) in groups:
                qT = qTp.next()
                kb.dma("sp", qT[:, 0:nq], s.QKT[h, :, g0:g0 + nq])
                for ki, kt in enumerate(kts):
                    for m in range(2):
                        ps = psS.next()
                        kb.mm(ps[:, 0:nq], kT[64 * m:64 * m + 64, kt * 128:(kt + 1) * 128], qT[64 * m:64 * m + 64, 0:nq])
                        PT = PTp.next()
                        kb.act(PT[:, 0:nq], ps[:, 0:nq], AF.Exp)
                        kb.mm(psO[m][:, 0:nq], Vh[:, kt, :], PT[:, 0:nq], start=(ki == 0), stop=(ki == len(kts) - 1))
                        kb.mm(psD[m][:, 0:nq], ones_b.v(), PT[:, 0:nq], start=(ki == 0), stop=(ki == len(kts) - 1))
                o1 = o1p.next()
                o2 = o2p.next()
                for m, o in ((0, o1), (1, o2)):
                    r = rp.next()
                    kb.recip(r[:, 0:nq], psD[m][:, 0:nq])
                    kb.tt(o[:, 0:nq], psO[m][:, 0:nq], r[:, 0:nq], ALU.mult)
                kb.stt(o1[:, 0:nq], o2[:, 0:nq], lam[:, 3:4], o1[:, 0:nq], ALU.mult, ALU.add)
                sqo = sqp.next()
                kb.tt(sqo[:, 0:nq], o1[:, 0:nq], o1[:, 0:nq], ALU.mult)
                pn = psN.next()
                kb.mm(pn[:, 0:nq], ones_f.v(), sqo[:, 0:nq])
                r = rp.next()
                kb.act(r[:, 0:nq], pn[:, 0:nq], AF.Sqrt, bias=s.epst[:, 0:1], scale=1.0 / 128)
                kb.recip(r[:, 0:nq], r[:, 0:nq])
                oT = oTp.next()
                kb.stt(oT[:, 0:nq], o1[:, 0:nq], gsub[:, 0:1], r[:, 0:nq], ALU.mult, ALU.mult)
                kb.dma("pool", s.QKT[16 + h, :, g0:g0 + nq], oT[:, 0:nq])
        kb.pop()
        kb.push()
        stage = kb.pool(2, [128, 8, 512], F32, "stage")
        w_out = kb.tile([128, 8, D], BF16, "w_out")
        s.load_w_bf16(w_out, s.c_w_out[j], D, stage)
        oTp = kb.pool(2, [128, 8, 128], BF16, "oT")
        psY = kb.pool(2, [128, 512], F32, "psY", space="psum")
        xp = kb.pool(2, [128, D], F32, "x")
        yt = kb.tile([128, D], F32, "yt")
        for ti in range(NT):
            t0 = ti * 128
            w = 0 if ti < NLT else 1
            oT = oTp.next()
            kb.dma("sp", oT.v(), s.QKT[16:24, :, t0:t0 + 128].rearrange("c p t -> p c t"))
            s.out_proj_res(ti, oT, w_out, mod[(w, 2)], psY, xp, yt)
        kb.pop()
        kb.pop()

    def layer_delta(s, l, j):
        kb = s.kb
        N, T, NT, NLT = s.N, s.T, s.NT, s.NLT
        kb.push()
        mod = s.adaln(l, [0, 1, 2], s.g_mix[l:l + 1, :])
        ab_all = kb.tile([128, NT, 32], F32, "ab_all")
        gb_all = kb.tile([128, NT, 32], F32, "gb_all")
        kb.push()
        stage = kb.pool(2, [128, 8, 512], F32, "stage")
        w_in = kb.tile([128, 8, 4128], BF16, "w_in")
        s.load_w_bf16(w_in, s.a_w_in[j], 4128, stage)
        xp = kb.pool(2, [128, D], F32, "x")
        junk = kb.tile([128, D], F32, "junk")
        tmpf = kb.tile([128, D], F32, "tmpf")
        ssp = kb.pool(2, [128, 2], F32, "ss")
        hbp = kb.pool(2, [128, D], BF16, "hb")
        hTp = kb.pool(2, [128, 8, 128], BF16, "hT")
        psT = kb.pool(2, [128, 512], F32, "psT", space="psum")
        psP = kb.pool(4, [128, 512], F32, "psP", space="psum")
        pap = kb.pool(2, [128, 3072], BF16, "pa")
        zbp = kb.pool(2, [128, D], BF16, "zb")
        chunks = [(n0, min(512, 4128 - n0)) for n0 in range(0, 4128, 512)]
        for ti in range(NT):
            w = 0 if ti < NLT else 1
            t0 = ti * 128
            xt = xp.next()
            kb.dma("sp", xt.v(), s.X[t0:t0 + 128, :])
            hb = hbp.next()
            s.norm_mod(xt, mod[(w, 1)], mod[(w, 0)], hb, tmpf, junk, ssp.next())
            hT = hTp.next()
            s.transpose_chunks(hT.v(), hb.v(), 8, psT, s.ident_b)
            pa = pap.next()
            zb = zbp.next()
            for ci, (n0, nsz) in enumerate(chunks):
                ps = psP.next()
                for k in range(8):
                    kb.mm(ps[:, 0:nsz], hT[:, k, :], w_in[:, k, n0:n0 + nsz], start=(k == 0), stop=(k == 7))
                eng = "act" if ci % 2 == 0 else "dve"
                if n0 < 3072:
                    kb.copy(pa[:, n0:n0 + nsz], ps[:, 0:nsz], eng=eng)
                elif n0 < 4096:
                    kb.copy(zb[:, n0 - 3072:n0 - 3072 + nsz], ps[:, 0:nsz], eng=eng)
                else:
                    kb.copy(ab_all[:, ti, :], ps[:, 0:32], eng=eng)
            kb.dma("pool", s.PA[t0:t0 + 128, :], pa.v())
            kb.dma("pool", s.H[t0:t0 + 128, :], zb.v())
        kb.pop()
        if getattr(s, "dbg_stop", 9) <= 1:
            kb.pop(); return
        kb.push()
        wc = []
        for k in range(5):
            t = kb.tile([128, 3072], F32, "wc%d" % k)
            kb.dma("sp", t.v(), s.a_conv[j, k:k + 1, :].to_broadcast([128, 3072]))
            wc.append(t)
        nA = kb.tile([128, 16], F32, "nA")
        kb.dma("sp", nA.v(), s.a_log[j:j + 1].rearrange("o d h -> o (d h)").to_broadcast([128, 16]))
        kb.act(nA.v(), nA.v(), AF.Exp)
        kb.ts(nA.v(), nA.v(), -1.0, None, ALU.mult)
        dtb = kb.tile([128, 16], F32, "dtb")
        kb.dma("sp", dtb.v(), s.a_dt_bias[j:j + 1].rearrange("o d h -> o (d h)").to_broadcast([128, 16]))
        onec = kb.tile([128, 1], F32, "onec")
        kb.memset(onec.v(), 1.0)
        shp = kb.pool(5, [128, 3072], BF16, "sh")
        acc = kb.tile([128, 3072], F32, "acc")
        tmp = kb.tile([128, 3072], F32, "tmp")
        qkv = kb.tile([128, 3072], F32, "qkv")
        sq = kb.tile([128, 2048], F32, "sq")
        ssh = kb.tile([128, 16], F32, "ssh")
        gt = kb.tile([128, 16], F32, "gt")
        qknp = kb.pool(1, [128, 2048], BF16, "qkn")
        kvsp = kb.pool(1, [128, 2048], F32, "kvs")
        qkTp = kb.pool(1, [128, 16, 128], BF16, "qkT")
        psT = kb.pool(2, [128, 512], F32, "psT", space="psum")
        for ti in range(NT):
            seg0, seg1 = (0, N) if ti < NLT else (N, T)
            t0 = ti * 128
            shs = []
            for k in range(5):
                r0 = t0 - 2 + k
                r1 = r0 + 128
                lo = max(r0, seg0)
                hi = min(r1, seg1)
                sh = shp.next()
                if lo > r0 or hi < r1:
                    kb.memset(sh.v(), 0.0)
                kb.dma("sp", sh[lo - r0:hi - r0, :], s.PA[lo:hi, :])
                shs.append(sh)
            kb.tt(acc.v(), shs[0].v(), wc[0].v(), ALU.mult)
            for k in range(1, 5):
                kb.tt(tmp.v(), shs[k].v(), wc[k].v(), ALU.mult, eng="pool")
                kb.tt(acc.v(), acc.v(), tmp.v(), ALU.add)
            kb.act(qkv.v(), acc.v(), AF.Silu)
            x3 = qkv[:, 0:2048].re("p (h d) -> p h d", d=128)
            kb.tt(sq.v().re("p (h d) -> p h d", d=128), x3, x3, ALU.mult)
            kb.red(ssh.v(), sq.v().re("p (h d) -> p h d", d=128), ALU.add)
            kb.act(ssh.v(), ssh.v(), AF.Sqrt, bias=s.epst[:, 0:1], scale=1.0)
            kb.recip(ssh.v(), ssh.v())
            kb.ts(ssh[:, 0:8], ssh[:, 0:8], 128.0 ** -0.5, None, ALU.mult)
            qkn = qknp.next()
            rb = ssh.v().re("p (h o) -> p h o", o=1).bc([128, 16, 128])
            kb.tt(qkn.v().re("p (h d) -> p h d", d=128), x3, rb, ALU.mult)
            kvs = kvsp.next()
            kb.tt(kvs[:, 0:1024].re("p (h d) -> p h d", d=128), qkv[:, 1024:2048].re("p (h d) -> p h d", d=128),
                  ssh[:, 8:16].re("p (h o) -> p h o", o=1).bc([128, 8, 128]), ALU.mult)
            kb.copy(kvs[:, 1024:2048], qkv[:, 2048:3072], eng="act")
            qkT = qkTp.next()
            s.transpose_chunks(qkT.v(), qkn.v(), 16, psT, s.ident_b)
            kb.dma("pool", s.QKT[0:16, :, t0:t0 + 128].rearrange("c p t -> p c t"), qkT.v())
            kb.dma("pool", s.KVS[t0:t0 + 128, :], kvs.v())
            kb.tt(gt.v(), ab_all[:, ti, 0:16], dtb.v(), ALU.add)
            kb.act(gt.v(), gt.v(), AF.Exp)
            kb.act(gt.v(), gt.v(), AF.Ln, bias=onec[:, 0:1])
            kb.tt(gb_all[:, ti, 0:16], gt.v(), nA.v(), ALU.mult)
            kb.act(gb_all[:, ti, 16:32], ab_all[:, ti, 16:32], AF.Sigmoid)
        kb.pop()
        if getattr(s, "dbg_stop", 9) <= 2:
            kb.pop(); return
        for d in (0, 1):
            if getattr(s, "dbg_stop", 9) <= 3 + d - 1 + 0 and d == 1:
                break
            kb.push()
            ones_f = kb.tile([128, 128], F32, "ones_f")
            kb.memset(ones_f.v(), 1.0)
            tril_i = kb.tile([128, 128], F32, "tril_i")
            triu_i = kb.tile([128, 128], F32, "triu_i")
            tril_s = kb.tile([128, 128], F32, "tril_s")
            triu_s = kb.tile([128, 128], F32, "triu_s")
            kb.dma("sp", tril_i.v(), s.k_tril)
            kb.dma("sp", triu_i.v(), s.k_triu)
            kb.tt(tril_s.v(), tril_i.v(), s.ident_f.v(), ALU.subtract)
            kb.tt(triu_s.v(), triu_i.v(), s.ident_f.v(), ALU.subtract)
            bd16 = kb.tile([128, 128], F32, "bd16")
            kb.dma("sp", bd16.v(), s.k_bd16)
            offm = []
            for li in range(3):
                t_ = kb.tile([128, 128], F32, "off%d" % li)
                kb.dma("sp", t_.v(), s.k_off[li])
                offm.append(t_)
            if d == 0:
                mA, mAT, mQK, cumM = tril_s, triu_s, triu_i, triu_i
                order = [NLT, NLT + 1] + list(range(NLT))
            else:
                mA, mAT, mQK, cumM = triu_s, tril_s, tril_i, tril_i
                order = [NLT + 1, NLT] + list(range(NLT - 1, -1, -1))
            S = [kb.tile([128, 128], F32, "S%d" % h_) for h_ in range(8)]
            for h_ in range(8):
                kb.memset(S[h_].v(), 0.0)
            psR = kb.pool(6, [128, 512], F32, "psR", space="psum")
            kvsp = kb.pool(1, [128, 2048], F32, "kvs")
            qkbp = kb.pool(2, [128, 16, 128], BF16, "qkb")
            qkfp = kb.pool(1, [128, 16, 128], F32, "qkf")
            sc = {nm: kb.pool(2, [128, 8], F32, nm) for nm in ("gcl2", "gam", "e_", "gamL", "bg", "lnb", "gcb", "nb", "tmp8")}
            gclp = kb.pool(2, [128, 16], F32, "gcl")
            GH = 4
            depth = {"P": 5, "Q": 5, "X": 2, "Xn": 2, "PL": 2, "QL": 2, "I1": 2, "I2": 2}
            mslots = [{nm: kb.pool(depth.get(nm, 1), [128, 128], F32, "%s%d" % (nm, sl)) for nm in
                       ("dg1", "dg2", "E1", "E2", "E3", "P", "Q", "X", "Xn", "PL", "QL", "I1", "I2", "Mqk", "bV", "bgK",
                        "Kt", "nWT", "Vn", "o1s")} for sl in range(GH)]
            otp = kb.pool(1, [128, D], F32, "ot")
            if d == 1:
                stage = kb.pool(1, [128, 8, 256], F32, "stage")
                w_out = kb.tile([128, 8, D], BF16, "w_out")
                s.load_w_bf16(w_out, s.a_w_out[j], D, stage, chunk=256)
                gout = kb.tile([128, 128], F32, "gout")
                kb.dma("sp", gout.v(), s.a_g_out[j:j + 1, :].to_broadcast([128, 128]))
                ofp = kb.pool(1, [128, D], F32, "of")
                zp = kb.pool(1, [128, D], BF16, "z")
                zf = kb.tile([128, D], F32, "zf")
                sqo = kb.tile([128, D], F32, "sqo")
                rs8 = kb.pool(2, [128, 8], F32, "rs8")
                obp = kb.pool(1, [128, D], BF16, "ob")
                oTp = kb.pool(1, [128, 8, 128], BF16, "oT")
                psY = kb.pool(2, [128, 512], F32, "psY", space="psum")
                xp = kb.pool(1, [128, D], F32, "x")
                yt = kb.tile([128, D], F32, "yt")
            for c in order[:int(_os.environ.get('DBG_C', '999'))]:
                t0 = c * 128
                kvs = kvsp.next()
                kb.dma("sp", kvs.v(), s.KVS[t0:t0 + 128, :])
                qkb = qkbp.next()
                kb.dma("sp", qkb.v(), s.QKT[0:16, :, t0:t0 + 128].rearrange("c p t -> p c t"))
                qkf = qkfp.next()
                kb.copy(qkf.v(), qkb.v(), eng="act")
                g8 = gb_all[:, c, d * 8:(d + 1) * 8]
                b8 = gb_all[:, c, 16 + d * 8:16 + (d + 1) * 8]
                ps = psR.next()
                kb.mm(ps[:, 0:8], cumM.v(), g8)
                kb.mm(ps[:, 8:16], ones_f.v(), g8)
                gcl = gclp.next()
                kb.copy(gcl.v(), ps[:, 0:16])
                gc = gcl[:, 0:8]
                gl = gcl[:, 8:16]
                gam = sc["gam"].next(); e_ = sc["e_"].next(); gamL = sc["gamL"].next(); bg = sc["bg"].next()
                lnb = sc["lnb"].next(); gcb = sc["gcb"].next(); nb = sc["nb"].next(); tmp8 = sc["tmp8"].next()
                kb.act(gam.v(), gc, AF.Exp)
                kb.tt(tmp8.v(), gl, gc, ALU.subtract)
                kb.act(e_.v(), tmp8.v(), AF.Exp)
                kb.act(gamL.v(), gl, AF.Exp)
                kb.tt(bg.v(), b8, gam.v(), ALU.mult)
                kb.act(lnb.v(), b8, AF.Ln)
                kb.tt(gcb.v(), gc, lnb.v(), ALU.add)
                kb.ts(nb.v(), b8, -1.0, None, ALU.mult)
                ot = otp.next()

                def head_gen(h, mp):
                    Kh = kvs[:, h * 128:(h + 1) * 128]
                    Vh = kvs[:, 1024 + h * 128:1024 + (h + 1) * 128]
                    QT = qkf[:, h, :]
                    KT = qkf[:, 8 + h, :]
                    gch = gcl[:, h:h + 1]
                    Sh = S[h]
                    dg1 = mp["dg1"].next(); dg2 = mp["dg2"].next()
                    kb.ts(dg1.v(), s.ident_f.v(), gch, None, ALU.mult)
                    kb.ts(dg2.v(), s.ident_f.v(), gcb[:, h:h + 1], None, ALU.mult)
                    bV = mp["bV"].next(); bgK = mp["bgK"].next(); Kt = mp["Kt"].next()
                    kb.ts(bV.v(), Vh, b8[:, h:h + 1], None, ALU.mult)
                    kb.ts(bgK.v(), Kh, bg[:, h:h + 1], None, ALU.mult)
                    kb.ts(Kt.v(), Kh, e_[:, h:h + 1], None, ALU.mult)
                    yield
                    pAB = psR.next()
                    kb.mm(pAB[:, 0:128], KT, KT)
                    kb.mm(pAB[:, 128:256], KT, QT)
                    kb.mm(pAB[:, 256:384], ones_f.v(), dg1.v())
                    kb.mm(pAB[:, 384:512], ones_f.v(), dg2.v())
                    yield
                    pKK0 = pAB[:, 0:128]; pQK = pAB[:, 128:256]; bc = pAB[:, 256:384]; bc2 = pAB[:, 384:512]
                    E1 = mp["E1"].next(); E2 = mp["E2"].next(); E3 = mp["E3"].next()
                    P0 = mp["P"].next(); Q0 = mp["Q"].next(); Mqk = mp["Mqk"].next()
                    kb.ts(E1.v(), bc, gch, 0.0, ALU.subtract, ALU.max)
                    kb.ts(E2.v(), bc2, gch, 0.0, ALU.subtract, ALU.min)
                    kb.ts(E3.v(), bc, gch, 0.0, ALU.subtract, ALU.min)
                    kb.act(E1.v(), E1.v(), AF.Exp, scale=-1.0)
                    kb.act(E2.v(), E2.v(), AF.Exp)
                    kb.act(E3.v(), E3.v(), AF.Exp)
                    kb.tt(E1.v(), E1.v(), pKK0, ALU.mult)
                    kb.stt(P0.v(), E1.v(), nb[:, h:h + 1], mA.v(), ALU.mult, ALU.mult)
                    kb.tt(E2.v(), E2.v(), pKK0, ALU.mult)
                    kb.stt(Q0.v(), E2.v(), -1.0, mAT.v(), ALU.mult, ALU.mult)
                    kb.tt(E3.v(), E3.v(), pQK, ALU.mult)
                    kb.tt(Mqk.v(), E3.v(), mQK.v(), ALU.mult)
                    Pb = mp["P"].next(); Qb = mp["Q"].next()
                    kb.tt(Pb.v(), P0.v(), bd16.v(), ALU.mult)
                    kb.tt(Qb.v(), Q0.v(), bd16.v(), ALU.mult)
                    Xt = mp["X"].next(); Xn = mp["Xn"].next()
                    kb.tt(Xt.v(), Qb.v(), s.ident_f.v(), ALU.add)
                    kb.tt(Xn.v(), Pb.v(), s.ident_f.v(), ALU.add)
                    yield
                    Pk, Qk = Pb, Qb
                    for k in range(1, 4):
                        pp = psR.next()
                        kb.mm(pp[:, 0:128], Qk.v(), Pk.v())
                        kb.mm(pp[:, 128:256], Pk.v(), Qk.v())
                        yield
                        Pn = mp["P"].next(); Qn = mp["Q"].next()
                        kb.copy(Pn.v(), pp[:, 0:128])
                        kb.copy(Qn.v(), pp[:, 128:256])
                        yield
                        pa = psR.next()
                        kb.mm(pa[:, 0:128], Pn.v(), Xt.v())
                        kb.mm(pa[:, 128:256], Qn.v(), Xn.v())
                        yield
                        Xt2 = mp["X"].next(); Xn2 = mp["Xn"].next()
                        kb.tt(Xt2.v(), Xt.v(), pa[:, 0:128], ALU.add)
                        kb.tt(Xn2.v(), Xn.v(), pa[:, 128:256], ALU.add)
                        Pk, Qk, Xt, Xn = Pn, Qn, Xt2, Xn2
                    for li in range(3):
                        last = li == 2
                        PL = mp["PL"].next()
                        kb.tt(PL.v(), P0.v(), offm[li].v(), ALU.mult)
                        if not last:
                            QL = mp["QL"].next()
                            kb.tt(QL.v(), Q0.v(), offm[li].v(), ALU.mult)
                        yield
                        pi = psR.next()
                        kb.mm(pi[:, 0:128], PL.v(), Xt.v())
                        if not last:
                            kb.mm(pi[:, 128:256], QL.v(), Xn.v())
                        yield
                        I1 = mp["I1"].next()
                        kb.copy(I1.v(), pi[:, 0:128])
                        if not last:
                            I2 = mp["I2"].next()
                            kb.copy(I2.v(), pi[:, 128:256])
                        yield
                        po2 = psR.next()
                        kb.mm(po2[:, 0:128], Xn.v(), I1.v())
                        if not last:
                            kb.mm(po2[:, 128:256], Xt.v(), I2.v())
                        yield
                        Xt2 = mp["X"].next()
                        kb.tt(Xt2.v(), Xt.v(), po2[:, 0:128], ALU.add)
                        if not last:
                            Xn2 = mp["Xn"].next()
                            kb.tt(Xn2.v(), Xn.v(), po2[:, 128:256], ALU.add)
                            Xn = Xn2
                        Xt = Xt2
                    X = Xt
                    yield
                    pW = psR.next()
                    kb.mm(pW[:, 0:128], bgK.v(), X.v())
                    yield
                    nWT = mp["nWT"].next()
                    kb.ts(nWT.v(), pW[:, 0:128], -1.0, None, ALU.mult)
                    yield
                    pV = psR.next()
                    kb.mm(pV[:, 0:128], X.v(), bV.v(), start=True, stop=False)
                    kb.mm(pV[:, 0:128], nWT.v(), Sh.v(), start=False, stop=True)
                    yield
                    Vn = mp["Vn"].next()
                    kb.copy(Vn.v(), pV[:, 0:128])
                    yield
                    pO = psR.next()
                    kb.mm(pO[:, 0:128], QT, Sh.v())
                    kb.mm(pO[:, 128:256], Mqk.v(), Vn.v())
                    kb.mm(pO[:, 256:384], Kt.v(), Vn.v())
                    yield
                    o1s = mp["o1s"].next()
                    kb.ts(o1s.v(), pO[:, 0:128], gam[:, h:h + 1], None, ALU.mult)
                    kb.tt(ot[:, h * 128:(h + 1) * 128], o1s.v(), pO[:, 128:256], ALU.add)
                    kb.stt(Sh.v(), Sh.v(), gamL[:, h:h + 1], pO[:, 256:384], ALU.mult, ALU.add)

                for g0 in range(0, 8, GH):
                    gens = [head_gen(h, mslots[h - g0]) for h in range(g0, min(8, g0 + GH))]
                    while gens:
                        for g in list(gens):
                            try:
                                next(g)
                            except StopIteration:
                                gens.remove(g)
                if d == 0:
                    kb.dma("pool", s.OF[t0:t0 + 128, :], ot.v())
                else:
                    w = 0 if c < NLT else 1
                    of = ofp.next()
                    kb.dma("sp", of.v(), s.OF[t0:t0 + 128, :])
                    kb.tt(ot.v(), ot.v(), of.v(), ALU.add)
                    o3 = ot.v().re("p (h d) -> p h d", d=128)
                    kb.tt(sqo.v(), ot.v(), ot.v(), ALU.mult)
                    r8 = rs8.next()
                    kb.red(r8.v(), sqo.v().re("p (h d) -> p h d", d=128), ALU.add)
                    kb.act(r8.v(), r8.v(), AF.Sqrt, bias=s.epst[:, 0:1], scale=1.0 / 128)
                    kb.recip(r8.v(), r8.v())
                    kb.tt(o3, o3, r8.v().re("p (h o) -> p h o", o=1).bc([128, 8, 128]), ALU.mult)
                    kb.tt(o3, o3, gout.v().re("p (o d) -> p o d", o=1).bc([128, 8, 128]), ALU.mult)
                    z = zp.next()
                    kb.dma("sp", z.v(), s.H[t0:t0 + 128, :])
                    kb.act(zf.v(), z.v(), AF.Silu)
                    ob = obp.next()
                    kb.tt(ob.v(), ot.v(), zf.v(), ALU.mult)
                    oT = oTp.next()
                    s.transpose_chunks(oT.v(), ob.v(), 8, psY, s.ident_b)
                    s.out_proj_res(c, oT, w_out, mod[(w, 2)], psY, xp, yt)
            kb.pop()
        kb.pop()

    def moe(s, l):
        kb = s.kb
        N, T, NT, NLT, S = s.N, s.T, s.NT, s.NLT, s.S
        kb.push()
        mod = s.adaln(l, [3, 4, 5], s.g_ffn[l:l + 1, :])
        affT = kb.tile([16, T], F32, "affT")
        slot_i = kb.tile([128, NT, 16], I32, "slot_i")
        gateT = kb.tile([128, NT, 16], F32, "gateT")
        kb.push()
        wr = kb.tile([128, 8, 16], F32, "wr")
        kb.dma("sp", wr.v(), s.w_router[l].rearrange("(k p) e -> p k e", p=128))
        xp = kb.pool(2, [128, D], F32, "x")
        junk = kb.tile([128, D], F32, "junk")
        tmpf = kb.tile([128, D], F32, "tmpf")
        ssp = kb.pool(2, [128, 2], F32, "ss")
        hfp = kb.pool(2, [128, D], F32, "hf")
        hbp = kb.pool(2, [128, D], BF16, "hb")
        hTf = kb.pool(2, [128, 8, 128], F32, "hTf")
        psT = kb.pool(4, [128, 512], F32, "psT", space="psum")
        psL = kb.pool(2, [128, 512], F32, "psL", space="psum")
        smp = kb.pool(2, [128, 4], F32, "sm")
        ep = kb.pool(2, [128, 16], F32, "e")
        for ti in range(NT):
            w = 0 if ti < NLT else 1
            t0 = ti * 128
            xt = xp.next()
            kb.dma("sp", xt.v(), s.X[t0:t0 + 128, :])
            hf = hfp.next()
            s.norm_mod(xt, mod[(w, 4)], mod[(w, 3)], hf, tmpf, junk, ssp.next())
            hb = hbp.next()
            kb.copy(hb.v(), hf.v(), eng="act")
            kb.dma("pool", s.H[t0:t0 + 128, :], hb.v())
            hT = hTf.next()
            for half in range(2):
                ps = psT.next()
                for i in range(4):
                    k = half * 4 + i
                    kb.tr(ps[:, i * 128:(i + 1) * 128], hf[:, k * 128:(k + 1) * 128], s.ident_f.v())
                kb.copy(hT[:, half * 4:half * 4 + 4, :], ps.v().re("p (k t) -> p k t", t=128), eng="act")
            pl = psL.next()
            for k in range(8):
                kb.mm(pl[:, 0:16], hT[:, k, :], wr[:, k, :], start=(k == 0), stop=(k == 7))
            sm = smp.next()
            e = ep.next()
            kb.red(sm[:, 0:1], pl[:, 0:16], ALU.max)
            kb.ts(sm[:, 1:2], sm[:, 0:1], -1.0, None, ALU.mult)
            kb.memset(sm[:, 2:3], 0.0)
            kb.act(e.v(), pl[:, 0:16], AF.Exp, bias=sm[:, 1:2], accum_out=sm[:, 2:3])
            kb.recip(sm[:, 3:4], sm[:, 2:3])
            kb.ts(e.v(), e.v(), sm[:, 3:4], None, ALU.mult)
            pt = psL.next()
            kb.tr(pt[0:16, 0:128], e.v(), s.ident_f.v())
            kb.copy(affT[:, t0:t0 + 128], pt[0:16, 0:128])
        kb.pop()
        kb.push()
        escal = kb.tile([16, 2], F32, "escal")
        kb.dma("sp", escal.v(), s.k_escal)
        Wmax = max(N, NCTX)
        work = kb.tile([16, Wmax], F32, "work")
        ones = kb.tile([16, 1], F32, "ones")
        kb.memset(ones.v(), 1.0)
        t8 = kb.tile([16, 8], F32, "t8")
        mask = kb.tile([16, T], F32, "mask")
        slotf = kb.tile([16, T], F32, "slotf")
        gsel = affT
        for (c0, c1, cap, ecol) in ((0, N, s.cap_lat, 0), (N, T, s.cap_ctx, 1)):
            n = c1 - c0
            src = affT[:, c0:c1]
            for r in range(cap // 8):
                kb.emit("dve", lambda E, src=src: E.max(out=t8.h[:], in_=src.ap), [src], [t8])
                kb.emit("dve", lambda E, src=src, n=n: E.match_replace(out=work.h[:, 0:n], in_to_replace=t8.h[:],
                                                                        in_values=src.ap, imm_value=0.0),
                        [src, t8], [work])
                src = work[:, 0:n]
            kb.ts(mask[:, c0:c1], work[:, 0:n], 0.0, None, ALU.is_equal)
            kb.emit("dve", lambda E, n=n, c0=c0, c1=c1: E.tensor_tensor_scan(
                out=slotf.h[:, c0:c1], data0=ones.h[:, 0:1].to_broadcast([16, n]), data1=mask.h[:, c0:c1], initial=0.0,
                op0=ALU.mult, op1=ALU.add), [ones, mask], [slotf])
            kb.stt(slotf[:, c0:c1], slotf[:, c0:c1], escal[:, ecol:ecol + 1], mask[:, c0:c1], ALU.add, ALU.mult)
            kb.ts(slotf[:, c0:c1], slotf[:, c0:c1], BIG, None, ALU.add)
        kb.tt(gsel.v(), affT.v(), mask.v(), ALU.mult)
        psS = kb.pool(2, [128, 512], F32, "psS", space="psum")
        slt = kb.tile([128, NT, 16], F32, "slt")
        for (srcT, dst) in ((slotf, slt), (gsel, gateT)):
            for g0 in range(0, NT, 32):
                g1 = min(NT, g0 + 32)
                ps = psS.next()
                for ti in range(g0, g1):
                    kb.tr(ps[:, (ti - g0) * 16:(ti - g0 + 1) * 16], srcT[:, ti * 128:(ti + 1) * 128], s.ident_f[0:16, 0:16])
                kb.copy(dst[:, g0:g1, :], ps[:, 0:(g1 - g0) * 16].re("p (t e) -> p t e", e=16))
        kb.copy(slot_i.v(), slt.v())
        kb.pop()
        kb.push()
        hbp = kb.pool(3, [128, D], BF16, "hb")
        for ti in range(NT):
            t0 = ti * 128
            hb = hbp.next()
            kb.dma("sp", hb.v(), s.H[t0:t0 + 128, :])
            for e in range(16):
                kb.dma("pool", s.XG, hb.v(), indirect=dict(idx=slot_i[:, ti, e:e + 1], side="out", bound=16 * S - 1))
        kb.pop()
        kb.push()
        stage = kb.pool(1, [128, 8, 512], F32, "stage")
        wgu = kb.tile([128, 8, 2 * D], BF16, "wgu")
        wd = kb.tile([128, 8, D], BF16, "wd")
        nst = (S + 127) // 128
        stiles = [(i * 128, min(128, S - i * 128)) for i in range(nst)]
        nchunks = [(n0, min(512, S - n0)) for n0 in range(0, S, 512)]
        xgp = kb.pool(1, [128, nst, D], BF16, "xg")
        xgT = kb.tile([128, 8, S], BF16, "xgT")
        actT = kb.tile([128, 8, S], BF16, "actT")
        psX = kb.pool(2, [128, 512], F32, "psX", space="psum")
        psG = kb.pool(2, [128, 512], F32, "psG", space="psum")
        psU = kb.pool(2, [128, 512], F32, "psU", space="psum")
        psD = kb.pool(2, [128, 512], F32, "psD", space="psum")
        sgp = kb.pool(2, [128, 512], F32, "sg")
        ysp = kb.pool(2, [128, D], BF16, "ys")
        for e in range(16):
            s.load_w_bf16(wgu, s.w_gate_up[l, e], 2 * D, stage)
            s.load_w_bf16(wd, s.w_down[l, e], D, stage)
            xg = xgp.next()
            for i, (s0, sz) in enumerate(stiles):
                kb.dma("sp", xg[0:sz, i, :], s.XG[e * S + s0:e * S + s0 + sz, :])
            for i, (s0, sz) in enumerate(stiles):
                ps = psX.next()
                psb = ps.v().bitcast(BF16)
                for k in range(8):
                    kb.tr(psb[:, k * 128:k * 128 + sz], xg[0:sz, i, k * 128:(k + 1) * 128], s.ident_b[0:sz, 0:sz])
                kb.copy(xgT[:, :, s0:s0 + sz], psb.re("p (k t) -> p k t", t=128)[:, :, 0:sz], eng="act")
            for fc in range(8):
                for (n0, nsz) in nchunks:
                    pg = psG.next()
                    pu = psU.next()
                    for k in range(8):
                        kb.mm(pg[:, 0:nsz], wgu[:, k, fc * 128:(fc + 1) * 128], xgT[:, k, n0:n0 + nsz],
                              start=(k == 0), stop=(k == 7))
                    for k in range(8):
                        kb.mm(pu[:, 0:nsz], wgu[:, k, D + fc * 128:D + (fc + 1) * 128], xgT[:, k, n0:n0 + nsz],
                              start=(k == 0), stop=(k == 7))
                    sg = sgp.next()
                    kb.act(sg[:, 0:nsz], pg[:, 0:nsz], AF.Silu)
                    kb.tt(actT[:, fc, n0:n0 + nsz], sg[:, 0:nsz], pu[:, 0:nsz], ALU.mult)
            for i, (s0, sz) in enumerate(stiles):
                ys = ysp.next()
                for nb in range(2):
                    pd = psD.next()
                    for fk in range(8):
                        kb.mm(pd[0:sz, :], actT[:, fk, s0:s0 + sz], wd[:, fk, nb * 512:(nb + 1) * 512],
                              start=(fk == 0), stop=(fk == 7))
                    kb.copy(ys[0:sz, nb * 512:(nb + 1) * 512], pd[0:sz, :], eng="act")
                kb.dma("pool", s.Y[e * S + s0:e * S + s0 + sz, :], ys[0:sz, :])
        kb.pop()
        kb.push()
        gp = kb.pool(4, [128, D], BF16, "gath")
        for b in gp.bufs:
            kb.memset(b.v(), 0.0)
        accp = kb.pool(2, [128, D], F32, "acc")
        xp = kb.pool(2, [128, D], F32, "x")
        for ti in range(NT):
            w = 0 if ti < NLT else 1
            t0 = ti * 128
            acc = accp.next()
            kb.memset(acc.v(), 0.0)
            for e in range(16):
                g = gp.next()
                kb.dma("pool", g.v(), s.Y, indirect=dict(idx=slot_i[:, ti, e:e + 1], side="in", bound=16 * S - 1))
                kb.stt(acc.v(), g.v(), gateT[:, ti, e:e + 1], acc.v(), ALU.mult, ALU.add)
            xt = xp.next()
            kb.dma("sp", xt.v(), s.X[t0:t0 + 128, :])
            kb.tt(acc.v(), acc.v(), mod[(w, 5)].v(), ALU.mult)
            kb.tt(xt.v(), xt.v(), acc.v(), ALU.add)
            kb.dma("pool", s.X[t0:t0 + 128, :], xt.v())
        kb.pop()
        kb.pop()

    def finish(s):
        kb = s.kb
        for r0 in range(0, s.T, 1024):
            r1 = min(s.T, r0 + 1024)
            kb.dma("sp", s.out[r0:r1, :], s.X[r0:r1, :])
        kb.barrier()


def host_consts(N):
    T = N + NCTX
    S = 2 * N // 16 + 2 * NCTX // 16
    GRID_W = 64
    rows = N // GRID_W
    t_row = np.repeat(np.arange(rows), GRID_W).astype(np.float32)
    t_col = np.tile(np.arange(GRID_W), rows).astype(np.float32)
    n_freq = 16
    inv = (10000.0 ** (-np.arange(n_freq, dtype=np.float32) / n_freq)).astype(np.float32)
    ang = np.concatenate([t_row[:, None] * inv, t_col[:, None] * inv], -1).astype(np.float32)
    cos = np.ones((T, 32), np.float32)
    sin = np.zeros((T, 32), np.float32)
    cos[:N] = np.cos(ang)
    sin[:N] = np.sin(ang)
    a = np.arange(128)
    tril = (a[None, :] <= a[:, None]).astype(np.float32)
    triu = (a[:, None] <= a[None, :]).astype(np.float32)
    e = np.arange(16, dtype=np.float32)
    escal = np.stack([e * S - 1 - BIG, e * S - 1 - BIG + 2 * N // 16], 1).astype(np.float32)
    ii = a[:, None]
    jj = a[None, :]
    bd16 = ((ii // 16) == (jj // 16)).astype(np.float32)
    offs = []
    for b in (16, 32, 64):
        same2b = (ii // (2 * b)) == (jj // (2 * b))
        diffb = (ii // b) != (jj // b)
        offs.append((same2b & diffb).astype(np.float32))
    return dict(k_bd16=bd16, k_off=np.stack(offs, 0), k_ident=np.eye(128, dtype=np.float32), k_cos=cos, k_sin=sin, k_tril=tril, k_triu=triu, k_escal=escal)


from concourse.bass_utils import run_bass_kernel_spmd

N_FULL = 8192


def kernel(**inputs):
    N = N_FULL
    mk = MK(N)
    mk.setup()
    for l in range(4):
        kind, j = l % 3, l // 3
        getattr(mk, ["layer_delta", "layer_swa", "layer_diff"][kind])(l, j)
        mk.moe(l)
    mk.finish()
    consts = host_consts(N)
    in_maps = []
    B = inputs["x"].shape[0]
    for b in range(B):
        im = {}
        for k in mk.inp:
            if k.startswith("k_"):
                im[k] = consts[k]
            elif k in ("x", "ctx", "c"):
                im[k] = np.ascontiguousarray(np.asarray(inputs[k])[b])
            else:
                im[k] = np.ascontiguousarray(np.asarray(inputs[k]))
        in_maps.append(im)
    res = run_bass_kernel_spmd(mk.nc, in_maps, core_ids=list(range(B)))
    out = np.stack([np.asarray(r["out"])[:N] for r in res.results], 0).astype(np.float32)
    return out
```

```python
import numpy as np
from contextlib import ExitStack
import concourse.bass as bass
import concourse.mybir as mybir

F32 = mybir.dt.float32
BF16 = mybir.dt.bfloat16
I32 = mybir.dt.int32
AF = mybir.ActivationFunctionType
ALU = mybir.AluOpType
AX = mybir.AxisListType

SELF_SYNC = True
FOLD_WAITS = True


class V:
    __slots__ = ("buf", "ap")

    def __init__(s, buf, ap):
        s.buf = buf
        s.ap = ap

    def __getitem__(s, k):
        return V(s.buf, s.ap[k])

    def re(s, pat, **kw):
        return V(s.buf, s.ap.rearrange(pat, **kw))

    def bc(s, shape):
        return V(s.buf, s.ap.to_broadcast(shape))

    def bitcast(s, dt):
        return V(s.buf, s.ap.bitcast(dt))


class Buf:
    def __init__(s, kb, handle, name):
        s.kb = kb
        s.h = handle
        s.name = name
        s.w = None
        s.r = {}
        s.sem = None
        s.semcnt = 0

    def __getitem__(s, k):
        return V(s, s.h[k])

    def v(s):
        return V(s, s.h[:])


class Pool:
    def __init__(s, bufs):
        s.bufs = bufs
        s.i = 0

    def next(s):
        b = s.bufs[s.i % len(s.bufs)]
        s.i += 1
        return b


def _ap(x):
    return x.ap if isinstance(x, V) else x


class KB:
    def __init__(s, nc):
        s.nc = nc
        s.E = {"pe": nc.tensor, "act": nc.scalar, "dve": nc.vector, "pool": nc.gpsimd, "sp": nc.sync}
        s.sem = {}
        s.cnt = {}
        s.waited = {e: {} for e in s.E}
        s.semh = {}
        for e in s.E:
            h = nc.alloc_semaphore("sem_" + e)
            s.sem[e] = h
            s.semh[e] = h
            s.cnt[e] = 0
        s.bar = nc.alloc_semaphore("sem_bar")
        s.barcnt = 0
        s.nbuf = 0
        s.dmabufs = []
        s.stack = [ExitStack()]
        s.phase_bufs = [[]]
        s.free_sems = []
        s.bar_t = s.tile([128, 8], F32, name="bar_t")
        s.n_ins = 0

    def tile(s, shape, dtype, name=None, space="sbuf"):
        s.nbuf += 1
        name = (name or "t") + "_%d" % s.nbuf
        if space == "sbuf":
            h = s.stack[-1].enter_context(s.nc.sbuf_tensor(name, list(shape), dtype))
        else:
            h = s.stack[-1].enter_context(s.nc.psum_tensor(name, list(shape), dtype))
        b = Buf(s, h, name)
        s.phase_bufs[-1].append(b)
        return b

    def pool(s, n, shape, dtype, name=None, space="sbuf"):
        return Pool([s.tile(shape, dtype, name=name, space=space) for _ in range(n)])

    def push(s):
        s.stack.append(ExitStack())
        s.phase_bufs.append([])

    def pop(s):
        s.barrier()
        for b in s.phase_bufs.pop():
            if b.sem is not None:
                s.free_sems.append((b.sem, b.semcnt, ("d", b.name)))
                b.sem = None
        s.stack.pop().close()

    def _getsem(s, b):
        if b.sem is None:
            if s.free_sems:
                h, c, oldkey = s.free_sems.pop()
                b.sem = h
                b.semcnt = c
                for e in s.E:
                    s.waited[e][("d", b.name)] = c
            else:
                b.sem = s.nc.alloc_semaphore("sd_" + b.name)
                b.semcnt = 0
            s.semh[("d", b.name)] = b.sem
            s.dmabufs.append(b)
        return b.sem

    def _wait(s, eng, ev):
        key, val = ev
        if s.waited[eng].get(key, 0) >= val:
            return
        if key == eng and (eng == "pe" or eng == "sp" or not SELF_SYNC):
            return
        s.E[eng].wait_ge(s.semh[key], val)
        s.waited[eng][key] = val

    def _need(s, eng, ev, lst):
        key, val = ev
        if s.waited[eng].get(key, 0) >= val:
            return
        if key == eng and (eng == "pe" or eng == "sp" or not SELF_SYNC):
            return
        for i, (k2, v2) in enumerate(lst):
            if k2 == key:
                if v2 < val:
                    lst[i] = (key, val)
                return
        lst.append((key, val))

    def _deps(s, eng, reads, writes, acc=False):
        lst = []
        for b in reads:
            if b.w is not None:
                s._need(eng, b.w, lst)
        for b in writes:
            if b.w is not None:
                if not (acc and b.w[0] == eng):
                    s._need(eng, b.w, lst)
            for k, v in b.r.items():
                s._need(eng, (k, v), lst)
        for ev in lst[:-1]:
            s._wait(eng, ev)
        return lst[-1] if lst else None

    def _fold(s, eng, ins, ev):
        if ev is None:
            return
        key, val = ev
        if FOLD_WAITS:
            ins._wait_ge(s.semh[key], val)
            s.waited[eng][key] = val
        else:
            raise RuntimeError("fold disabled")

    def emit(s, eng, fn, reads, writes, acc=False):
        reads = [x.buf if isinstance(x, V) else x for x in reads if isinstance(x, (V, Buf))]
        writes = [x.buf if isinstance(x, V) else x for x in writes if isinstance(x, (V, Buf))]
        last = s._deps(eng, reads, writes, acc)
        if last is not None and not FOLD_WAITS:
            s._wait(eng, last)
            last = None
        ins = fn(s.E[eng])
        s._fold(eng, ins, last)
        s.cnt[eng] += 1
        ins.then_inc(s.sem[eng], 1)
        ev = (eng, s.cnt[eng])
        for b in reads:
            b.r[eng] = s.cnt[eng]
        for b in writes:
            b.w = ev
            b.r = {}
        s.n_ins += 1
        return ins

    def dma(s, q, out, in_, indirect=None, **kw):
        obuf = out.buf if isinstance(out, V) else None
        ibuf = in_.buf if isinstance(in_, V) else None
        extra = []
        if indirect is not None:
            extra = [indirect["idx"].buf]
        carrier = obuf or ibuf or s.bar_t
        sem = s._getsem(carrier)
        reads = [b for b in [ibuf] + extra if b is not None]
        writes = [b for b in [obuf] if b is not None]
        last = s._deps(q, reads, writes)
        if last is not None and (not FOLD_WAITS or indirect is not None):
            s._wait(q, last)
            last = None
        E = s.E[q]
        if indirect is None:
            ins = E.dma_start(out=_ap(out), in_=_ap(in_), **kw)
        else:
            off = bass.IndirectOffsetOnAxis(ap=indirect["idx"].ap, axis=0)
            if not hasattr(s, "_bregs"):
                s._bregs = {}
            if indirect["bound"] not in s._bregs:
                s._bregs[indirect["bound"]] = E.to_reg(indirect["bound"])
            breg = s._bregs[indirect["bound"]]
            if indirect["side"] == "out":
                ins = E.indirect_dma_start(out=_ap(out), out_offset=off, in_=_ap(in_), in_offset=None,
                                           bounds_check=breg, oob_is_err=False)
            else:
                ins = E.indirect_dma_start(out=_ap(out), out_offset=None, in_=_ap(in_), in_offset=off,
                                           bounds_check=breg, oob_is_err=False)
        s._fold(q, ins, last)
        carrier.semcnt += 16
        ins.then_inc(sem, 16)
        key = ("d", carrier.name)
        ev = (key, carrier.semcnt)
        for b in reads:
            b.r[key] = carrier.semcnt
        for b in writes:
            b.w = ev
            b.r = {}
        s.n_ins += 1
        return ins

    def barrier(s):
        for e in s.E:
            if e != "pool" and s.cnt[e] > 0:
                s._wait("pool", (e, s.cnt[e]))
        if s.cnt["pool"] > 0:
            key, val = "pool", s.cnt["pool"]
            if s.waited["pool"].get(key, 0) < val:
                s.E["pool"].wait_ge(s.sem["pool"], val)
                s.waited["pool"][key] = val
        for b in s.dmabufs:
            if b.sem is not None and b.semcnt > 0:
                s._wait("pool", (("d", b.name), b.semcnt))
        s.barcnt += 1
        s.E["pool"].memset(s.bar_t.h[:, 0:1], 0.0).then_inc(s.bar, 1)
        for e in s.E:
            if e != "pool":
                s.E[e].wait_ge(s.bar, s.barcnt)
        s.E["pool"].wait_ge(s.bar, s.barcnt)
        for e in s.E:
            for e2 in s.E:
                s.waited[e][e2] = s.cnt[e2]
            for b in s.dmabufs:
                if b.sem is not None:
                    s.waited[e][("d", b.name)] = b.semcnt
        for lst in s.phase_bufs:
            for b in lst:
                b.w = None
                b.r = {}
        s.dmabufs = [b for b in s.dmabufs if b.sem is not None]

    def mm(s, out, lhsT, rhs, start=True, stop=True):
        return s.emit("pe", lambda E: E.matmul(_ap(out), _ap(lhsT), _ap(rhs), start=start, stop=stop),
                      [lhsT, rhs], [out], acc=not start)

    def tr(s, out, in_, ident):
        return s.emit("pe", lambda E: E.transpose(_ap(out), _ap(in_), _ap(ident)), [in_, ident], [out])

    def act(s, out, in_, func, bias=None, scale=None, accum_out=None, eng="act"):
        kw = {}
        rd = [in_]
        wr = [out]
        if bias is not None:
            kw["bias"] = _ap(bias)
            rd.append(bias)
        if scale is not None:
            kw["scale"] = _ap(scale)
            rd.append(scale)
        if accum_out is not None:
            kw["accum_out"] = _ap(accum_out)
            wr.append(accum_out)
        return s.emit(eng, lambda E: E.activation(out=_ap(out), in_=_ap(in_), func=func, **kw), rd, wr)

    def tt(s, out, a, b, op, eng="dve"):
        return s.emit(eng, lambda E: E.tensor_tensor(out=_ap(out), in0=_ap(a), in1=_ap(b), op=op), [a, b], [out])

    def ts(s, out, a, s1, s2, op0, op1=None, eng="dve"):
        kw = {}
        if op1 is not None:
            kw["op1"] = op1
        return s.emit(eng, lambda E: E.tensor_scalar(out=_ap(out), in0=_ap(a), scalar1=_ap(s1), scalar2=_ap(s2),
                                                     op0=op0, **kw), [a, s1, s2], [out])

    def stt(s, out, in0, scalar, in1, op0, op1, eng="dve"):
        return s.emit(eng, lambda E: E.scalar_tensor_tensor(out=_ap(out), in0=_ap(in0), scalar=_ap(scalar),
                                                            in1=_ap(in1), op0=op0, op1=op1),
                      [in0, scalar, in1], [out])

    def copy(s, out, in_, eng="dve"):
        if eng == "act":
            return s.emit(eng, lambda E: E.copy(out=_ap(out), in_=_ap(in_)), [in_], [out])
        return s.emit(eng, lambda E: E.tensor_copy(out=_ap(out), in_=_ap(in_)), [in_], [out])

    def red(s, out, in_, op, axis=AX.X, eng="dve"):
        return s.emit(eng, lambda E: E.tensor_reduce(out=_ap(out), in_=_ap(in_), axis=axis, op=op), [in_], [out])

    def memset(s, out, val, eng="dve"):
        return s.emit(eng, lambda E: E.memset(_ap(out), val), [], [out])

    def recip(s, out, in_):
        return s.emit("dve", lambda E: E.reciprocal(out=_ap(out), in_=_ap(in_)), [in_], [out])


import os as _os

D = 1024
NCTX = 256
EPS = 1e-6
BIG = 32768.0


class MK:
    def __init__(s, N):
        s.N = N
        s.T = N + NCTX
        s.NT = s.T // 128
        s.NLT = N // 128
        s.cap_lat = 2 * N // 16
        s.cap_ctx = 2 * NCTX // 16
        s.S = s.cap_lat + s.cap_ctx
        nc = bass.Bass("TRN2", target_bir_lowering=False)
        s.nc = nc
        s.inp = {}
        s.kb = KB(nc)

    def din(s, name, shape, dt=F32):
        t = s.nc.dram_tensor(name, list(shape), dt, kind="ExternalInput").ap()
        s.inp[name] = t
        return t

    def dscr(s, name, shape, dt):
        return s.nc.dram_tensor(name, list(shape), dt, kind="Internal").ap()

    def setup(s):
        kb = s.kb
        N, T = s.N, s.T
        s.x_in = s.din("x", [N, D])
        s.ctx_in = s.din("ctx", [NCTX, D])
        s.c_in = s.din("c", [D])
        s.cc_in = s.din("c_ctx", [D])
        s.w_ada = s.din("w_ada", [4, D, 6 * D])
        s.b_ada = s.din("b_ada", [4, 6 * D])
        s.g_mix = s.din("g_mix", [4, D])
        s.g_ffn = s.din("g_ffn", [4, D])
        s.a_w_in = s.din("a_w_in", [2, D, 4128])
        s.a_conv = s.din("a_conv", [2, 5, 3072])
        s.a_log = s.din("a_log", [2, 2, 8])
        s.a_dt_bias = s.din("a_dt_bias", [2, 2, 8])
        s.a_g_out = s.din("a_g_out", [2, 128])
        s.a_w_out = s.din("a_w_out", [2, D, D])
        s.b_w_in = s.din("b_w_in", [1, D, 1536])
        s.b_q_norm = s.din("b_q_norm", [1, 64])
        s.b_k_norm = s.din("b_k_norm", [1, 64])
        s.b_sink = s.din("b_sink", [1, 16])
        s.b_w_out = s.din("b_w_out", [1, D, D])
        s.c_w_in = s.din("c_w_in", [1, D, 3072])
        s.c_q_norm = s.din("c_q_norm", [1, 64])
        s.c_k_norm = s.din("c_k_norm", [1, 64])
        s.c_lambda = s.din("c_lambda", [1, 4, 64])
        s.c_g_sub = s.din("c_g_sub", [1, 128])
        s.c_w_out = s.din("c_w_out", [1, D, D])
        s.w_router = s.din("w_router", [4, D, 16])
        s.w_gate_up = s.din("w_gate_up", [4, 16, D, 2 * D])
        s.w_down = s.din("w_down", [4, 16, D, D])
        s.k_ident = s.din("k_ident", [128, 128])
        s.k_cos = s.din("k_cos", [T, 32])
        s.k_sin = s.din("k_sin", [T, 32])
        s.k_tril = s.din("k_tril", [128, 128])
        s.k_triu = s.din("k_triu", [128, 128])
        s.k_bd16 = s.din("k_bd16", [128, 128])
        s.k_off = s.din("k_off", [3, 128, 128])
        s.k_escal = s.din("k_escal", [16, 2])
        s.out = s.nc.dram_tensor("out", [T, D], F32, kind="ExternalOutput").ap()
        s.X = s.dscr("X", [T, D], F32)
        s.H = s.dscr("H", [T, D], BF16)
        s.XG = s.dscr("XG", [16 * s.S, D], BF16)
        s.Y = s.dscr("Y", [16 * s.S, D], BF16)
        s.QKT = s.dscr("QKT", [24, 128, T], BF16)
        s.VA = s.dscr("VA", [T, 4 * 65], BF16)
        s.PA = s.dscr("PA", [T, 3072], BF16)
        s.KVS = s.dscr("KVS", [T, 2048], F32)
        s.OF = s.dscr("OF", [T, D], F32)
        s.ident_f = kb.tile([128, 128], F32, "ident_f")
        s.ident_b = kb.tile([128, 128], BF16, "ident_b")
        kb.dma("sp", s.ident_f.v(), s.k_ident)
        kb.copy(s.ident_b.v(), s.ident_f.v())
        s.epst = kb.tile([128, 1], F32, "eps")
        kb.memset(s.epst.v(), EPS)
        s.scb = []
        for i, cin in enumerate((s.c_in, s.cc_in)):
            ct = kb.tile([128, 8], F32, "c%d" % i)
            kb.dma("sp", ct.v(), cin.rearrange("(k p) -> p k", p=128), allow_slow_non_contiguous=True)
            kb.act(ct.v(), ct.v(), AF.Silu)
            sb = kb.tile([128, 8, 128], F32, "scb%d" % i)
            kb.copy(sb.v(), ct.v().re("p (k o) -> p k o", o=1).bc([128, 8, 128]))
            s.scb.append(sb)
        for r0 in range(0, N, 1024):
            kb.dma("sp", s.X[r0:r0 + 1024, :], s.x_in[r0:r0 + 1024, :])
        kb.dma("sp", s.X[N:T, :], s.ctx_in)
        kb.barrier()

    def adaln(s, l, js, g_ap):
        kb = s.kb
        res = {}
        for w in (0, 1):
            for j in js:
                res[(w, j)] = kb.tile([128, D], F32, "mod%d_%d" % (w, j))
        kb.push()
        wpool = kb.pool(2, [128, 8, 512], F32, "wada")
        bpool = kb.pool(2, [128, 512], F32, "bada")
        pspool = kb.pool(2, [128, 512], F32, "ps_ada", space="psum")
        for j in js:
            for half in (0, 1):
                n0 = j * D + half * 512
                wt = wpool.next()
                kb.dma("sp", wt.v(), s.w_ada[l, :, n0:n0 + 512].rearrange("(k p) n -> p k n", p=128))
                bt = bpool.next()
                kb.dma("sp", bt.v(), s.b_ada[l:l + 1, n0:n0 + 512].to_broadcast([128, 512]))
                for w in (0, 1):
                    ps = pspool.next()
                    for k in range(8):
                        kb.mm(ps.v(), s.scb[w][:, k, :], wt[:, k, :], start=(k == 0), stop=(k == 7))
                    kb.tt(res[(w, j)][:, half * 512:(half + 1) * 512], ps.v(), bt.v(), ALU.add)
        gt = kb.tile([128, D], F32, "gt")
        kb.dma("sp", gt.v(), g_ap.to_broadcast([128, D]))
        for w in (0, 1):
            t = res[(w, js[1])]
            kb.stt(t.v(), t.v(), 1.0, gt.v(), ALU.add, ALU.mult)
        kb.pop()
        return res

    def norm_mod(s, xt, gs, sh, out, tmp_f, junk, ss):
        kb = s.kb
        kb.memset(ss.v(), 0.0)
        kb.act(junk.v(), xt.v(), AF.Square, accum_out=ss[:, 0:1])
        kb.act(ss[:, 1:2], ss[:, 0:1], AF.Sqrt, bias=s.epst[:, 0:1], scale=1.0 / D)
        kb.recip(ss[:, 1:2], ss[:, 1:2])
        kb.stt(tmp_f.v(), xt.v(), ss[:, 1:2], gs.v(), ALU.mult, ALU.mult)
        kb.tt(out.v(), tmp_f.v(), sh.v(), ALU.add)

    def transpose_chunks(s, dst, src, n, pspool, ident, evac="act"):
        kb = s.kb
        c = 0
        while c < n:
            m = min(8, n - c)
            ps = pspool.next()
            psb = ps.v().bitcast(BF16)
            for i in range(m):
                kb.tr(psb[:, i * 128:(i + 1) * 128], src[:, (c + i) * 128:(c + i + 1) * 128], ident.v())
            kb.copy(dst[:, c:c + m, :], psb[:, 0:m * 128].re("p (k t) -> p k t", t=128), eng=evac)
            c += m

    def load_w_bf16(s, dst, src_ap, ncols, stage_pool, chunk=512, engs=("dve", "act")):
        kb = s.kb
        i = 0
        for n0 in range(0, ncols, chunk):
            nsz = min(chunk, ncols - n0)
            st = stage_pool.next()
            kb.dma("sp", st[:, :, 0:nsz], src_ap[:, n0:n0 + nsz].rearrange("(k p) n -> p k n", p=128))
            kb.copy(dst[:, :, n0:n0 + nsz], st[:, :, 0:nsz], eng=engs[i % len(engs)])
            i += 1

    def qk_post(s, pqs, nh, G, cs, sn, qn, sq, ssh, t1, t2):
        kb = s.kb
        x3 = pqs.re("p (h d) -> p h d", d=64)
        kb.tt(sq.v().re("p (h d) -> p h d", d=64)[:, 0:nh, :], x3, x3, ALU.mult)
        kb.red(ssh[:, 0:nh], sq.v().re("p (h d) -> p h d", d=64)[:, 0:nh, :], ALU.add)
        kb.act(ssh[:, 0:nh], ssh[:, 0:nh], AF.Sqrt, bias=s.epst[:, 0:1], scale=1.0 / 64)
        kb.recip(ssh[:, 0:nh], ssh[:, 0:nh])
        q3 = qn.v().re("p (h d) -> p h d", d=64)[:, 0:nh, :]
        kb.tt(q3, x3, ssh[:, 0:nh].re("p (h o) -> p h o", o=1).bc([128, nh, 64]), ALU.mult)
        kb.tt(q3, q3, G.v().re("p (h d) -> p h d", d=64)[:, 0:nh, :], ALU.mult)
        x1 = q3[:, :, 0:32]
        x2 = q3[:, :, 32:64]
        cb = cs.v().re("p (o d) -> p o d", o=1).bc([128, nh, 32])
        sb = sn.v().re("p (o d) -> p o d", o=1).bc([128, nh, 32])
        t13 = t1.v().re("p (h d) -> p h d", d=64)[:, 0:nh, :]
        t23 = t2.v().re("p (h d) -> p h d", d=64)[:, 0:nh, :]
        kb.tt(t13[:, :, 0:32], x1, cb, ALU.mult)
        kb.tt(t13[:, :, 32:64], x2, cb, ALU.mult)
        kb.tt(t23[:, :, 0:32], x2, sb, ALU.mult)
        kb.tt(t23[:, :, 32:64], x1, sb, ALU.mult)
        return t13, t23

    def layer_swa(s, l, j):
        kb = s.kb
        N, T, NT, NLT = s.N, s.T, s.NT, s.NLT
        kb.push()
        mod = s.adaln(l, [0, 1, 2], s.g_mix[l:l + 1, :])
        kb.push()
        stage = kb.pool(2, [128, 8, 512], F32, "stage")
        w_in = kb.tile([128, 8, 1536], BF16, "w_in")
        s.load_w_bf16(w_in, s.b_w_in[j], 1536, stage)
        G = kb.tile([128, 20 * 64], F32, "G")
        kb.dma("sp", G[:, 0:64], s.b_q_norm[j:j + 1, :].to_broadcast([128, 64]))
        kb.dma("sp", G[:, 1024:1088], s.b_k_norm[j:j + 1, :].to_broadcast([128, 64]))
        kb.ts(G[:, 0:64], G[:, 0:64], 0.125, None, ALU.mult)
        for h in range(1, 16):
            kb.copy(G[:, h * 64:(h + 1) * 64], G[:, 0:64])
        for h in range(1, 4):
            kb.copy(G[:, 1024 + h * 64:1024 + (h + 1) * 64], G[:, 1024:1088])
        xp = kb.pool(2, [128, D], F32, "x")
        junk = kb.tile([128, D], F32, "junk")
        tmpf = kb.tile([128, D], F32, "tmpf")
        ssp = kb.pool(2, [128, 2], F32, "ss")
        hbp = kb.pool(2, [128, D], BF16, "hb")
        hTp = kb.pool(2, [128, 8, 128], BF16, "hT")
        psT = kb.pool(2, [128, 512], F32, "psT", space="psum")
        psP = kb.pool(3, [128, 512], F32, "psP", space="psum")
        pqs = kb.tile([128, 1536], F32, "pqs")
        sq = kb.tile([128, 1280], F32, "sq")
        ssh = kb.tile([128, 20], F32, "ssh")
        qn = kb.tile([128, 1280], F32, "qn")
        t1 = kb.tile([128, 1280], F32, "t1")
        t2 = kb.tile([128, 1280], F32, "t2")
        csp = kb.pool(2, [128, 32], F32, "cs")
        snp = kb.pool(2, [128, 32], F32, "sn")
        qkbp = kb.pool(2, [128, 1536], BF16, "qkb")
        qkTp = kb.pool(2, [128, 12, 128], BF16, "qkT")
        vap = kb.pool(2, [128, 4, 65], BF16, "va")
        for b in vap.bufs:
            kb.memset(b.v(), 1.0)
        for ti in range(NT):
            w = 0 if ti < NLT else 1
            t0 = ti * 128
            xt = xp.next()
            kb.dma("sp", xt.v(), s.X[t0:t0 + 128, :])
            hb = hbp.next()
            s.norm_mod(xt, mod[(w, 1)], mod[(w, 0)], hb, tmpf, junk, ssp.next())
            hT = hTp.next()
            s.transpose_chunks(hT.v(), hb.v(), 8, psT, s.ident_b)
            for nb in range(3):
                ps = psP.next()
                for k in range(8):
                    kb.mm(ps.v(), hT[:, k, :], w_in[:, k, nb * 512:(nb + 1) * 512], start=(k == 0), stop=(k == 7))
                kb.copy(pqs[:, nb * 512:(nb + 1) * 512], ps.v(), eng="act")
            cs = csp.next()
            sn = snp.next()
            kb.dma("sp", cs.v(), s.k_cos[t0:t0 + 128, :])
            kb.dma("sp", sn.v(), s.k_sin[t0:t0 + 128, :])
            t13, t23 = s.qk_post(pqs[:, 0:1280], 20, G, cs, sn, qn, sq, ssh, t1, t2)
            qkb = qkbp.next()
            q3 = qkb[:, 0:1024].re("p (h d) -> p h d", d=64)
            k3 = qkb[:, 1024:1536].re("p (h d) -> p h d", d=128)
            kb.tt(q3[:, :, 0:32], t13[:, 0:16, 0:32], t23[:, 0:16, 0:32], ALU.subtract)
            kb.tt(q3[:, :, 32:64], t13[:, 0:16, 32:64], t23[:, 0:16, 32:64], ALU.add)
            kb.tt(k3[:, :, 0:32], t13[:, 16:20, 0:32], t23[:, 16:20, 0:32], ALU.subtract)
            kb.tt(k3[:, :, 32:64], t13[:, 16:20, 32:64], t23[:, 16:20, 32:64], ALU.add)
            kb.copy(k3[:, :, 64:128], k3[:, :, 0:64])
            qkT = qkTp.next()
            s.transpose_chunks(qkT.v(), qkb.v(), 12, psT, s.ident_b)
            kb.dma("pool", s.QKT[0:12, :, t0:t0 + 128].rearrange("c p t -> p c t"), qkT.v())
            va = vap.next()
            kb.copy(va[:, :, 0:64], pqs[:, 1280:1536].re("p (h d) -> p h d", d=64), eng="act")
            kb.dma("pool", s.VA[t0:t0 + 128, :], va.v().re("p h d -> p (h d)"))
        kb.pop()
        kb.push()
        stage = kb.pool(2, [128, 8, 512], F32, "stage")
        w_out = kb.tile([128, 8, D], BF16, "w_out")
        s.load_w_bf16(w_out, s.b_w_out[j], D, stage)
        esink = kb.tile([128, 16], F32, "esink")
        kb.dma("sp", esink.v(), s.b_sink[j:j + 1, :].to_broadcast([128, 16]))
        kb.act(esink.v(), esink.v(), AF.Exp)
        mf = kb.tile([128, 128], F32, "mf")
        m_prev = kb.tile([128, 128], BF16, "m_prev")
        m_next = kb.tile([128, 128], BF16, "m_next")
        kb.dma("sp", mf.v(), s.k_tril)
        kb.copy(m_prev.v(), mf.v())
        kb.dma("sp", mf.v(), s.k_triu)
        kb.copy(m_next.v(), mf.v())
        kTc = kb.tile([128, 4, 256], BF16, "kTc")
        kb.dma("sp", kTc.v(), s.QKT[8:12, :, N:T].rearrange("c p t -> p c t"))
        Vc = kb.tile([128, 2, 260], BF16, "Vc")
        kb.dma("sp", Vc.v(), s.VA[N:T, :].rearrange("(j p) e -> p j e", p=128))
        qTp = kb.pool(2, [128, 8, 128], BF16, "qT")
        kTwp = kb.pool(2, [128, 4, 384], BF16, "kTw")
        Vwp = kb.pool(2, [128, 3, 260], BF16, "Vw")
        psA = kb.pool(2, [128, 512], F32, "psA", space="psum")
        psB = kb.pool(2, [128, 512], F32, "psB", space="psum")
        psO = kb.pool(2, [128, 512], F32, "psO", space="psum")
        psY = kb.pool(2, [128, 512], F32, "psY", space="psum")
        PTp = kb.pool(3, [128, 5, 128], BF16, "PT")
        osbp = kb.pool(2, [128, D], BF16, "osb")
        oTp = kb.pool(2, [128, 8, 128], BF16, "oT")
        denp = kb.pool(4, [128, 2], F32, "den")
        xp = kb.pool(2, [128, D], F32, "x")
        yt = kb.tile([128, D], F32, "yt")
        for ti in range(NT):
            lat = ti < NLT
            t0 = ti * 128
            qT = qTp.next()
            kb.dma("sp", qT.v(), s.QKT[0:8, :, t0:t0 + 128].rearrange("c p t -> p c t"))
            if lat:
                j0 = max(0, ti - 1)
                j1 = min(NLT, ti + 2)
                nw = j1 - j0
                kTw = kTwp.next()
                kb.dma("sp", kTw[:, :, 0:nw * 128], s.QKT[8:12, :, j0 * 128:j1 * 128].rearrange("c p t -> p c t"))
                Vw = Vwp.next()
                kb.dma("sp", Vw[:, 0:nw, :], s.VA[j0 * 128:j1 * 128, :].rearrange("(j p) e -> p j e", p=128))
            else:
                nw = 0
                j0 = 0
            osb = osbp.next()
            for hq in range(16):
                kv = hq // 4
                po = (hq % 2) * 64
                qh = qT[po:po + 64, hq // 2, :]
                pa = psA.next()
                pb = psB.next()
                PT = PTp.next()
                for jj in range(nw):
                    kb.mm(pa[:, jj * 128:(jj + 1) * 128], kTw[po:po + 64, kv, jj * 128:(jj + 1) * 128], qh)
                for jj in range(2):
                    kb.mm(pb[:, jj * 128:(jj + 1) * 128], kTc[po:po + 64, kv, jj * 128:(jj + 1) * 128], qh)
                if nw:
                    kb.act(PT[:, 0:nw, :], pa[:, 0:nw * 128].re("p (j t) -> p j t", t=128), AF.Exp)
                    if ti - 1 >= 0:
                        kb.tt(PT[:, 0, :], PT[:, 0, :], m_prev.v(), ALU.mult)
                    if ti + 1 < NLT:
                        kb.tt(PT[:, nw - 1, :], PT[:, nw - 1, :], m_next.v(), ALU.mult)
                kb.act(PT[:, 3:5, :], pb[:, 0:256].re("p (j t) -> p j t", t=128), AF.Exp)
                po_ = psO.next()
                nmm = nw + 2
                i = 0
                for jj in range(nw):
                    kb.mm(po_[:, 0:65], PT[:, jj, :], Vw[:, jj, kv * 65:(kv + 1) * 65], start=(i == 0), stop=(i == nmm - 1))
                    i += 1
                for jj in range(2):
                    kb.mm(po_[:, 0:65], PT[:, 3 + jj, :], Vc[:, jj, kv * 65:(kv + 1) * 65], start=(i == 0), stop=(i == nmm - 1))
                    i += 1
                den = denp.next()
                kb.tt(den[:, 0:1], po_[:, 64:65], esink[:, hq:hq + 1], ALU.add)
                kb.recip(den[:, 1:2], den[:, 0:1])
                kb.ts(osb[:, hq * 64:(hq + 1) * 64], po_[:, 0:64], den[:, 1:2], None, ALU.mult)
            oT = oTp.next()
            s.transpose_chunks(oT.v(), osb.v(), 8, psY, s.ident_b)
            xt = xp.next()
            kb.dma("sp", xt.v(), s.X[t0:t0 + 128, :])
            w = 0 if lat else 1
            for nb in range(2):
                ps = psY.next()
                for k in range(8):
                    kb.mm(ps.v(), oT[:, k, :], w_out[:, k, nb * 512:(nb + 1) * 512], start=(k == 0), stop=(k == 7))
                kb.tt(yt[:, nb * 512:(nb + 1) * 512], ps.v(), mod[(w, 2)][:, nb * 512:(nb + 1) * 512], ALU.mult)
            kb.tt(xt.v(), xt.v(), yt.v(), ALU.add)
            kb.dma("pool", s.X[t0:t0 + 128, :], xt.v())
        kb.pop()
        kb.pop()

    def out_proj_res(s, ti, oT, w_out, gate, psY, xp, yt):
        kb = s.kb
        t0 = ti * 128
        xt = xp.next()
        kb.dma("sp", xt.v(), s.X[t0:t0 + 128, :])
        for nb in range(2):
            ps = psY.next()
            for k in range(8):
                kb.mm(ps.v(), oT[:, k, :], w_out[:, k, nb * 512:(nb + 1) * 512], start=(k == 0), stop=(k == 7))
            kb.tt(yt[:, nb * 512:(nb + 1) * 512], ps.v(), gate[:, nb * 512:(nb + 1) * 512], ALU.mult)
        kb.tt(xt.v(), xt.v(), yt.v(), ALU.add)
        kb.dma("pool", s.X[t0:t0 + 128, :], xt.v())

    def layer_diff(s, l, j):
        import math
        kb = s.kb
        N, T, NT, NLT = s.N, s.T, s.NT, s.NLT
        lam_init = 0.8 - 0.6 * math.exp(-0.3 * l)
        kb.push()
        mod = s.adaln(l, [0, 1, 2], s.g_mix[l:l + 1, :])
        kb.push()
        stage = kb.pool(2, [128, 8, 512], F32, "stage")
        w_in = kb.tile([128, 8, 3072], BF16, "w_in")
        s.load_w_bf16(w_in, s.c_w_in[j], 3072, stage)
        G = kb.tile([128, 2048], F32, "G")
        kb.dma("sp", G[:, 0:64], s.c_q_norm[j:j + 1, :].to_broadcast([128, 64]))
        kb.dma("sp", G[:, 1024:1088], s.c_k_norm[j:j + 1, :].to_broadcast([128, 64]))
        kb.ts(G[:, 0:64], G[:, 0:64], 0.125, None, ALU.mult)
        for h in range(1, 16):
            kb.copy(G[:, h * 64:(h + 1) * 64], G[:, 0:64])
            kb.copy(G[:, 1024 + h * 64:1024 + (h + 1) * 64], G[:, 1024:1088])
        xp = kb.pool(2, [128, D], F32, "x")
        junk = kb.tile([128, D], F32, "junk")
        tmpf = kb.tile([128, D], F32, "tmpf")
        ssp = kb.pool(2, [128, 2], F32, "ss")
        hbp = kb.pool(2, [128, D], BF16, "hb")
        hTp = kb.pool(2, [128, 8, 128], BF16, "hT")
        psT = kb.pool(2, [128, 512], F32, "psT", space="psum")
        psP = kb.pool(4, [128, 512], F32, "psP", space="psum")
        pqs = kb.tile([128, 2048], F32, "pqs")
        sq = kb.tile([128, 2048], F32, "sq")
        ssh = kb.tile([128, 32], F32, "ssh")
        qn = kb.tile([128, 2048], F32, "qn")
        t1 = kb.tile([128, 2048], F32, "t1")
        t2 = kb.tile([128, 2048], F32, "t2")
        csp = kb.pool(2, [128, 32], F32, "cs")
        snp = kb.pool(2, [128, 32], F32, "sn")
        qkbp = kb.pool(2, [128, 2048], BF16, "qkb")
        qkTp = kb.pool(2, [128, 16, 128], BF16, "qkT")
        vbp = kb.pool(2, [128, D], BF16, "vb")
        for ti in range(NT):
            w = 0 if ti < NLT else 1
            t0 = ti * 128
            xt = xp.next()
            kb.dma("sp", xt.v(), s.X[t0:t0 + 128, :])
            hb = hbp.next()
            s.norm_mod(xt, mod[(w, 1)], mod[(w, 0)], hb, tmpf, junk, ssp.next())
            hT = hTp.next()
            s.transpose_chunks(hT.v(), hb.v(), 8, psT, s.ident_b)
            vb = vbp.next()
            for nb in range(6):
                ps = psP.next()
                for k in range(8):
                    kb.mm(ps.v(), hT[:, k, :], w_in[:, k, nb * 512:(nb + 1) * 512], start=(k == 0), stop=(k == 7))
                if nb < 4:
                    kb.copy(pqs[:, nb * 512:(nb + 1) * 512], ps.v(), eng="act")
                else:
                    kb.copy(vb[:, (nb - 4) * 512:(nb - 3) * 512], ps.v(), eng="act")
            kb.dma("pool", s.H[t0:t0 + 128, :], vb.v())
            cs = csp.next()
            sn = snp.next()
            kb.dma("sp", cs.v(), s.k_cos[t0:t0 + 128, :])
            kb.dma("sp", sn.v(), s.k_sin[t0:t0 + 128, :])
            t13, t23 = s.qk_post(pqs[:, 0:2048], 32, G, cs, sn, qn, sq, ssh, t1, t2)
            qkb = qkbp.next()
            q3 = qkb.v().re("p (h d) -> p h d", d=64)
            kb.tt(q3[:, :, 0:32], t13[:, :, 0:32], t23[:, :, 0:32], ALU.subtract)
            kb.tt(q3[:, :, 32:64], t13[:, :, 32:64], t23[:, :, 32:64], ALU.add)
            qkT = qkTp.next()
            s.transpose_chunks(qkT.v(), qkb.v(), 16, psT, s.ident_b)
            kb.dma("pool", s.QKT[0:16, :, t0:t0 + 128].rearrange("c p t -> p c t"), qkT.v())
        kb.pop()
        kb.push()
        lv = kb.tile([128, 256], F32, "lv")
        kb.dma("sp", lv.v(), s.c_lambda[j:j + 1].rearrange("o a d -> o (a d)").to_broadcast([128, 256]))
        lt = kb.tile([128, 128], F32, "lt")
        lam = kb.tile([128, 4], F32, "lam")
        kb.tt(lt[:, 0:64], lv[:, 0:64], lv[:, 64:128], ALU.mult)
        kb.tt(lt[:, 64:128], lv[:, 128:192], lv[:, 192:256], ALU.mult)
        kb.red(lam[:, 0:2], lt.v().re("p (a d) -> p a d", d=64), ALU.add)
        kb.act(lam[:, 0:2], lam[:, 0:2], AF.Exp)
        kb.tt(lam[:, 2:3], lam[:, 1:2], lam[:, 0:1], ALU.subtract)
        kb.ts(lam[:, 3:4], lam[:, 2:3], -lam_init, None, ALU.add)
        gsub = kb.tile([128, 1], F32, "gsub")
        kb.dma("sp", gsub.v(), s.c_g_sub[j].rearrange("(p o) -> p o", o=1))
        kb.ts(gsub.v(), gsub.v(), 1.0 - lam_init, None, ALU.mult)
        ones_b = kb.tile([128, 128], BF16, "ones_b")
        kb.memset(ones_b.v(), 1.0)
        ones_f = kb.tile([128, 128], F32, "ones_f")
        kb.memset(ones_f.v(), 1.0)
        kTp = kb.pool(2, [128, T], BF16, "kT")
        Vp = kb.pool(2, [128, NT, 128], BF16, "V")
        qTp = kb.pool(2, [128, 512], BF16, "qT")
        psS = kb.pool(4, [128, 512], F32, "psS", space="psum")
        psO = [kb.tile([128, 512], F32, "psO%d" % m, space="psum") for m in range(2)]
        accD = [kb.tile([128, 512], F32, "accD%d" % m) for m in range(2)]
        psN = kb.pool(2, [128, 512], F32, "psN", space="psum")
        PTp = kb.pool(6, [128, 512], BF16, "PT")
        rp = kb.pool(2, [128, 512], F32, "r")
        o1p = kb.pool(2, [128, 512], F32, "o1")
        o2p = kb.pool(2, [128, 512], F32, "o2")
        sqp = kb.pool(2, [128, 512], F32, "sqo")
        oTp = kb.pool(2, [128, 512], BF16, "oT")
        groups = [(g0, 512, list(range(NT))) for g0 in range(0, N, 512)] + [(N, NCTX, [NLT, NLT + 1])]
        for h in range(8):
            kT = kTp.next()
            kb.dma("sp", kT.v(), s.QKT[8 + h, :, :])
            Vh = Vp.next()
            kb.dma("sp", Vh.v(), s.H[:, h * 128:(h + 1) * 128].rearrange("(j p) e -> p j e", p=128))
            for (g0, nq, kts) in groups:
                qT = qTp.next()
                kb.dma("sp", qT[:, 0:nq], s.QKT[h, :, g0:g0 + nq])
                units = [(ki, kt, m) for ki, kt in enumerate(kts) for m in range(2)]
                LA = 3
                pend = []
                for i in range(len(units) + LA):
                    if i < len(units):
                        ki, kt, m = units[i]
                        ps = psS.next()
                        kb.mm(ps[:, 0:nq], kT[64 * m:64 * m + 64, kt * 128:(kt + 1) * 128], qT[64 * m:64 * m + 64, 0:nq])
                        PT = PTp.next()
                        kb.act(PT[:, 0:nq], ps[:, 0:nq], AF.Exp)
                        pend.append(PT)
                    if i - LA >= 0:
                        ki, kt, m = units[i - LA]
                        PT = pend.pop(0)
                        kb.mm(psO[m][:, 0:nq], Vh[:, kt, :], PT[:, 0:nq], start=(ki == 0), stop=(ki == len(kts) - 1))
                        if ki == 0:
                            kb.copy(accD[m][:, 0:nq], PT[:, 0:nq])
                        else:
                            kb.tt(accD[m][:, 0:nq], accD[m][:, 0:nq], PT[:, 0:nq], ALU.add)
                o1 = o1p.next()
                o2 = o2p.next()
                for m, o in ((0, o1), (1, o2)):
                    r = rp.next()
                    pd_ = psN.next()
                    kb.mm(pd_[:, 0:nq], ones_f.v(), accD[m][:, 0:nq])
                    kb.recip(r[:, 0:nq], pd_[:, 0:nq])
                    kb.tt(o[:, 0:nq], psO[m][:, 0:nq], r[:, 0:nq], ALU.mult)
                kb.stt(o1[:, 0:nq], o2[:, 0:nq], lam[:, 3:4], o1[:, 0:nq], ALU.mult, ALU.add)
                sqo = sqp.next()
                kb.tt(sqo[:, 0:nq], o1[:, 0:nq], o1[:, 0:nq], ALU.mult)
                pn = psN.next()
                kb.mm(pn[:, 0:nq], ones_f.v(), sqo[:, 0:nq])
                r = rp.next()
                kb.act(r[:, 0:nq], pn[:, 0:nq], AF.Sqrt, bias=s.epst[:, 0:1], scale=1.0 / 128)
                kb.recip(r[:, 0:nq], r[:, 0:nq])
                oT = oTp.next()
                kb.stt(oT[:, 0:nq], o1[:, 0:nq], gsub[:, 0:1], r[:, 0:nq], ALU.mult, ALU.mult)
                kb.dma("pool", s.QKT[16 + h, :, g0:g0 + nq], oT[:, 0:nq])
        kb.pop()
        kb.push()
        stage = kb.pool(2, [128, 8, 512], F32, "stage")
        w_out = kb.tile([128, 8, D], BF16, "w_out")
        s.load_w_bf16(w_out, s.c_w_out[j], D, stage)
        oTp = kb.pool(2, [128, 8, 128], BF16, "oT")
        psY = kb.pool(2, [128, 512], F32, "psY", space="psum")
        xp = kb.pool(2, [128, D], F32, "x")
        yt = kb.tile([128, D], F32, "yt")
        for ti in range(NT):
            t0 = ti * 128
            w = 0 if ti < NLT else 1
            oT = oTp.next()
            kb.dma("sp", oT.v(), s.QKT[16:24, :, t0:t0 + 128].rearrange("c p t -> p c t"))
            s.out_proj_res(ti, oT, w_out, mod[(w, 2)], psY, xp, yt)
        kb.pop()
        kb.pop()

    def layer_delta(s, l, j):
        kb = s.kb
        N, T, NT, NLT = s.N, s.T, s.NT, s.NLT
        kb.push()
        mod = s.adaln(l, [0, 1, 2], s.g_mix[l:l + 1, :])
        ab_all = kb.tile([128, NT, 32], F32, "ab_all")
        gb_all = kb.tile([128, NT, 32], F32, "gb_all")
        kb.push()
        stage = kb.pool(2, [128, 8, 512], F32, "stage")
        w_in = kb.tile([128, 8, 4128], BF16, "w_in")
        s.load_w_bf16(w_in, s.a_w_in[j], 4128, stage)
        xp = kb.pool(2, [128, D], F32, "x")
        junk = kb.tile([128, D], F32, "junk")
        tmpf = kb.tile([128, D], F32, "tmpf")
        ssp = kb.pool(2, [128, 2], F32, "ss")
        hbp = kb.pool(2, [128, D], BF16, "hb")
        hTp = kb.pool(2, [128, 8, 128], BF16, "hT")
        psT = kb.pool(2, [128, 512], F32, "psT", space="psum")
        psP = kb.pool(4, [128, 512], F32, "psP", space="psum")
        pap = kb.pool(2, [128, 3072], BF16, "pa")
        zbp = kb.pool(2, [128, D], BF16, "zb")
        chunks = [(n0, min(512, 4128 - n0)) for n0 in range(0, 4128, 512)]
        for ti in range(NT):
            w = 0 if ti < NLT else 1
            t0 = ti * 128
            xt = xp.next()
            kb.dma("sp", xt.v(), s.X[t0:t0 + 128, :])
            hb = hbp.next()
            s.norm_mod(xt, mod[(w, 1)], mod[(w, 0)], hb, tmpf, junk, ssp.next())
            hT = hTp.next()
            s.transpose_chunks(hT.v(), hb.v(), 8, psT, s.ident_b)
            pa = pap.next()
            zb = zbp.next()
            for ci, (n0, nsz) in enumerate(chunks):
                ps = psP.next()
                for k in range(8):
                    kb.mm(ps[:, 0:nsz], hT[:, k, :], w_in[:, k, n0:n0 + nsz], start=(k == 0), stop=(k == 7))
                eng = "act" if ci % 2 == 0 else "dve"
                if n0 < 3072:
                    kb.copy(pa[:, n0:n0 + nsz], ps[:, 0:nsz], eng=eng)
                elif n0 < 4096:
                    kb.copy(zb[:, n0 - 3072:n0 - 3072 + nsz], ps[:, 0:nsz], eng=eng)
                else:
                    kb.copy(ab_all[:, ti, :], ps[:, 0:32], eng=eng)
            kb.dma("pool", s.PA[t0:t0 + 128, :], pa.v())
            kb.dma("pool", s.H[t0:t0 + 128, :], zb.v())
        kb.pop()
        if getattr(s, "dbg_stop", 9) <= 1:
            kb.pop(); return
        kb.push()
        wc = []
        for k in range(5):
            t = kb.tile([128, 3072], F32, "wc%d" % k)
            kb.dma("sp", t.v(), s.a_conv[j, k:k + 1, :].to_broadcast([128, 3072]))
            wc.append(t)
        nA = kb.tile([128, 16], F32, "nA")
        kb.dma("sp", nA.v(), s.a_log[j:j + 1].rearrange("o d h -> o (d h)").to_broadcast([128, 16]))
        kb.act(nA.v(), nA.v(), AF.Exp)
        kb.ts(nA.v(), nA.v(), -1.0, None, ALU.mult)
        dtb = kb.tile([128, 16], F32, "dtb")
        kb.dma("sp", dtb.v(), s.a_dt_bias[j:j + 1].rearrange("o d h -> o (d h)").to_broadcast([128, 16]))
        onec = kb.tile([128, 1], F32, "onec")
        kb.memset(onec.v(), 1.0)
        shp = kb.pool(5, [128, 3072], BF16, "sh")
        acc = kb.tile([128, 3072], F32, "acc")
        tmp = kb.tile([128, 3072], F32, "tmp")
        qkv = kb.tile([128, 3072], F32, "qkv")
        sq = kb.tile([128, 2048], F32, "sq")
        ssh = kb.tile([128, 16], F32, "ssh")
        gt = kb.tile([128, 16], F32, "gt")
        qknp = kb.pool(1, [128, 2048], BF16, "qkn")
        kvsp = kb.pool(1, [128, 2048], F32, "kvs")
        qkTp = kb.pool(1, [128, 16, 128], BF16, "qkT")
        psT = kb.pool(2, [128, 512], F32, "psT", space="psum")
        for ti in range(NT):
            seg0, seg1 = (0, N) if ti < NLT else (N, T)
            t0 = ti * 128
            shs = []
            for k in range(5):
                r0 = t0 - 2 + k
                r1 = r0 + 128
                lo = max(r0, seg0)
                hi = min(r1, seg1)
                sh = shp.next()
                if lo > r0 or hi < r1:
                    kb.memset(sh.v(), 0.0)
                kb.dma("sp", sh[lo - r0:hi - r0, :], s.PA[lo:hi, :])
                shs.append(sh)
            kb.tt(acc.v(), shs[0].v(), wc[0].v(), ALU.mult)
            for k in range(1, 5):
                kb.tt(tmp.v(), shs[k].v(), wc[k].v(), ALU.mult, eng="pool")
                kb.tt(acc.v(), acc.v(), tmp.v(), ALU.add)
            kb.act(qkv.v(), acc.v(), AF.Silu)
            x3 = qkv[:, 0:2048].re("p (h d) -> p h d", d=128)
            kb.tt(sq.v().re("p (h d) -> p h d", d=128), x3, x3, ALU.mult)
            kb.red(ssh.v(), sq.v().re("p (h d) -> p h d", d=128), ALU.add)
            kb.act(ssh.v(), ssh.v(), AF.Sqrt, bias=s.epst[:, 0:1], scale=1.0)
            kb.recip(ssh.v(), ssh.v())
            kb.ts(ssh[:, 0:8], ssh[:, 0:8], 128.0 ** -0.5, None, ALU.mult)
            qkn = qknp.next()
            rb = ssh.v().re("p (h o) -> p h o", o=1).bc([128, 16, 128])
            kb.tt(qkn.v().re("p (h d) -> p h d", d=128), x3, rb, ALU.mult)
            kvs = kvsp.next()
            kb.tt(kvs[:, 0:1024].re("p (h d) -> p h d", d=128), qkv[:, 1024:2048].re("p (h d) -> p h d", d=128),
                  ssh[:, 8:16].re("p (h o) -> p h o", o=1).bc([128, 8, 128]), ALU.mult)
            kb.copy(kvs[:, 1024:2048], qkv[:, 2048:3072], eng="act")
            qkT = qkTp.next()
            s.transpose_chunks(qkT.v(), qkn.v(), 16, psT, s.ident_b)
            kb.dma("pool", s.QKT[0:16, :, t0:t0 + 128].rearrange("c p t -> p c t"), qkT.v())
            kb.dma("pool", s.KVS[t0:t0 + 128, :], kvs.v())
            kb.tt(gt.v(), ab_all[:, ti, 0:16], dtb.v(), ALU.add)
            kb.act(gt.v(), gt.v(), AF.Exp)
            kb.act(gt.v(), gt.v(), AF.Ln, bias=onec[:, 0:1])
            kb.tt(gb_all[:, ti, 0:16], gt.v(), nA.v(), ALU.mult)
            kb.act(gb_all[:, ti, 16:32], ab_all[:, ti, 16:32], AF.Sigmoid)
        kb.pop()
        if getattr(s, "dbg_stop", 9) <= 2:
            kb.pop(); return
        for d in (0, 1):
            if getattr(s, "dbg_stop", 9) <= 3 + d - 1 + 0 and d == 1:
                break
            kb.push()
            ones_f = kb.tile([128, 128], F32, "ones_f")
            kb.memset(ones_f.v(), 1.0)
            tril_i = kb.tile([128, 128], F32, "tril_i")
            triu_i = kb.tile([128, 128], F32, "triu_i")
            tril_s = kb.tile([128, 128], F32, "tril_s")
            triu_s = kb.tile([128, 128], F32, "triu_s")
            kb.dma("sp", tril_i.v(), s.k_tril)
            kb.dma("sp", triu_i.v(), s.k_triu)
            kb.tt(tril_s.v(), tril_i.v(), s.ident_f.v(), ALU.subtract)
            kb.tt(triu_s.v(), triu_i.v(), s.ident_f.v(), ALU.subtract)
            bd16 = kb.tile([128, 128], F32, "bd16")
            kb.dma("sp", bd16.v(), s.k_bd16)
            offm = []
            for li in range(3):
                t_ = kb.tile([128, 128], F32, "off%d" % li)
                kb.dma("sp", t_.v(), s.k_off[li])
                offm.append(t_)
            if d == 0:
                mA, mAT, mQK, cumM = tril_s, triu_s, triu_i, triu_i
                order = [NLT, NLT + 1] + list(range(NLT))
            else:
                mA, mAT, mQK, cumM = triu_s, tril_s, tril_i, tril_i
                order = [NLT + 1, NLT] + list(range(NLT - 1, -1, -1))
            S = [kb.tile([128, 128], F32, "S%d" % h_) for h_ in range(8)]
            for h_ in range(8):
                kb.memset(S[h_].v(), 0.0)
            psR = kb.pool(6, [128, 512], F32, "psR", space="psum")
            kvsp = kb.pool(1, [128, 2048], F32, "kvs")
            qkbp = kb.pool(2, [128, 16, 128], BF16, "qkb")
            qkfp = kb.pool(1, [128, 16, 128], F32, "qkf")
            sc = {nm: kb.pool(2, [128, 8], F32, nm) for nm in ("gcl2", "gam", "e_", "gamL", "bg", "lnb", "gcb", "nb", "tmp8")}
            gclp = kb.pool(2, [128, 16], F32, "gcl")
            GH = 4
            depth = {"P": 5, "Q": 5, "X": 2, "Xn": 2, "PL": 2, "QL": 2, "I1": 2, "I2": 2}
            mslots = [{nm: kb.pool(depth.get(nm, 1), [128, 128], F32, "%s%d" % (nm, sl)) for nm in
                       ("dg1", "dg2", "E1", "E2", "E3", "P", "Q", "X", "Xn", "PL", "QL", "I1", "I2", "Mqk", "bV", "bgK",
                        "Kt", "nWT", "Vn", "o1s")} for sl in range(GH)]
            otp = kb.pool(1, [128, D], F32, "ot")
            if d == 1:
                stage = kb.pool(1, [128, 8, 256], F32, "stage")
                w_out = kb.tile([128, 8, D], BF16, "w_out")
                s.load_w_bf16(w_out, s.a_w_out[j], D, stage, chunk=256)
                gout = kb.tile([128, 128], F32, "gout")
                kb.dma("sp", gout.v(), s.a_g_out[j:j + 1, :].to_broadcast([128, 128]))
                ofp = kb.pool(1, [128, D], F32, "of")
                zp = kb.pool(1, [128, D], BF16, "z")
                zf = kb.tile([128, D], F32, "zf")
                sqo = kb.tile([128, D], F32, "sqo")
                rs8 = kb.pool(2, [128, 8], F32, "rs8")
                obp = kb.pool(1, [128, D], BF16, "ob")
                oTp = kb.pool(1, [128, 8, 128], BF16, "oT")
                psY = kb.pool(2, [128, 512], F32, "psY", space="psum")
                xp = kb.pool(1, [128, D], F32, "x")
                yt = kb.tile([128, D], F32, "yt")
            for c in order[:int(_os.environ.get('DBG_C', '999'))]:
                t0 = c * 128
                kvs = kvsp.next()
                kb.dma("sp", kvs.v(), s.KVS[t0:t0 + 128, :])
                qkb = qkbp.next()
                kb.dma("sp", qkb.v(), s.QKT[0:16, :, t0:t0 + 128].rearrange("c p t -> p c t"))
                qkf = qkfp.next()
                kb.copy(qkf.v(), qkb.v(), eng="act")
                g8 = gb_all[:, c, d * 8:(d + 1) * 8]
                b8 = gb_all[:, c, 16 + d * 8:16 + (d + 1) * 8]
                ps = psR.next()
                kb.mm(ps[:, 0:8], cumM.v(), g8)
                kb.mm(ps[:, 8:16], ones_f.v(), g8)
                gcl = gclp.next()
                kb.copy(gcl.v(), ps[:, 0:16])
                gc = gcl[:, 0:8]
                gl = gcl[:, 8:16]
                gam = sc["gam"].next(); e_ = sc["e_"].next(); gamL = sc["gamL"].next(); bg = sc["bg"].next()
                lnb = sc["lnb"].next(); gcb = sc["gcb"].next(); nb = sc["nb"].next(); tmp8 = sc["tmp8"].next()
                kb.act(gam.v(), gc, AF.Exp)
                kb.tt(tmp8.v(), gl, gc, ALU.subtract)
                kb.act(e_.v(), tmp8.v(), AF.Exp)
                kb.act(gamL.v(), gl, AF.Exp)
                kb.tt(bg.v(), b8, gam.v(), ALU.mult)
                kb.act(lnb.v(), b8, AF.Ln)
                kb.tt(gcb.v(), gc, lnb.v(), ALU.add)
                kb.ts(nb.v(), b8, -1.0, None, ALU.mult)
                ot = otp.next()

                def head_gen(h, mp):
                    Kh = kvs[:, h * 128:(h + 1) * 128]
                    Vh = kvs[:, 1024 + h * 128:1024 + (h + 1) * 128]
                    QT = qkf[:, h, :]
                    KT = qkf[:, 8 + h, :]
                    gch = gcl[:, h:h + 1]
                    Sh = S[h]
                    dg1 = mp["dg1"].next(); dg2 = mp["dg2"].next()
                    kb.ts(dg1.v(), s.ident_f.v(), gch, None, ALU.mult)
                    kb.ts(dg2.v(), s.ident_f.v(), gcb[:, h:h + 1], None, ALU.mult)
                    bV = mp["bV"].next(); bgK = mp["bgK"].next(); Kt = mp["Kt"].next()
                    kb.ts(bV.v(), Vh, b8[:, h:h + 1], None, ALU.mult)
                    kb.ts(bgK.v(), Kh, bg[:, h:h + 1], None, ALU.mult)
                    kb.ts(Kt.v(), Kh, e_[:, h:h + 1], None, ALU.mult)
                    yield
                    pAB = psR.next()
                    kb.mm(pAB[:, 0:128], KT, KT)
                    kb.mm(pAB[:, 128:256], KT, QT)
                    kb.mm(pAB[:, 256:384], ones_f.v(), dg1.v())
                    kb.mm(pAB[:, 384:512], ones_f.v(), dg2.v())
                    yield
                    pKK0 = pAB[:, 0:128]; pQK = pAB[:, 128:256]; bc = pAB[:, 256:384]; bc2 = pAB[:, 384:512]
                    E1 = mp["E1"].next(); E2 = mp["E2"].next(); E3 = mp["E3"].next()
                    P0 = mp["P"].next(); Q0 = mp["Q"].next(); Mqk = mp["Mqk"].next()
                    kb.ts(E1.v(), bc, gch, 0.0, ALU.subtract, ALU.max)
                    kb.ts(E2.v(), bc2, gch, 0.0, ALU.subtract, ALU.min)
                    kb.ts(E3.v(), bc, gch, 0.0, ALU.subtract, ALU.min)
                    kb.act(E1.v(), E1.v(), AF.Exp, scale=-1.0)
                    kb.act(E2.v(), E2.v(), AF.Exp)
                    kb.act(E3.v(), E3.v(), AF.Exp)
                    kb.tt(E1.v(), E1.v(), pKK0, ALU.mult)
                    kb.stt(P0.v(), E1.v(), nb[:, h:h + 1], mA.v(), ALU.mult, ALU.mult)
                    kb.tt(E2.v(), E2.v(), pKK0, ALU.mult)
                    kb.stt(Q0.v(), E2.v(), -1.0, mAT.v(), ALU.mult, ALU.mult)
                    kb.tt(E3.v(), E3.v(), pQK, ALU.mult)
                    kb.tt(Mqk.v(), E3.v(), mQK.v(), ALU.mult)
                    Pb = mp["P"].next(); Qb = mp["Q"].next()
                    kb.tt(Pb.v(), P0.v(), bd16.v(), ALU.mult)
                    kb.tt(Qb.v(), Q0.v(), bd16.v(), ALU.mult)
                    Xt = mp["X"].next(); Xn = mp["Xn"].next()
                    kb.tt(Xt.v(), Qb.v(), s.ident_f.v(), ALU.add)
                    kb.tt(Xn.v(), Pb.v(), s.ident_f.v(), ALU.add)
                    yield
                    Pk, Qk = Pb, Qb
                    for k in range(1, 4):
                        pp = psR.next()
                        kb.mm(pp[:, 0:128], Qk.v(), Pk.v())
                        kb.mm(pp[:, 128:256], Pk.v(), Qk.v())
                        yield
                        Pn = mp["P"].next(); Qn = mp["Q"].next()
                        kb.copy(Pn.v(), pp[:, 0:128], eng="act")
                        kb.copy(Qn.v(), pp[:, 128:256], eng="act")
                        yield
                        pa = psR.next()
                        kb.mm(pa[:, 0:128], Pn.v(), Xt.v())
                        kb.mm(pa[:, 128:256], Qn.v(), Xn.v())
                        yield
                        Xt2 = mp["X"].next(); Xn2 = mp["Xn"].next()
                        kb.tt(Xt2.v(), Xt.v(), pa[:, 0:128], ALU.add)
                        kb.tt(Xn2.v(), Xn.v(), pa[:, 128:256], ALU.add)
                        Pk, Qk, Xt, Xn = Pn, Qn, Xt2, Xn2
                    for li in range(3):
                        last = li == 2
                        PL = mp["PL"].next()
                        kb.tt(PL.v(), P0.v(), offm[li].v(), ALU.mult)
                        if not last:
                            QL = mp["QL"].next()
                            kb.tt(QL.v(), Q0.v(), offm[li].v(), ALU.mult)
                        yield
                        pi = psR.next()
                        kb.mm(pi[:, 0:128], PL.v(), Xt.v())
                        if not last:
                            kb.mm(pi[:, 128:256], QL.v(), Xn.v())
                        yield
                        I1 = mp["I1"].next()
                        kb.copy(I1.v(), pi[:, 0:128], eng="act")
                        if not last:
                            I2 = mp["I2"].next()
                            kb.copy(I2.v(), pi[:, 128:256], eng="act")
                        yield
                        po2 = psR.next()
                        kb.mm(po2[:, 0:128], Xn.v(), I1.v())
                        if not last:
                            kb.mm(po2[:, 128:256], Xt.v(), I2.v())
                        yield
                        Xt2 = mp["X"].next()
                        kb.tt(Xt2.v(), Xt.v(), po2[:, 0:128], ALU.add)
                        if not last:
                            Xn2 = mp["Xn"].next()
                            kb.tt(Xn2.v(), Xn.v(), po2[:, 128:256], ALU.add)
                            Xn = Xn2
                        Xt = Xt2
                    X = Xt
                    yield
                    pW = psR.next()
                    kb.mm(pW[:, 0:128], bgK.v(), X.v())
                    yield
                    nWT = mp["nWT"].next()
                    kb.ts(nWT.v(), pW[:, 0:128], -1.0, None, ALU.mult)
                    yield
                    pV = psR.next()
                    kb.mm(pV[:, 0:128], X.v(), bV.v(), start=True, stop=False)
                    kb.mm(pV[:, 0:128], nWT.v(), Sh.v(), start=False, stop=True)
                    yield
                    Vn = mp["Vn"].next()
                    kb.copy(Vn.v(), pV[:, 0:128])
                    yield
                    pO = psR.next()
                    kb.mm(pO[:, 0:128], QT, Sh.v())
                    kb.mm(pO[:, 128:256], Mqk.v(), Vn.v())
                    kb.mm(pO[:, 256:384], Kt.v(), Vn.v())
                    yield
                    o1s = mp["o1s"].next()
                    kb.ts(o1s.v(), pO[:, 0:128], gam[:, h:h + 1], None, ALU.mult)
                    kb.tt(ot[:, h * 128:(h + 1) * 128], o1s.v(), pO[:, 128:256], ALU.add)
                    kb.stt(Sh.v(), Sh.v(), gamL[:, h:h + 1], pO[:, 256:384], ALU.mult, ALU.add)

                for g0 in range(0, 8, GH):
                    gens = [head_gen(h, mslots[h - g0]) for h in range(g0, min(8, g0 + GH))]
                    while gens:
                        for g in list(gens):
                            try:
                                next(g)
                            except StopIteration:
                                gens.remove(g)
                if d == 0:
                    kb.dma("pool", s.OF[t0:t0 + 128, :], ot.v())
                else:
                    w = 0 if c < NLT else 1
                    of = ofp.next()
                    kb.dma("sp", of.v(), s.OF[t0:t0 + 128, :])
                    kb.tt(ot.v(), ot.v(), of.v(), ALU.add)
                    o3 = ot.v().re("p (h d) -> p h d", d=128)
                    kb.tt(sqo.v(), ot.v(), ot.v(), ALU.mult)
                    r8 = rs8.next()
                    kb.red(r8.v(), sqo.v().re("p (h d) -> p h d", d=128), ALU.add)
                    kb.act(r8.v(), r8.v(), AF.Sqrt, bias=s.epst[:, 0:1], scale=1.0 / 128)
                    kb.recip(r8.v(), r8.v())
                    kb.tt(o3, o3, r8.v().re("p (h o) -> p h o", o=1).bc([128, 8, 128]), ALU.mult)
                    kb.tt(o3, o3, gout.v().re("p (o d) -> p o d", o=1).bc([128, 8, 128]), ALU.mult)
                    z = zp.next()
                    kb.dma("sp", z.v(), s.H[t0:t0 + 128, :])
                    kb.act(zf.v(), z.v(), AF.Silu)
                    ob = obp.next()
                    kb.tt(ob.v(), ot.v(), zf.v(), ALU.mult)
                    oT = oTp.next()
                    s.transpose_chunks(oT.v(), ob.v(), 8, psY, s.ident_b)
                    s.out_proj_res(c, oT, w_out, mod[(w, 2)], psY, xp, yt)
            kb.pop()
        kb.pop()

    def moe(s, l):
        kb = s.kb
        N, T, NT, NLT, S = s.N, s.T, s.NT, s.NLT, s.S
        kb.push()
        mod = s.adaln(l, [3, 4, 5], s.g_ffn[l:l + 1, :])
        affT = kb.tile([16, T], F32, "affT")
        slot_i = kb.tile([128, NT, 16], I32, "slot_i")
        gateT = kb.tile([128, NT, 16], F32, "gateT")
        kb.push()
        wr = kb.tile([128, 8, 16], F32, "wr")
        kb.dma("sp", wr.v(), s.w_router[l].rearrange("(k p) e -> p k e", p=128))
        xp = kb.pool(2, [128, D], F32, "x")
        junk = kb.tile([128, D], F32, "junk")
        tmpf = kb.tile([128, D], F32, "tmpf")
        ssp = kb.pool(2, [128, 2], F32, "ss")
        hfp = kb.pool(2, [128, D], F32, "hf")
        hbp = kb.pool(2, [128, D], BF16, "hb")
        hTf = kb.pool(2, [128, 8, 128], F32, "hTf")
        psT = kb.pool(4, [128, 512], F32, "psT", space="psum")
        psL = kb.pool(2, [128, 512], F32, "psL", space="psum")
        smp = kb.pool(2, [128, 4], F32, "sm")
        ep = kb.pool(2, [128, 16], F32, "e")
        for ti in range(NT):
            w = 0 if ti < NLT else 1
            t0 = ti * 128
            xt = xp.next()
            kb.dma("sp", xt.v(), s.X[t0:t0 + 128, :])
            hf = hfp.next()
            s.norm_mod(xt, mod[(w, 4)], mod[(w, 3)], hf, tmpf, junk, ssp.next())
            hb = hbp.next()
            kb.copy(hb.v(), hf.v(), eng="act")
            kb.dma("pool", s.H[t0:t0 + 128, :], hb.v())
            hT = hTf.next()
            for half in range(2):
                ps = psT.next()
                for i in range(4):
                    k = half * 4 + i
                    kb.tr(ps[:, i * 128:(i + 1) * 128], hf[:, k * 128:(k + 1) * 128], s.ident_f.v())
                kb.copy(hT[:, half * 4:half * 4 + 4, :], ps.v().re("p (k t) -> p k t", t=128), eng="act")
            pl = psL.next()
            for k in range(8):
                kb.mm(pl[:, 0:16], hT[:, k, :], wr[:, k, :], start=(k == 0), stop=(k == 7))
            sm = smp.next()
            e = ep.next()
            kb.red(sm[:, 0:1], pl[:, 0:16], ALU.max)
            kb.ts(sm[:, 1:2], sm[:, 0:1], -1.0, None, ALU.mult)
            kb.memset(sm[:, 2:3], 0.0)
            kb.act(e.v(), pl[:, 0:16], AF.Exp, bias=sm[:, 1:2], accum_out=sm[:, 2:3])
            kb.recip(sm[:, 3:4], sm[:, 2:3])
            kb.ts(e.v(), e.v(), sm[:, 3:4], None, ALU.mult)
            pt = psL.next()
            kb.tr(pt[0:16, 0:128], e.v(), s.ident_f.v())
            kb.copy(affT[:, t0:t0 + 128], pt[0:16, 0:128])
        kb.pop()
        kb.push()
        escal = kb.tile([16, 2], F32, "escal")
        kb.dma("sp", escal.v(), s.k_escal)
        Wmax = max(N, NCTX)
        work = kb.tile([16, Wmax], F32, "work")
        ones = kb.tile([16, 1], F32, "ones")
        kb.memset(ones.v(), 1.0)
        t8 = kb.tile([16, 8], F32, "t8")
        mask = kb.tile([16, T], F32, "mask")
        slotf = kb.tile([16, T], F32, "slotf")
        gsel = affT
        for (c0, c1, cap, ecol) in ((0, N, s.cap_lat, 0), (N, T, s.cap_ctx, 1)):
            n = c1 - c0
            src = affT[:, c0:c1]
            for r in range(cap // 8):
                kb.emit("dve", lambda E, src=src: E.max(out=t8.h[:], in_=src.ap), [src], [t8])
                kb.emit("dve", lambda E, src=src, n=n: E.match_replace(out=work.h[:, 0:n], in_to_replace=t8.h[:],
                                                                        in_values=src.ap, imm_value=0.0),
                        [src, t8], [work])
                src = work[:, 0:n]
            kb.ts(mask[:, c0:c1], work[:, 0:n], 0.0, None, ALU.is_equal)
            kb.emit("dve", lambda E, n=n, c0=c0, c1=c1: E.tensor_tensor_scan(
                out=slotf.h[:, c0:c1], data0=ones.h[:, 0:1].to_broadcast([16, n]), data1=mask.h[:, c0:c1], initial=0.0,
                op0=ALU.mult, op1=ALU.add), [ones, mask], [slotf])
            kb.stt(slotf[:, c0:c1], slotf[:, c0:c1], escal[:, ecol:ecol + 1], mask[:, c0:c1], ALU.add, ALU.mult)
            kb.ts(slotf[:, c0:c1], slotf[:, c0:c1], BIG, None, ALU.add)
        kb.tt(gsel.v(), affT.v(), mask.v(), ALU.mult)
        psS = kb.pool(2, [128, 512], F32, "psS", space="psum")
        slt = kb.tile([128, NT, 16], F32, "slt")
        for (srcT, dst) in ((slotf, slt), (gsel, gateT)):
            for g0 in range(0, NT, 32):
                g1 = min(NT, g0 + 32)
                ps = psS.next()
                for ti in range(g0, g1):
                    kb.tr(ps[:, (ti - g0) * 16:(ti - g0 + 1) * 16], srcT[:, ti * 128:(ti + 1) * 128], s.ident_f[0:16, 0:16])
                kb.copy(dst[:, g0:g1, :], ps[:, 0:(g1 - g0) * 16].re("p (t e) -> p t e", e=16))
        kb.copy(slot_i.v(), slt.v())
        kb.pop()
        kb.push()
        hbp = kb.pool(3, [128, D], BF16, "hb")
        for ti in range(NT):
            t0 = ti * 128
            hb = hbp.next()
            kb.dma("sp", hb.v(), s.H[t0:t0 + 128, :])
            for e in range(16):
                kb.dma("pool", s.XG, hb.v(), indirect=dict(idx=slot_i[:, ti, e:e + 1], side="out", bound=16 * S - 1))
        kb.pop()
        kb.push()
        stage = kb.pool(1, [128, 8, 512], F32, "stage")
        wgu = kb.tile([128, 8, 2 * D], BF16, "wgu")
        wd = kb.tile([128, 8, D], BF16, "wd")
        nst = (S + 127) // 128
        stiles = [(i * 128, min(128, S - i * 128)) for i in range(nst)]
        nchunks = [(n0, min(512, S - n0)) for n0 in range(0, S, 512)]
        xgp = kb.pool(1, [128, nst, D], BF16, "xg")
        xgT = kb.tile([128, 8, S], BF16, "xgT")
        actT = kb.tile([128, 8, S], BF16, "actT")
        psX = kb.pool(2, [128, 512], F32, "psX", space="psum")
        psG = kb.pool(2, [128, 512], F32, "psG", space="psum")
        psU = kb.pool(2, [128, 512], F32, "psU", space="psum")
        psD = kb.pool(2, [128, 512], F32, "psD", space="psum")
        sgp = kb.pool(2, [128, 512], F32, "sg")
        ysp = kb.pool(2, [128, D], BF16, "ys")
        for e in range(16):
            s.load_w_bf16(wgu, s.w_gate_up[l, e], 2 * D, stage)
            s.load_w_bf16(wd, s.w_down[l, e], D, stage)
            xg = xgp.next()
            for i, (s0, sz) in enumerate(stiles):
                kb.dma("sp", xg[0:sz, i, :], s.XG[e * S + s0:e * S + s0 + sz, :])
            for i, (s0, sz) in enumerate(stiles):
                ps = psX.next()
                psb = ps.v().bitcast(BF16)
                for k in range(8):
                    kb.tr(psb[:, k * 128:k * 128 + sz], xg[0:sz, i, k * 128:(k + 1) * 128], s.ident_b[0:sz, 0:sz])
                kb.copy(xgT[:, :, s0:s0 + sz], psb.re("p (k t) -> p k t", t=128)[:, :, 0:sz], eng="act")
            for fc in range(8):
                for (n0, nsz) in nchunks:
                    pg = psG.next()
                    pu = psU.next()
                    for k in range(8):
                        kb.mm(pg[:, 0:nsz], wgu[:, k, fc * 128:(fc + 1) * 128], xgT[:, k, n0:n0 + nsz],
                              start=(k == 0), stop=(k == 7))
                    for k in range(8):
                        kb.mm(pu[:, 0:nsz], wgu[:, k, D + fc * 128:D + (fc + 1) * 128], xgT[:, k, n0:n0 + nsz],
                              start=(k == 0), stop=(k == 7))
                    sg = sgp.next()
                    kb.act(sg[:, 0:nsz], pg[:, 0:nsz], AF.Silu)
                    kb.tt(actT[:, fc, n0:n0 + nsz], sg[:, 0:nsz], pu[:, 0:nsz], ALU.mult)
            for i, (s0, sz) in enumerate(stiles):
                ys = ysp.next()
                for nb in range(2):
                    pd = psD.next()
                    for fk in range(8):
                        kb.mm(pd[0:sz, :], actT[:, fk, s0:s0 + sz], wd[:, fk, nb * 512:(nb + 1) * 512],
                              start=(fk == 0), stop=(fk == 7))
                    kb.copy(ys[0:sz, nb * 512:(nb + 1) * 512], pd[0:sz, :], eng="act")
                kb.dma("pool", s.Y[e * S + s0:e * S + s0 + sz, :], ys[0:sz, :])
        kb.pop()
        kb.push()
        gp = kb.pool(4, [128, D], BF16, "gath")
        for b in gp.bufs:
            kb.memset(b.v(), 0.0)
        accp = kb.pool(2, [128, D], F32, "acc")
        xp = kb.pool(2, [128, D], F32, "x")
        for ti in range(NT):
            w = 0 if ti < NLT else 1
            t0 = ti * 128
            acc = accp.next()
            kb.memset(acc.v(), 0.0)
            for e in range(16):
                g = gp.next()
                kb.dma("pool", g.v(), s.Y, indirect=dict(idx=slot_i[:, ti, e:e + 1], side="in", bound=16 * S - 1))
                kb.stt(acc.v(), g.v(), gateT[:, ti, e:e + 1], acc.v(), ALU.mult, ALU.add)
            xt = xp.next()
            kb.dma("sp", xt.v(), s.X[t0:t0 + 128, :])
            kb.tt(acc.v(), acc.v(), mod[(w, 5)].v(), ALU.mult)
            kb.tt(xt.v(), xt.v(), acc.v(), ALU.add)
            kb.dma("pool", s.X[t0:t0 + 128, :], xt.v())
        kb.pop()
        kb.pop()

    def finish(s):
        kb = s.kb
        for r0 in range(0, s.T, 1024):
            r1 = min(s.T, r0 + 1024)
            kb.dma("sp", s.out[r0:r1, :], s.X[r0:r1, :])
        kb.barrier()


def host_consts(N):
    T = N + NCTX
    S = 2 * N // 16 + 2 * NCTX // 16
    GRID_W = 64
    rows = N // GRID_W
    t_row = np.repeat(np.arange(rows), GRID_W).astype(np.float32)
    t_col = np.tile(np.arange(GRID_W), rows).astype(np.float32)
    n_freq = 16
    inv = (10000.0 ** (-np.arange(n_freq, dtype=np.float32) / n_freq)).astype(np.float32)
    ang = np.concatenate([t_row[:, None] * inv, t_col[:, None] * inv], -1).astype(np.float32)
    cos = np.ones((T, 32), np.float32)
    sin = np.zeros((T, 32), np.float32)
    cos[:N] = np.cos(ang)
    sin[:N] = np.sin(ang)
    a = np.arange(128)
    tril = (a[None, :] <= a[:, None]).astype(np.float32)
    triu = (a[:, None] <= a[None, :]).astype(np.float32)
    e = np.arange(16, dtype=np.float32)
    escal = np.stack([e * S - 1 - BIG, e * S - 1 - BIG + 2 * N // 16], 1).astype(np.float32)
    ii = a[:, None]
    jj = a[None, :]
    bd16 = ((ii // 16) == (jj // 16)).astype(np.float32)
    offs = []
    for b in (16, 32, 64):
        same2b = (ii // (2 * b)) == (jj // (2 * b))
        diffb = (ii // b) != (jj // b)
        offs.append((same2b & diffb).astype(np.float32))
    return dict(k_bd16=bd16, k_off=np.stack(offs, 0), k_ident=np.eye(128, dtype=np.float32), k_cos=cos, k_sin=sin, k_tril=tril, k_triu=triu, k_escal=escal)


from concourse.bass_utils import run_bass_kernel_spmd

N_FULL = 8192


def kernel(**inputs):
    N = N_FULL
    mk = MK(N)
    mk.setup()
    for l in range(4):
        kind, j = l % 3, l // 3
        getattr(mk, ["layer_delta", "layer_swa", "layer_diff"][kind])(l, j)
        mk.moe(l)
    mk.finish()
    consts = host_consts(N)
    in_maps = []
    B = inputs["x"].shape[0]
    for b in range(B):
        im = {}
        for k in mk.inp:
            if k.startswith("k_"):
                im[k] = consts[k]
            elif k in ("x", "ctx", "c"):
                im[k] = np.ascontiguousarray(np.asarray(inputs[k])[b])
            else:
                im[k] = np.ascontiguousarray(np.asarray(inputs[k]))
        in_maps.append(im)
    res = run_bass_kernel_spmd(mk.nc, in_maps, core_ids=list(range(B)))
    out = np.stack([np.asarray(r["out"])[:N] for r in res.results], 0).astype(np.float32)
    return out
```
